# Optimizing a Trainium2 kernel written in Bass

```python
import math
import jax, jax.numpy as jnp
from jax import lax
import numpy as np

D_MODEL = 4096
BATCH = 4
SEQ = 2048
DEPTH = 1

MIX_WIDTH = D_MODEL
ATTN_WIDTH = MIX_WIDTH // 2
SSM_WIDTH = MIX_WIDTH - ATTN_WIDTH
HEAD_DIM = 128
ATTN_HEADS = ATTN_WIDTH // HEAD_DIM
MOBA_BLOCK = 256
MOBA_TOPK = 3
QUERY_CHUNK = 32
NUM_BUCKETS = 32
MAX_DISTANCE = 128
SSM_GROUP_CH = 16
SSM_GROUPS = SSM_WIDTH // SSM_GROUP_CH
SSM_STATE = 64
IN_PROJ_WIDTH = 3 * ATTN_WIDTH + SSM_WIDTH
PEER_HEADS = 8
PEER_NKEYS = 128
PEER_EXPERTS = PEER_NKEYS * PEER_NKEYS
PEER_TOPK = 16
PEER_QDIM = 256
PEER_TOKEN_CHUNK = 128
PEER_V_SCALE = 0.25
RMS_EPS = 1e-6
NEG_INF = -1e30

kernel_name = 'hymba_moba_s5_peer_block'


def rmsnorm(x, gain):
    xf = x.astype(jnp.float32)
    y = xf * lax.rsqrt(jnp.mean(xf * xf, axis=-1, keepdims=True) + RMS_EPS)
    return (y * gain.astype(jnp.float32)).astype(x.dtype)


def t5_bucket(dist):
    n = jnp.maximum(dist, 0)
    max_exact = NUM_BUCKETS // 2
    ratio = jnp.log(jnp.maximum(n, 1).astype(jnp.float32) / max_exact) / math.log(MAX_DISTANCE / max_exact)
    large = max_exact + (ratio * (NUM_BUCKETS - max_exact)).astype(jnp.int32)
    large = jnp.minimum(large, NUM_BUCKETS - 1)
    return jnp.where(n < max_exact, n, large)


def moba_attention(q, k, v, rel_bias):
    Bsz, S, H, Dh = q.shape
    nb = -(-S // MOBA_BLOCK)
    pad = nb * MOBA_BLOCK - S
    q = q.transpose(0, 2, 1, 3)
    kp = jnp.pad(k.transpose(0, 2, 1, 3), ((0, 0), (0, 0), (0, pad), (0, 0))).reshape(Bsz, H, nb, MOBA_BLOCK, Dh)
    vp = jnp.pad(v.transpose(0, 2, 1, 3), ((0, 0), (0, 0), (0, pad), (0, 0))).reshape(Bsz, H, nb, MOBA_BLOCK, Dh)
    kmean = jnp.mean(kp.astype(jnp.float32), axis=3)
    qblk = jnp.arange(S) // MOBA_BLOCK
    gate = jnp.einsum('bhsd,bhnd->bhsn', q.astype(jnp.float32), kmean)
    past = jnp.arange(nb)[None, :] < qblk[:, None]
    gate = jnp.where(past, gate, NEG_INF)
    ktop = min(MOBA_TOPK, nb)
    _, sel = lax.top_k(gate, ktop)
    valid = sel < qblk[None, None, :, None]
    nq = S // QUERY_CHUNK
    scale = Dh ** -0.5
    hidx = jnp.arange(H)[:, None, None]
    offs = jnp.arange(MOBA_BLOCK)

    def one_chunk(i):
        b = i // nq
        q0 = (i % nq) * QUERY_CHUNK
        qc = lax.dynamic_slice_in_dim(q[b], q0, QUERY_CHUNK, axis=1)
        kb, vb = kp[b], vp[b]
        selc = lax.dynamic_slice_in_dim(sel[b], q0, QUERY_CHUNK, axis=1)
        validc = lax.dynamic_slice_in_dim(valid[b], q0, QUERY_CHUNK, axis=1)
        ksel = kb[hidx, selc]
        vsel = vb[hidx, selc].reshape(H, QUERY_CHUNK, ktop * MOBA_BLOCK, Dh)
        own = q0 // MOBA_BLOCK
        kown = lax.dynamic_index_in_dim(kb, own, axis=1, keepdims=False)
        vown = lax.dynamic_index_in_dim(vb, own, axis=1, keepdims=False)
        s_sel = jnp.einsum('hqd,hqnkd->hqnk', qc, ksel).reshape(H, QUERY_CHUNK, ktop * MOBA_BLOCK)
        s_own = jnp.einsum('hqd,hkd->hqk', qc, kown)
        qpos = q0 + jnp.arange(QUERY_CHUNK)
        kpos_sel = (selc[..., None] * MOBA_BLOCK + offs).reshape(H, QUERY_CHUNK, ktop * MOBA_BLOCK)
        kpos_own = own * MOBA_BLOCK + offs
        kpos = jnp.concatenate([kpos_sel, jnp.broadcast_to(kpos_own, (H, QUERY_CHUNK, MOBA_BLOCK))], axis=-1)
        mask_sel = jnp.broadcast_to(validc[..., None], (H, QUERY_CHUNK, ktop, MOBA_BLOCK)).reshape(H, QUERY_CHUNK, ktop * MOBA_BLOCK)
        mask_own = jnp.broadcast_to(kpos_own[None, None, :] <= qpos[None, :, None], (H, QUERY_CHUNK, MOBA_BLOCK))
        mask = jnp.concatenate([mask_sel, mask_own], axis=-1)
        bias = rel_bias[t5_bucket(qpos[None, :, None] - kpos), hidx]
        logits = jnp.concatenate([s_sel, s_own], axis=-1).astype(jnp.float32) * scale + bias.astype(jnp.float32)
        p = jax.nn.softmax(jnp.where(mask, logits, NEG_INF), axis=-1).astype(v.dtype)
        n_sel = ktop * MOBA_BLOCK
        return (jnp.einsum('hqk,hqkd->hqd', p[..., :n_sel], vsel)
                + jnp.einsum('hqk,hkd->hqd', p[..., n_sel:], vown))

    out = lax.map(one_chunk, jnp.arange(Bsz * nq))
    return out.reshape(Bsz, nq, H, QUERY_CHUNK, Dh).transpose(0, 1, 3, 2, 4).reshape(Bsz, S, H * Dh)


def _complex_affine_combine(e1, e2):
    a1r, a1i, b1r, b1i = e1
    a2r, a2i, b2r, b2i = e2
    return (a2r * a1r - a2i * a1i,
            a2r * a1i + a2i * a1r,
            a2r * b1r - a2i * b1i + b2r,
            a2r * b1i + a2i * b1r + b2i)


def s5_mixer(u, lam_re, lam_im, log_dt, b_re, b_im, c_re, c_im, d_skip, w_glu):
    Bsz, S, _ = u.shape
    ug = u.reshape(Bsz, S, SSM_GROUPS, SSM_GROUP_CH)
    dt = jnp.exp(log_dt)[:, None]
    mag = jnp.exp(lam_re * dt)
    ab_re = mag * jnp.cos(lam_im * dt)
    ab_im = mag * jnp.sin(lam_im * dt)
    den = lam_re * lam_re + lam_im * lam_im
    nr, ni = ab_re - 1.0, ab_im
    coef_re = ((nr * lam_re + ni * lam_im) / den)[..., None]
    coef_im = ((ni * lam_re - nr * lam_im) / den)[..., None]
    bb_re = coef_re * b_re - coef_im * b_im
    bb_im = coef_re * b_im + coef_im * b_re
    bu_re = jnp.einsum('bsgc,gpc->bsgp', ug, bb_re)
    bu_im = jnp.einsum('bsgc,gpc->bsgp', ug, bb_im)
    a_re = jnp.broadcast_to(ab_re.astype(bu_re.dtype), bu_re.shape)
    a_im = jnp.broadcast_to(ab_im.astype(bu_re.dtype), bu_re.shape)
    _, _, h_re, h_im = lax.associative_scan(_complex_affine_combine, (a_re, a_im, bu_re, bu_im), axis=1)
    y = jnp.einsum('gcp,bsgp->bsgc', c_re, h_re) - jnp.einsum('gcp,bsgp->bsgc', c_im, h_im)
    y = y.reshape(Bsz, S, SSM_WIDTH) + d_skip * u
    y = jax.nn.gelu(y, approximate=False)
    return y * jax.nn.sigmoid(y @ w_glu)


def peer_ffn(h, w_q, keys_a, keys_b, u_tab, v_tab):
    Bsz, S, D = h.shape
    T = Bsz * S
    ht = h.reshape(T, D)
    q = (ht @ w_q).reshape(T, PEER_HEADS, PEER_QDIM)
    half = PEER_QDIM // 2
    sa = jnp.einsum('thd,hkd->thk', q[..., :half], keys_a).astype(jnp.float32)
    sb = jnp.einsum('thd,hkd->thk', q[..., half:], keys_b).astype(jnp.float32)
    va, ia = lax.top_k(sa, PEER_TOPK)
    vb, ib = lax.top_k(sb, PEER_TOPK)
    cand = (va[..., :, None] + vb[..., None, :]).reshape(T, PEER_HEADS, PEER_TOPK * PEER_TOPK)
    vbest, cbest = lax.top_k(cand, PEER_TOPK)
    ea = jnp.take_along_axis(ia, cbest // PEER_TOPK, axis=-1)
    eb = jnp.take_along_axis(ib, cbest % PEER_TOPK, axis=-1)
    experts = ea * PEER_NKEYS + eb
    gates = jax.nn.softmax(vbest, axis=-1).astype(h.dtype)
    nchunk = T // PEER_TOKEN_CHUNK

    def one_chunk(args):
        xc, ec, gc = args
        act = jax.nn.gelu(jnp.einsum('td,thkd->thk', xc, u_tab[ec]), approximate=False)
        return jnp.einsum('thk,thkd->td', gc * act, v_tab[ec])

    out = lax.map(one_chunk, (ht.reshape(nchunk, PEER_TOKEN_CHUNK, D),
                              experts.reshape(nchunk, PEER_TOKEN_CHUNK, PEER_HEADS, PEER_TOPK),
                              gates.reshape(nchunk, PEER_TOKEN_CHUNK, PEER_HEADS, PEER_TOPK)))
    return out.reshape(Bsz, S, D)


def setup_inputs(seed: int = 0) -> dict:
    key = jax.random.key(seed)
    ks = jax.random.split(key, 24)

    def nrm(k, shape, scale):
        return jax.random.normal(k, shape, jnp.float32) * scale

    n_idx = jnp.arange(SSM_STATE, dtype=jnp.float32)
    return {
        'x': nrm(ks[0], (BATCH, SEQ, D_MODEL), 1.0),
        'norm_mix_gain': 1.0 + nrm(ks[1], (DEPTH, D_MODEL), 0.02),
        'w_in': nrm(ks[2], (DEPTH, D_MODEL, IN_PROJ_WIDTH), D_MODEL ** -0.5),
        'rel_bias': nrm(ks[3], (NUM_BUCKETS, ATTN_HEADS), 0.5),
        'ssm_lambda_re': -0.5 + nrm(ks[4], (DEPTH, SSM_GROUPS, SSM_STATE), 0.01),
        'ssm_lambda_im': math.pi * n_idx + nrm(ks[5], (DEPTH, SSM_GROUPS, SSM_STATE), 0.01),
        'ssm_log_dt': jax.random.uniform(ks[6], (DEPTH, SSM_GROUPS), jnp.float32, math.log(1e-3), math.log(1e-1)),
        'ssm_b_re': nrm(ks[7], (DEPTH, SSM_GROUPS, SSM_STATE, SSM_GROUP_CH), (2 * SSM_GROUP_CH) ** -0.5),
        'ssm_b_im': nrm(ks[8], (DEPTH, SSM_GROUPS, SSM_STATE, SSM_GROUP_CH), (2 * SSM_GROUP_CH) ** -0.5),
        'ssm_c_re': nrm(ks[9], (DEPTH, SSM_GROUPS, SSM_GROUP_CH, SSM_STATE), (2 * SSM_STATE) ** -0.5),
        'ssm_c_im': nrm(ks[10], (DEPTH, SSM_GROUPS, SSM_GROUP_CH, SSM_STATE), (2 * SSM_STATE) ** -0.5),
        'ssm_d': nrm(ks[11], (DEPTH, SSM_WIDTH), 1.0),
        'ssm_w_glu': nrm(ks[12], (DEPTH, SSM_WIDTH, SSM_WIDTH), SSM_WIDTH ** -0.5),
        'attn_out_gain': 1.0 + nrm(ks[13], (DEPTH, ATTN_WIDTH), 0.02),
        'ssm_out_gain': 1.0 + nrm(ks[14], (DEPTH, SSM_WIDTH), 0.02),
        'w_out': nrm(ks[15], (DEPTH, MIX_WIDTH, D_MODEL), MIX_WIDTH ** -0.5),
        'norm_ffn_gain': 1.0 + nrm(ks[16], (DEPTH, D_MODEL), 0.02),
        'peer_w_q': nrm(ks[17], (DEPTH, D_MODEL, PEER_HEADS * PEER_QDIM), D_MODEL ** -0.5),
        'peer_keys_a': nrm(ks[18], (DEPTH, PEER_HEADS, PEER_NKEYS, PEER_QDIM // 2), (PEER_QDIM // 2) ** -0.5),
        'peer_keys_b': nrm(ks[19], (DEPTH, PEER_HEADS, PEER_NKEYS, PEER_QDIM // 2), (PEER_QDIM // 2) ** -0.5),
        'peer_u': nrm(ks[20], (DEPTH, PEER_EXPERTS, D_MODEL), D_MODEL ** -0.5),
        'peer_v': nrm(ks[21], (DEPTH, PEER_EXPERTS, D_MODEL), PEER_V_SCALE),
        'norm_final_gain': 1.0 + nrm(ks[22], (D_MODEL,), 0.02),
    }


def reference(x, norm_mix_gain, w_in, rel_bias, ssm_lambda_re, ssm_lambda_im, ssm_log_dt,
              ssm_b_re, ssm_b_im, ssm_c_re, ssm_c_im, ssm_d, ssm_w_glu, attn_out_gain,
              ssm_out_gain, w_out, norm_ffn_gain, peer_w_q, peer_keys_a, peer_keys_b,
              peer_u, peer_v, norm_final_gain):
    Bsz, S, _ = x.shape
    for l in range(DEPTH):
        h = rmsnorm(x, norm_mix_gain[l])
        proj = h @ w_in[l]
        q, k, v, u = jnp.split(proj, [ATTN_WIDTH, 2 * ATTN_WIDTH, 3 * ATTN_WIDTH], axis=-1)
        q = q.reshape(Bsz, S, ATTN_HEADS, HEAD_DIM)
        k = k.reshape(Bsz, S, ATTN_HEADS, HEAD_DIM)
        v = v.reshape(Bsz, S, ATTN_HEADS, HEAD_DIM)
        y_attn = moba_attention(q, k, v, rel_bias)
        y_ssm = s5_mixer(u, ssm_lambda_re[l], ssm_lambda_im[l], ssm_log_dt[l], ssm_b_re[l], ssm_b_im[l],
                         ssm_c_re[l], ssm_c_im[l], ssm_d[l], ssm_w_glu[l])
        y = jnp.concatenate([rmsnorm(y_attn, attn_out_gain[l]), rmsnorm(y_ssm, ssm_out_gain[l])], axis=-1)
        x = x + y @ w_out[l]
        x = x + peer_ffn(rmsnorm(x, norm_ffn_gain[l]), peer_w_q[l], peer_keys_a[l], peer_keys_b[l],
                         peer_u[l], peer_v[l])
    return rmsnorm(x, norm_final_gain)
```

```python
import numpy as np
import concourse.bass as bass
import concourse.mybir as mybir
F32 = mybir.dt.float32
BF16 = mybir.dt.bfloat16
I32 = mybir.dt.int32
U32 = mybir.dt.uint32
AF = mybir.ActivationFunctionType
ALU = mybir.AluOpType
AX = mybir.AxisListType

class Buf:
    __slots__ = ("t", "w", "r", "name")
    def __init__(self, t, name=""):
        self.t = t; self.w = None; self.r = {}; self.name = name
    def __getitem__(self, k):
        return self.t[k]

class Sched:
    NDMA = 12
    def __init__(self, nc):
        self.nc = nc
        self.eng = {"pe": nc.tensor, "act": nc.scalar, "dve": nc.vector, "pool": nc.gpsimd, "sp": nc.sync}
        self.sem = {}
        self.cnt = {}
        self.waited = {k: {} for k in self.eng}
        self._ctx = []
        for k in self.eng:
            s = nc.semaphore("s_" + k); self.sem[k] = s.__enter__(); self._ctx.append(s); self.cnt[k] = 0
        self.dsem = {}; self.dcnt = {}
        for q in ("sp", "pool", "act"):
            l = []
            for i in range(self.NDMA):
                s = nc.semaphore("d_%s%d" % (q, i)); l.append(s.__enter__()); self._ctx.append(s)
            self.dsem[q] = l; self.dcnt[q] = 0
        self.semid = {}
    def close(self):
        for s in reversed(self._ctx):
            s.__exit__(None, None, None)
    def _wait(self, e, ev):
        if ev is None: return
        sem, val, key = ev
        w = self.waited[e]
        if w.get(key, 0) >= val: return
        w[key] = val
        self.eng[e].wait_ge(sem, val)
    def _deps(self, e, reads, writes, skip_self=False):
        for b in reads:
            if b.w is not None and not (skip_self and b.w[2] == e):
                self._wait(e, b.w)
        for b in writes:
            if b.w is not None and not (skip_self and b.w[2] == e):
                self._wait(e, b.w)
            for ev in b.r.values():
                if not (skip_self and ev[2] == e):
                    self._wait(e, ev)
    def _record(self, ev, reads, writes):
        for b in writes:
            b.w = ev; b.r = {}
        for b in reads:
            b.r[ev[2]] = ev
    def op(self, e, fn, reads=(), writes=()):
        self._deps(e, reads, writes, skip_self=(e == "pe"))
        ins = fn(self.eng[e])
        self.cnt[e] += 1
        ins.then_inc(self.sem[e], 1)
        ev = (self.sem[e], self.cnt[e], e)
        self._record(ev, reads, writes)
        return ev
    def dma(self, q, fn, reads=(), writes=()):
        i = self.dcnt[q]; self.dcnt[q] += 1
        slot = i % self.NDMA; rnd = i // self.NDMA
        sem = self.dsem[q][slot]
        key = "d_%s%d" % (q, slot)
        if rnd > 0:
            self._wait(q, (sem, 16 * rnd, key))
        self._deps(q, reads, writes)
        ins = fn(self.eng[q])
        ins.then_inc(sem, 16)
        ev = (sem, 16 * (rnd + 1), key)
        self._record(ev, reads, writes)
        return ev
    def wait_all(self, e, bufs):
        for b in bufs:
            self._wait(e, b.w)

import numpy as np

class Ctx:
    pass

def make_cfg(D=4096):
    c = Ctx()
    c.D = D; c.Da = D // 2; c.Ds = D // 2; c.H = c.Da // 128; c.G = c.Ds // 16
    c.NC = D // 128; c.TA = 2048; c.TO = 1024
    c.CB = min(512, c.Da)
    c.NE = 16384; c.PH = 8; c.QW = 2048
    return c

class Pool:
    def __init__(self, bufs): self.bufs = bufs; self.i = 0
    def next(self):
        b = self.bufs[self.i % len(self.bufs)]; self.i += 1; return b

class Alloc:
    def __init__(self, nc): self.nc = nc; self.stack = []; self.n = 0
    def sb(self, name, shape, dt):
        self.n += 1; c = self.nc.sbuf_tensor("sb%d_%s" % (self.n, name), shape, dt); t = c.__enter__(); self.stack.append(c); return Buf(t, name)
    def ps(self, name, shape, dt):
        self.n += 1; c = self.nc.psum_tensor("ps%d_%s" % (self.n, name), shape, dt); t = c.__enter__(); self.stack.append(c); return Buf(t, name)
    def mark(self): return len(self.stack)
    def release(self, mark):
        while len(self.stack) > mark:
            self.stack.pop().__exit__(None, None, None)

def barrier(S):
    engs = ["pe", "act", "dve", "pool", "sp"]
    for e in engs:
        for o in engs:
            if o != e and S.cnt[o] > 0:
                S._wait(e, (S.sem[o], S.cnt[o], o))
        for q in S.dsem:
            for i in range(min(S.dcnt[q], S.NDMA)):
                n = (S.dcnt[q] - 1 - i) // S.NDMA + 1
                S._wait(e, (S.dsem[q][i], 16 * n, "d_%s%d" % (q, i)))

def build_identity(S, A, dt=F32, name="ident"):
    ident = A.sb(name, [128, 128], dt)
    S.op("pool", lambda e: e.memset(ident[:], 0.0), writes=[ident])
    S.op("pool", lambda e: e.affine_select(out=ident[:], in_=ident[:], pattern=[[-1, 128]], compare_op=ALU.not_equal, fill=1.0, base=0, channel_multiplier=1), reads=[ident], writes=[ident])
    return ident

def rms_rstd(S, src, W, junk, ssq, rstd, eps=1e-6):
    S.op("act", lambda e: e.activation(out=junk[:, 0:W], in_=src[:, 0:W], func=AF.Square, accum_out=ssq[:, 0:1]), reads=[src], writes=[junk, ssq])
    S.op("dve", lambda e: e.tensor_scalar(out=ssq[:, 0:1], in0=ssq[:, 0:1], scalar1=1.0 / W, scalar2=eps, op0=ALU.mult, op1=ALU.add), reads=[ssq], writes=[ssq])
    S.op("act", lambda e: e.activation(out=ssq[:, 0:1], in_=ssq[:, 0:1], func=AF.Sqrt), reads=[ssq], writes=[ssq])
    S.op("dve", lambda e: e.reciprocal(out=rstd[:, 0:1], in_=ssq[:, 0:1]), reads=[ssq], writes=[rstd])

def transpose_to(S, src, W, ident, ptpool, dst, dst_tok0, gainT, c0=0):
    nch = W // 128
    for cg in range(0, nch, 4):
        n = min(4, nch - cg)
        pt = ptpool.next()
        for j in range(n):
            c = cg + j
            S.op("pe", lambda e, c=c, j=j: e.transpose(out=pt[:, j * 128:(j + 1) * 128], in_=src[:, c * 128:(c + 1) * 128], identity=ident[:]), reads=[src, ident], writes=[pt])
        S.op("dve", lambda e, cg=cg, n=n: e.tensor_tensor(
            out=dst[:, c0 + cg:c0 + cg + n, dst_tok0:dst_tok0 + 128],
            in0=pt[:, 0:n * 128].rearrange("p (a b) -> p a b", a=n),
            in1=gainT[:, c0 + cg:c0 + cg + n].unsqueeze(2).to_broadcast([128, n, 128]), op=ALU.mult),
            reads=[pt, gainT], writes=[dst])

def phase1(nc, S, A, c, T):
    mk = A.mark()
    D, NC, CB = c.D, c.NC, c.CB
    ident = build_identity(S, A)
    gmix = A.sb("gmix", [128, NC], F32)
    S.dma("sp", lambda e: e.dma_start(out=gmix[:], in_=T.g_mix[:, :]), writes=[gmix])
    hnT = A.sb("hnT", [128, NC, 1024], BF16)
    xts = Pool([A.sb("xt%d" % i, [128, D], F32) for i in range(2)])
    junk = A.sb("junk", [128, D], BF16)
    ssq = Pool([A.sb("ssq%d" % i, [128, 1], F32) for i in range(2)])
    rstd = Pool([A.sb("rstd%d" % i, [128, 1], F32) for i in range(2)])
    wts = Pool([A.sb("wt%d" % i, [128, NC, CB], BF16) for i in range(2)])
    ptp = Pool([A.ps("pt%d" % i, [128, 512], F32) for i in range(2)])
    pmp = Pool([A.ps("pm%d" % i, [128, 512], F32) for i in range(4)])
    stq = Pool([A.sb("stq%d" % i, [128, 1024], BF16) for i in range(2)])
    stv = Pool([A.sb("stv%d" % i, [128, CB], BF16) for i in range(2)])
    stu = Pool([A.sb("stu%d" % i, [128, CB], F32) for i in range(2)])
    nqb = c.Da // CB
    hpb = CB // 128
    w_view = T.w_in.rearrange("(c p) n -> p c n", p=128)
    for blk in range(2):
        for tt in range(8):
            xt = xts.next(); sq = ssq.next(); rs = rstd.next()
            r0 = blk * 1024 + tt * 128
            S.dma("sp", lambda e: e.dma_start(out=xt[:], in_=T.xall[r0:r0 + 128, :]), writes=[xt])
            rms_rstd(S, xt, D, junk, sq, rs)
            S.op("act", lambda e: e.activation(out=xt[:], in_=xt[:], func=AF.Copy, scale=rs[:, 0:1]), reads=[xt, rs], writes=[xt])
            transpose_to(S, xt, D, ident, ptp, hnT, tt * 128, gmix)
        for kind in range(4):
            if kind == 0 and blk == 0:
                continue
            for b in range(nqb):
                col0 = kind * c.Da + b * CB
                wt = wts.next()
                S.dma("pool", lambda e: e.dma_start(out=wt[:], in_=w_view[:, :, col0:col0 + CB]), writes=[wt])
                if kind < 2:
                    for j in range(hpb):
                        h = b * hpb + j
                        st = stq.next()
                        for half in range(2):
                            pm = pmp.next()
                            for cc in range(NC):
                                S.op("pe", lambda e, cc=cc: e.matmul(pm[:, :], lhsT=wt[:, cc, j * 128:(j + 1) * 128], rhs=hnT[:, cc, half * 512:(half + 1) * 512], start=(cc == 0), stop=(cc == NC - 1)), reads=[wt, hnT], writes=[pm])
                            eng = "act" if half == 0 else "dve"
                            if eng == "act":
                                S.op("act", lambda e: e.activation(out=st[:, half * 512:(half + 1) * 512], in_=pm[:, :], func=AF.Copy), reads=[pm], writes=[st])
                            else:
                                S.op("dve", lambda e: e.tensor_copy(out=st[:, half * 512:(half + 1) * 512], in_=pm[:, :]), reads=[pm], writes=[st])
                        if kind == 0:
                            S.dma("sp", lambda e: e.dma_start(out=T.QT[h, :, :], in_=st[:, :]), reads=[st])
                        else:
                            S.dma("sp", lambda e: e.dma_start(out=T.KT[h, :, blk * 1024:(blk + 1) * 1024], in_=st[:, :]), reads=[st])
                else:
                    for tt in range(8):
                        pm = pmp.next()
                        for cc in range(NC):
                            S.op("pe", lambda e, cc=cc: e.matmul(pm[:, 0:CB], lhsT=hnT[:, cc, tt * 128:(tt + 1) * 128], rhs=wt[:, cc, :], start=(cc == 0), stop=(cc == NC - 1)), reads=[wt, hnT], writes=[pm])
                        r0 = blk * 1024 + tt * 128
                        cl = b * CB
                        if kind == 2:
                            st = stv.next()
                            S.op("act", lambda e: e.activation(out=st[:, :], in_=pm[:, 0:CB], func=AF.Copy), reads=[pm], writes=[st])
                            S.dma("sp", lambda e: e.dma_start(out=T.V[r0:r0 + 128, cl:cl + CB], in_=st[:, :]), reads=[st])
                        else:
                            st = stu.next()
                            S.op("dve", lambda e: e.tensor_copy(out=st[:, :], in_=pm[:, 0:CB]), reads=[pm], writes=[st])
                            S.dma("sp", lambda e: e.dma_start(out=T.U[r0:r0 + 128, cl:cl + CB], in_=st[:, :]), reads=[st])
    barrier(S)
    A.release(mk)

def phase2(nc, S, A, c, T):
    mk = A.mark()
    H = c.H
    scale = 128 ** -0.5
    identb = build_identity(S, A, BF16, "identb")
    cb = A.sb("cbias", [128, H], F32)
    eladd = A.sb("eladd", [128, 8, 8], F32); eligp = A.sb("eligp", [128, 8, 8], F32); ownm = A.sb("ownm", [128, 8, 8], F32)
    S.dma("sp", lambda e: e.dma_start(out=cb[:], in_=T.cbias[:, :]), writes=[cb])
    S.dma("sp", lambda e: e.dma_start(out=eladd[:], in_=T.eladd[:, :, :]), writes=[eladd])
    S.dma("sp", lambda e: e.dma_start(out=eligp[:], in_=T.eligp[:, :, :]), writes=[eligp])
    S.dma("sp", lambda e: e.dma_start(out=ownm[:], in_=T.ownm[:, :, :]), writes=[ownm])
    qTs = Pool([A.sb("qT%d" % i, [128, 1024], BF16) for i in range(2)])
    kTs = Pool([A.sb("kT%d" % i, [128, 2048], BF16) for i in range(2)])
    vhs = Pool([A.sb("vh%d" % i, [128, 16, 136], BF16) for i in range(2)])
    for vb_ in vhs.bufs:
        S.op("pool", lambda e: e.memset(vb_[:, :, 128:136], 1.0), writes=[vb_])
    bts = Pool([A.sb("bt%d" % i, [128, 4, 256], F32) for i in range(2)])
    km = A.sb("km", [128, 8], F32); kmb = A.sb("kmb", [128, 8], F32); qf = A.sb("qf", [128, 1024], F32)
    pss = [A.ps("pss%d" % i, [128, 512], F32) for i in range(4)]
    bankA = A.ps("bankA", [128, 512], F32); bankB = A.ps("bankB", [128, 512], F32)
    pg = Buf(bankA[:, 0:8], "pg")
    ptb = Pool([A.ps("ptb%d" % i, [128, 512], BF16) for i in range(2)])
    pop = Pool([Buf(bankA[:, 128:264], "po0"), Buf(bankB[:, 128:264], "po1")])
    gm = A.sb("gm", [128, 8], F32); t8 = A.sb("t8", [128, 8], F32); m1 = A.sb("m1", [128, 8], F32)
    selb = A.sb("selb", [128, 8], F32); selc = A.sb("selc", [128, 8], F32)
    lgs = Pool([A.sb("lg%d" % i, [128, 256], F32) for i in range(2)])
    pexps = Pool([A.sb("pexp%d" % i, [128, 2048], BF16) for i in range(2)])
    rss = Pool([A.sb("rs%d" % i, [128, 8], F32) for i in range(2)])
    rsum = A.sb("rsum", [128, 1], F32); rinv = A.sb("rinv", [128, 1], F32)
    pTs = Pool([A.sb("pT%d" % i, [128, 4, 128], BF16) for i in range(4)])
    ysts = Pool([A.sb("yst%d" % i, [128, 128], F32) for i in range(2)])
    Vv = T.V.rearrange("(kt p) d -> p kt d", p=128)
    heads = {}
    def load_head(h):
        qT = qTs.next(); kT = kTs.next(); vh = vhs.next(); bt = bts.next()
        S.dma("sp", lambda e: e.dma_start(out=qT[:], in_=T.QT[h, :, :]), writes=[qT])
        S.dma("sp", lambda e: e.dma_start(out=kT[:], in_=T.KT[h, :, :]), writes=[kT])
        S.dma("sp", lambda e: e.dma_start(out=vh[:, :, 0:128], in_=Vv[:, :, h * 128:(h + 1) * 128]), writes=[vh])
        S.dma("sp", lambda e: e.dma_start(out=bt[:], in_=T.abias[h, :, :, :]), writes=[bt])
        heads[h] = (qT, kT, vh, bt)
    def stage_a(h, qt):
        if qt == 0:
            if h not in heads:
                load_head(h)
            qT, kT, vh, bt = heads[h]
            S.op("dve", lambda e: e.tensor_reduce(out=km[:, :], in_=kT[:, :].rearrange("p (n k) -> p n k", k=256), axis=AX.X, op=ALU.add), reads=[kT], writes=[km])
            S.op("dve", lambda e: e.tensor_scalar(out=kmb[:, :], in0=km[:, :], scalar1=1.0 / 256, scalar2=None, op0=ALU.mult), reads=[km], writes=[kmb])
            S.op("act", lambda e: e.activation(out=qf[:, :], in_=qT[:, :], func=AF.Copy), reads=[qT], writes=[qf])
        if qt == 1 and h + 1 < H:
            load_head(h + 1)
        qT, kT, vh, bt = heads[h]
        nown = 4 + qt // 2
        nblk = nown + 1
        qs = qT[:, qt * 128:(qt + 1) * 128]
        convert_tables(S, T, T.conv_per_step)
        S.op("pe", lambda e: e.matmul(pg[:, :], lhsT=qf[:, qt * 128:(qt + 1) * 128], rhs=kmb[:, :], start=True, stop=True), reads=[qf, kmb], writes=[pg])
        S.op("dve", lambda e: e.tensor_tensor(out=gm[:, :], in0=pg[:, :], in1=eladd[:, qt, :], op=ALU.add), reads=[pg, eladd], writes=[gm])
        S.op("dve", lambda e: e.max(out=t8[:, :], in_=gm[:, :]), reads=[gm], writes=[t8])
        S.op("dve", lambda e: e.tensor_scalar(out=m1[:, :], in0=gm[:, :], scalar1=t8[:, 2:3], scalar2=None, op0=ALU.is_ge), reads=[gm, t8], writes=[m1])
        S.op("dve", lambda e: e.tensor_tensor(out=m1[:, :], in0=m1[:, :], in1=eligp[:, qt, :], op=ALU.mult), reads=[m1, eligp], writes=[m1])
        S.op("dve", lambda e: e.tensor_tensor(out=m1[:, :], in0=m1[:, :], in1=ownm[:, qt, :], op=ALU.add), reads=[m1, ownm], writes=[m1])
        S.op("dve", lambda e: e.tensor_scalar(out=selb[:, :], in0=m1[:, :], scalar1=1.0, scalar2=1e30, op0=ALU.subtract, op1=ALU.mult), reads=[m1], writes=[selb])
        S.op("dve", lambda e: e.tensor_scalar(out=selc[:, :], in0=selb[:, :], scalar1=cb[:, h:h + 1], scalar2=None, op0=ALU.add), reads=[selb, cb], writes=[selc])
        def bank_of(n):
            if n >= nown - 1:
                return pss[3], (n - (nown - 1)) * 256
            return pss[n // 2], (n % 2) * 256
        for n in range(nblk):
            ps, po_ = bank_of(n)
            S.op("pe", lambda e: e.matmul(ps[:, po_:po_ + 256], lhsT=qs, rhs=kT[:, n * 256:(n + 1) * 256], start=True, stop=True), reads=[qT, kT], writes=[ps])
        pexp = pexps.next()
        for n in range(nblk):
            ps, po_ = bank_of(n)
            pslice = ps[:, po_:po_ + 256]
            if n >= nown - 1:
                v = (0 if n == nown else 2) + (qt % 2)
                lg = lgs.next()
                S.op("dve", lambda e: e.scalar_tensor_tensor(out=lg[:, :], in0=pslice, scalar=scale, in1=bt[:, v, :], op0=ALU.mult, op1=ALU.add), reads=[ps, bt], writes=[lg])
                S.op("act", lambda e: e.activation(out=pexp[:, n * 256:(n + 1) * 256], in_=lg[:, :], func=AF.Exp, bias=selb[:, n:n + 1], scale=1.0), reads=[lg, selb], writes=[pexp])
            else:
                S.op("act", lambda e: e.activation(out=pexp[:, n * 256:(n + 1) * 256], in_=pslice, func=AF.Exp, bias=selc[:, n:n + 1], scale=scale), reads=[ps, selc], writes=[pexp])
        return pexp, nblk
    def stage_b(h, qt, pexp, nblk):
        qT, kT, vh, bt = heads[h]
        po = pop.next()
        nkt = 2 * nblk
        groups = list(range(0, nkt, 4))
        tiles = {}
        def emit_t(gi):
            k0 = groups[gi]
            pt = ptb.next(); pT = pTs.next()
            nj = min(4, nkt - k0)
            for j in range(nj):
                kt = k0 + j
                S.op("pe", lambda e: e.transpose(out=pt[:, j * 128:(j + 1) * 128], in_=pexp[:, kt * 128:(kt + 1) * 128], identity=identb[:]), reads=[pexp, identb], writes=[pt])
            if gi % 2 == 0:
                S.op("act", lambda e: e.activation(out=pT[:, 0:nj, :], in_=pt[:, 0:nj * 128].rearrange("p (a b) -> p a b", a=nj), func=AF.Copy), reads=[pt], writes=[pT])
            else:
                S.op("dve", lambda e: e.tensor_copy(out=pT[:, 0:nj, :], in_=pt[:, 0:nj * 128].rearrange("p (a b) -> p a b", a=nj)), reads=[pt], writes=[pT])
            tiles[gi] = (pT, nj, k0)
        def emit_pv(gi):
            pT, nj, k0 = tiles.pop(gi)
            for j in range(nj):
                kt = k0 + j
                S.op("pe", lambda e: e.matmul(po[:, 0:129], lhsT=pT[:, j, :], rhs=vh[:, kt, 0:129], start=(kt == 0), stop=(kt == nkt - 1)), reads=[pT, vh], writes=[po])
        emit_t(0)
        for gi in range(len(groups)):
            if gi + 1 < len(groups):
                emit_t(gi + 1)
            emit_pv(gi)
        yst = ysts.next()
        S.op("dve", lambda e: e.reciprocal(out=rinv[:, :], in_=po[:, 128:129]), reads=[po], writes=[rinv])
        S.op("dve", lambda e: e.tensor_scalar(out=yst[:, :], in0=po[:, 0:128], scalar1=rinv[:, 0:1], scalar2=None, op0=ALU.mult), reads=[po, rinv], writes=[yst])
        S.dma("sp", lambda e: e.dma_start(out=T.YATT[qt * 128:(qt + 1) * 128, h * 128:(h + 1) * 128], in_=yst[:, :]), reads=[yst])
    units = [(h, qt) for h in range(H) for qt in range(8)]
    prev = None
    for (h, qt) in units:
        cur = stage_a(h, qt)
        if prev is not None:
            stage_b(*prev)
        prev = (h, qt) + cur
    stage_b(*prev)
    barrier(S)
    A.release(mk)

def t5_bucket_np(n):
    import math
    n = np.maximum(n, 0)
    r = np.log(np.maximum(n, 1).astype(np.float32) / np.float32(16)) / np.float32(math.log(128 / 16))
    large = 16 + (r.astype(np.float32) * np.float32(16)).astype(np.int32)
    large = np.minimum(large, 31)
    return np.where(n < 16, n, large)

def host_attn_tables(rel_bias, half):
    H = rel_bias.shape[1]
    i = np.arange(128)[:, None]; j = np.arange(256)[None, :]
    ab = np.empty((H, 128, 4, 256), np.float32)
    for v in range(4):
        qoff = (v % 2) * 128
        dist = qoff + i - j + (256 if v >= 2 else 0)
        bk = t5_bucket_np(dist)
        btl = rel_bias[bk, :]
        btl = np.where((dist >= 0)[:, :, None], btl, np.float32(-1e30))
        ab[:, :, v, :] = btl.transpose(2, 0, 1)
    cbias = np.ascontiguousarray(np.broadcast_to(rel_bias[31, :][None, :], (128, H))).astype(np.float32)
    eligp = np.zeros((8, 8), np.float32); ownm = np.zeros((8, 8), np.float32)
    for qt in range(8):
        own = 4 + qt // 2
        ownm[qt, own] = 1.0
        lo = 0 if half == 1 else 4
        eligp[qt, lo:own] = 1.0
    eladd = ((eligp - 1.0) * np.float32(1e30)).astype(np.float32)
    bc = lambda a: np.ascontiguousarray(np.broadcast_to(a[None], (128, 8, 8))).astype(np.float32)
    return ab, cbias, bc(eladd), bc(eligp), bc(ownm)

TWO_PI = 6.283185307179586
MAGIC = 12582912.0

def sincos(S, ang, shape, tmpa, tmpb, out_sin, out_cos, defer_act=False):
    a, ta, tb, osn, ocs = ang, tmpa, tmpb, out_sin, out_cos
    S.op("dve", lambda e: e.tensor_scalar(out=ta[1](ta[0]), in0=a[1](a[0]), scalar1=1.0 / TWO_PI, scalar2=MAGIC, op0=ALU.mult, op1=ALU.add), reads=[a[0]], writes=[ta[0]])
    S.op("dve", lambda e: e.tensor_scalar(out=ta[1](ta[0]), in0=ta[1](ta[0]), scalar1=MAGIC, scalar2=None, op0=ALU.subtract), reads=[ta[0]], writes=[ta[0]])
    S.op("dve", lambda e: e.scalar_tensor_tensor(out=tb[1](tb[0]), in0=ta[1](ta[0]), scalar=-TWO_PI, in1=a[1](a[0]), op0=ALU.mult, op1=ALU.add), reads=[ta[0], a[0]], writes=[tb[0]])
    S.op("dve", lambda e: e.tensor_scalar(out=tb[1](tb[0]), in0=tb[1](tb[0]), scalar1=-3.1415925, scalar2=3.1415925, op0=ALU.max, op1=ALU.min), reads=[tb[0]], writes=[tb[0]])
    late = []
    sin1 = lambda: S.op("act", lambda e: e.activation(out=osn[1](osn[0]), in_=tb[1](tb[0]), func=AF.Sin), reads=[tb[0]], writes=[osn[0]])
    if defer_act:
        late.append(sin1)
    else:
        sin1()
    S.op("dve", lambda e: e.tensor_scalar(out=ta[1](ta[0]), in0=tb[1](tb[0]), scalar1=1.5707963, scalar2=-TWO_PI, op0=ALU.is_gt, op1=ALU.mult), reads=[tb[0]], writes=[ta[0]])
    S.op("dve", lambda e: e.scalar_tensor_tensor(out=ta[1](ta[0]), in0=tb[1](tb[0]), scalar=1.5707963, in1=ta[1](ta[0]), op0=ALU.add, op1=ALU.add), reads=[tb[0], ta[0]], writes=[ta[0]])
    S.op("dve", lambda e: e.tensor_scalar(out=ta[1](ta[0]), in0=ta[1](ta[0]), scalar1=-3.1415925, scalar2=3.1415925, op0=ALU.max, op1=ALU.min), reads=[ta[0]], writes=[ta[0]])
    sin2 = lambda: S.op("act", lambda e: e.activation(out=ocs[1](ocs[0]), in_=ta[1](ta[0]), func=AF.Sin), reads=[ta[0]], writes=[ocs[0]])
    if defer_act:
        late.append(sin2)
    else:
        sin2()
    return late

def phase3(nc, S, A, c, T):
    mk = A.mark()
    G = c.G
    full = lambda b: b[:]
    ident = build_identity(S, A)
    def load(name, shape, src):
        b = A.sb(name, shape, F32)
        S.dma("sp", lambda e: e.dma_start(out=b[:], in_=src), writes=[b])
        return b
    sh3 = [64, G, 24]; sB = [64, G, 16]
    Are = A.sb("Are", sh3, F32); Aim = A.sb("Aim", sh3, F32)
    Bre = A.sb("Bre", sB, F32); Bim = A.sb("Bim", sB, F32)
    th8 = A.sb("th8", [64, G], F32); r8 = A.sb("r8", [64, G], F32)
    cre = load("cre", [64, G, 16], T.c_re[:, :, :]); cim = load("cim", [64, G, 16], T.c_im[:, :, :])
    dbcs = Pool([A.sb("dbc%d" % i, [128, 128], F32) for i in range(2)])
    nidx = load("nidx", [64, 256], T.nidx[:, :]); cmask = load("cmask", [128, 128], T.cmask[:, :])
    mk_tmp = A.mark()
    lre = load("lre", [64, G], T.lam_re[:, :]); lim = load("lim", [64, G], T.lam_im[:, :]); ldt = load("ldt", [64, G], T.logdt[:, :])
    bre = load("bre", [64, G, 16], T.b_re[:, :, :]); bim = load("bim", [64, G, 16], T.b_im[:, :, :])
    mv = load("mv", [64, 24], T.mv24[:, :])
    dt = A.sb("dt", [64, G], F32); lrd = A.sb("lrd", [64, G], F32); lid = A.sb("lid", [64, G], F32)
    S.op("act", lambda e: e.activation(out=dt[:], in_=ldt[:], func=AF.Exp), reads=[ldt], writes=[dt])
    S.op("dve", lambda e: e.tensor_tensor(out=lrd[:], in0=lre[:], in1=dt[:], op=ALU.mult), reads=[lre, dt], writes=[lrd])
    S.op("dve", lambda e: e.tensor_tensor(out=lid[:], in0=lim[:], in1=dt[:], op=ALU.mult), reads=[lim, dt], writes=[lid])
    arg = A.sb("arg", sh3, F32); mag = A.sb("mag", sh3, F32); ang = A.sb("ang", sh3, F32)
    ta = A.sb("ta", sh3, F32); tb = A.sb("tb", sh3, F32)
    mvb = mv[:, :].unsqueeze(1).to_broadcast(sh3)
    S.op("dve", lambda e: e.tensor_tensor(out=arg[:], in0=lrd[:, :].unsqueeze(2).to_broadcast(sh3), in1=mvb, op=ALU.mult), reads=[lrd, mv], writes=[arg])
    S.op("act", lambda e: e.activation(out=mag[:], in_=arg[:], func=AF.Exp), reads=[arg], writes=[mag])
    S.op("dve", lambda e: e.tensor_tensor(out=ang[:], in0=lid[:, :].unsqueeze(2).to_broadcast(sh3), in1=mvb, op=ALU.mult), reads=[lid, mv], writes=[ang])
    sincos(S, (ang, full), sh3, (ta, full), (tb, full), (Aim, full), (Are, full))
    S.op("dve", lambda e: e.tensor_copy(out=th8[:], in_=tb[:, :, 23]), reads=[tb], writes=[th8])
    S.op("dve", lambda e: e.tensor_copy(out=r8[:], in_=mag[:, :, 23]), reads=[mag], writes=[r8])
    S.op("dve", lambda e: e.tensor_tensor(out=Are[:], in0=Are[:], in1=mag[:], op=ALU.mult), reads=[Are, mag], writes=[Are])
    S.op("dve", lambda e: e.tensor_tensor(out=Aim[:], in0=Aim[:], in1=mag[:], op=ALU.mult), reads=[Aim, mag], writes=[Aim])
    nr = A.sb("nr", [64, G], F32); t1 = A.sb("t1", [64, G], F32); t2 = A.sb("t2", [64, G], F32); den = A.sb("den", [64, G], F32)
    cfr = A.sb("cfr", [64, G], F32); cfi = A.sb("cfi", [64, G], F32)
    S.op("dve", lambda e: e.tensor_scalar(out=nr[:], in0=Are[:, :, 16], scalar1=-1.0, scalar2=None, op0=ALU.add), reads=[Are], writes=[nr])
    S.op("dve", lambda e: e.tensor_tensor(out=t1[:], in0=lre[:], in1=lre[:], op=ALU.mult), reads=[lre], writes=[t1])
    S.op("dve", lambda e: e.tensor_tensor(out=t2[:], in0=lim[:], in1=lim[:], op=ALU.mult), reads=[lim], writes=[t2])
    S.op("dve", lambda e: e.tensor_tensor(out=den[:], in0=t1[:], in1=t2[:], op=ALU.add), reads=[t1, t2], writes=[den])
    S.op("dve", lambda e: e.reciprocal(out=den[:], in_=den[:]), reads=[den], writes=[den])
    S.op("dve", lambda e: e.tensor_tensor(out=t1[:], in0=nr[:], in1=lre[:], op=ALU.mult), reads=[nr, lre], writes=[t1])
    S.op("dve", lambda e: e.tensor_tensor(out=t2[:], in0=Aim[:, :, 16], in1=lim[:], op=ALU.mult), reads=[Aim, lim], writes=[t2])
    S.op("dve", lambda e: e.tensor_tensor(out=t1[:], in0=t1[:], in1=t2[:], op=ALU.add), reads=[t1, t2], writes=[t1])
    S.op("dve", lambda e: e.tensor_tensor(out=cfr[:], in0=t1[:], in1=den[:], op=ALU.mult), reads=[t1, den], writes=[cfr])
    S.op("dve", lambda e: e.tensor_tensor(out=t1[:], in0=Aim[:, :, 16], in1=lre[:], op=ALU.mult), reads=[Aim, lre], writes=[t1])
    S.op("dve", lambda e: e.tensor_tensor(out=t2[:], in0=nr[:], in1=lim[:], op=ALU.mult), reads=[nr, lim], writes=[t2])
    S.op("dve", lambda e: e.tensor_tensor(out=t1[:], in0=t1[:], in1=t2[:], op=ALU.subtract), reads=[t1, t2], writes=[t1])
    S.op("dve", lambda e: e.tensor_tensor(out=cfi[:], in0=t1[:], in1=den[:], op=ALU.mult), reads=[t1, den], writes=[cfi])
    tB1 = A.sb("tB1", sB, F32); tB2 = A.sb("tB2", sB, F32)
    cfrb = cfr[:, :].unsqueeze(2).to_broadcast(sB); cfib = cfi[:, :].unsqueeze(2).to_broadcast(sB)
    S.op("dve", lambda e: e.tensor_tensor(out=tB1[:], in0=bre[:], in1=cfrb, op=ALU.mult), reads=[bre, cfr], writes=[tB1])
    S.op("dve", lambda e: e.tensor_tensor(out=tB2[:], in0=bim[:], in1=cfib, op=ALU.mult), reads=[bim, cfi], writes=[tB2])
    S.op("dve", lambda e: e.tensor_tensor(out=Bre[:], in0=tB1[:], in1=tB2[:], op=ALU.subtract), reads=[tB1, tB2], writes=[Bre])
    S.op("dve", lambda e: e.tensor_tensor(out=tB1[:], in0=bim[:], in1=cfrb, op=ALU.mult), reads=[bim, cfr], writes=[tB1])
    S.op("dve", lambda e: e.tensor_tensor(out=tB2[:], in0=bre[:], in1=cfib, op=ALU.mult), reads=[bre, cfi], writes=[tB2])
    S.op("dve", lambda e: e.tensor_tensor(out=Bim[:], in0=tB1[:], in1=tB2[:], op=ALU.add), reads=[tB1, tB2], writes=[Bim])
    barrier(S)
    A.release(mk_tmp)
    s4 = [64, 8, 8, 16]
    s4f = [64, 8, 128]
    NTre = A.sb("NTre", s4f, F32); NTim = A.sb("NTim", s4f, F32); NPre = A.sb("NPre", s4f, F32); NPim = A.sb("NPim", s4f, F32)
    Rs = [(A.sb("Rre%d" % i, s4f, F32), A.sb("Rim%d" % i, s4f, F32)) for i in range(2)]
    q1 = A.sb("q1", s4, F32); q2 = A.sb("q2", s4, F32)
    v4 = lambda b: b[:].rearrange("p g (s c) -> p g s c", s=8)
    Utm2 = A.sb("Utm2", [128, 2, 8, 128], F32)
    Utms = Pool([A.sb("Utm%d" % i, [128, 2, 8, 128], F32) for i in range(1)])
    Ytm = A.sb("Ytm", [128, 8, 128], F32); Ytmp = A.sb("Ytmp", [128, 8, 128], F32)
    Ug = A.sb("Ug", [128, 8, 256], F32); Mg = A.sb("Mg", [128, 8, 128], F32)
    Ngs = Pool([A.sb("Ng%d" % i, [128, 128], F32) for i in range(2)])
    s3 = [64, 8, 256]
    Xre = A.sb("Xre", s3, F32); Xim = A.sb("Xim", s3, F32); Ec = A.sb("Ec", s3, F32); Es = A.sb("Es", s3, F32)
    w1 = A.sb("w1", s3, F32); w2 = A.sb("w2", s3, F32); Gre = A.sb("Gre", s3, F32); Gim = A.sb("Gim", s3, F32)
    Sre = Xre; Sim = Xim
    Ygs = Pool([A.sb("Yg%d" % i, [128, 128], F32) for i in range(2)])
    def bank(name, p, w):
        b = A.ps(name, [128, 512], F32)
        return Buf(b[0:p, 0:w], name)
    ptU = Pool([bank("ptU%d" % i, 128, 256) for i in range(2)])
    pN = bank("pN", 128, 128); px = bank("px", 64, 512); pmgs = [bank("pmgA", 128, 512), bank("pmgB", 128, 512)]
    py = bank("py", 128, 128); ptY = bank("ptY", 128, 128)
    Uv = T.U.rearrange("(b n s) ch -> n b s ch", b=2, n=128, s=8)
    Yv = T.YSSM.rearrange("(n t) ch -> n t ch", t=8)
    def cmul(outre, outim, are, aim, xre, xim, rd, neg_im=False):
        S.op("dve", lambda e: e.tensor_tensor(out=q1[:], in0=are, in1=xre, op=ALU.mult), reads=rd, writes=[q1])
        S.op("dve", lambda e: e.tensor_tensor(out=q2[:], in0=aim, in1=xim, op=ALU.mult), reads=rd, writes=[q2])
        S.op("dve", lambda e: e.tensor_tensor(out=v4(outre), in0=q1[:], in1=q2[:], op=ALU.subtract), reads=[q1, q2], writes=[outre])
        S.op("dve", lambda e: e.tensor_tensor(out=q1[:], in0=are, in1=xim, op=ALU.mult), reads=rd, writes=[q1])
        S.op("dve", lambda e: e.tensor_tensor(out=q2[:], in0=aim, in1=xre, op=ALU.mult), reads=rd, writes=[q2])
        if neg_im:
            S.op("dve", lambda e: e.scalar_tensor_tensor(out=v4(outim), in0=q1[:], scalar=-1.0, in1=q2[:], op0=ALU.mult, op1=ALU.subtract), reads=[q1, q2], writes=[outim])
        else:
            S.op("dve", lambda e: e.tensor_tensor(out=v4(outim), in0=q1[:], in1=q2[:], op=ALU.add), reads=[q1, q2], writes=[outim])
    stop = getattr(T, "stop", 0)
    NB = G // 8
    def apw(tab, g0, m0):
        return tab[:, g0:g0 + 8, m0:m0 + 8].unsqueeze(3).to_broadcast(s4)
    def apv(tab, g0):
        return tab[:, g0:g0 + 8, :].unsqueeze(2).to_broadcast(s4)
    def emit_cmul(gb):
        g0 = gb * 8
        Rre, Rim = Rs[gb % 2]
        cmul(NTre, NTim, apw(Are, g0, 0), apw(Aim, g0, 0), apv(Bre, g0), apv(Bim, g0), [Are, Aim, Bre, Bim])
        cmul(NPre, NPim, apw(Are, g0, 8), apw(Aim, g0, 8), apv(Bre, g0), apv(Bim, g0), [Are, Aim, Bre, Bim])
        cmul(Rre, Rim, apw(Are, g0, 16), apw(Aim, g0, 16), apv(cre, g0), apv(cim, g0), [Are, Aim, cre, cim], neg_im=True)
    emit_cmul(0)
    def load_u(gb_):
        ch_ = gb_ * 128
        Utm_ = Utms.next(); dbc_ = dbcs.next()
        for blk_ in range(2):
            S.dma("sp", lambda e: e.dma_start(out=Utm_[:, blk_, :, :], in_=Uv[:, blk_, :, ch_:ch_ + 128]), writes=[Utm_])
        S.dma("sp", lambda e: e.dma_start(out=dbc_[:], in_=T.d_bc[:, ch_:ch_ + 128]), writes=[dbc_])
        return Utm_, dbc_
    nxt_u = load_u(0)
    for gb in range(NB):
        g0 = gb * 8; ch0 = gb * 128
        Rre, Rim = Rs[gb % 2]
        Utm, dbc = nxt_u
        for blk in range(2):
            S.op("dve", lambda e: e.tensor_copy(out=Utm2[:, blk, :, :].rearrange("p j (s c) -> p j s c", s=8), in_=Utm[:, blk, :, :].rearrange("p s (j c) -> p j s c", j=8)), reads=[Utm], writes=[Utm2])
        if gb + 1 < NB:
            nxt_u = load_u(gb + 1)
        th = th8[:, g0:g0 + 8].unsqueeze(2).to_broadcast(s3)
        S.op("dve", lambda e: e.tensor_tensor(out=w1[:], in0=th, in1=nidx[:, :].unsqueeze(1).to_broadcast(s3), op=ALU.mult), reads=[th8, nidx], writes=[w1])
        late = sincos(S, (w1, full), s3, (w2, full), (Gre, full), (Es, full), (Ec, full), defer_act=True)
        for j in range(8):
            g = g0 + j
            pt = ptU.next()
            for blk in range(2):
                S.op("pe", lambda e: e.transpose(out=pt[:, blk * 128:(blk + 1) * 128], in_=Utm2[:, blk, j, :], identity=ident[:]), reads=[Utm2, ident], writes=[pt])
            S.op("act", lambda e: e.activation(out=Ug[:, j, :], in_=pt[:, :], func=AF.Copy), reads=[pt], writes=[Ug])
            S.op("pe", lambda e: e.transpose(out=pN[:, 0:64], in_=NTre[:, j, :], identity=ident[0:64, 0:64]), reads=[NTre, ident], writes=[pN])
            S.op("pe", lambda e: e.transpose(out=pN[:, 64:128], in_=NTim[:, j, :], identity=ident[0:64, 0:64]), reads=[NTim, ident], writes=[pN])
            Ng = Ngs.next()
            S.op("act", lambda e: e.activation(out=Ng[:, :], in_=pN[:, :], func=AF.Copy), reads=[pN], writes=[Ng])
            S.op("pe", lambda e: e.matmul(px[:, 0:256], lhsT=Ng[:, 0:64], rhs=Ug[:, j, :], start=True, stop=True), reads=[Ng, Ug], writes=[px])
            S.op("pe", lambda e: e.matmul(px[:, 256:512], lhsT=Ng[:, 64:128], rhs=Ug[:, j, :], start=True, stop=True), reads=[Ng, Ug], writes=[px])
            S.op("act", lambda e: e.activation(out=Xre[:, j, :], in_=px[:, 0:256], func=AF.Copy), reads=[px], writes=[Xre])
            S.op("act", lambda e: e.activation(out=Xim[:, j, :], in_=px[:, 256:512], func=AF.Copy), reads=[px], writes=[Xim])
            pm_ = pmgs[j // 4]; c0_ = (j % 4) * 128
            S.op("pe", lambda e: e.matmul(pm_[:, c0_:c0_ + 128], lhsT=NPre[:, j, :], rhs=Rre[:, j, :], start=True, stop=False), reads=[NPre, Rre], writes=[pm_])
            S.op("pe", lambda e: e.matmul(pm_[:, c0_:c0_ + 128], lhsT=NPim[:, j, :], rhs=Rim[:, j, :], start=False, stop=True), reads=[NPim, Rim], writes=[pm_])
        for th_ in late:
            th_()
        for hb_ in range(2):
            S.op("dve", lambda e: e.tensor_tensor(out=Mg[:, hb_ * 4:(hb_ + 1) * 4, :], in0=pmgs[hb_][:, :].rearrange("p (a b) -> p a b", a=4), in1=cmask[:, :].unsqueeze(1).to_broadcast([128, 4, 128]), op=ALU.mult), reads=[pmgs[hb_], cmask], writes=[Mg])
        S.op("dve", lambda e: e.tensor_tensor(out=w1[:], in0=Xre[:], in1=Ec[:], op=ALU.mult), reads=[Xre, Ec], writes=[w1])
        S.op("dve", lambda e: e.tensor_tensor(out=w2[:], in0=Xim[:], in1=Es[:], op=ALU.mult), reads=[Xim, Es], writes=[w2])
        S.op("dve", lambda e: e.tensor_tensor(out=Gre[:], in0=w1[:], in1=w2[:], op=ALU.add), reads=[w1, w2], writes=[Gre])
        S.op("dve", lambda e: e.tensor_tensor(out=w1[:], in0=Xim[:], in1=Ec[:], op=ALU.mult), reads=[Xim, Ec], writes=[w1])
        S.op("dve", lambda e: e.tensor_tensor(out=w2[:], in0=Xre[:], in1=Es[:], op=ALU.mult), reads=[Xre, Es], writes=[w2])
        S.op("dve", lambda e: e.tensor_tensor(out=Gim[:], in0=w1[:], in1=w2[:], op=ALU.subtract), reads=[w1, w2], writes=[Gim])
        for j in range(8):
            g = g0 + j
            coef = r8[:, g:g + 1].to_broadcast([64, 256])
            S.op("dve", lambda e: e.tensor_tensor_scan(out=Sre[:, j, :], data0=coef, data1=Gre[:, j, :], initial=0.0, op0=ALU.mult, op1=ALU.add), reads=[r8, Gre], writes=[Sre])
            S.op("dve", lambda e: e.tensor_tensor_scan(out=Sim[:, j, :], data0=coef, data1=Gim[:, j, :], initial=0.0, op0=ALU.mult, op1=ALU.add), reads=[r8, Gim], writes=[Sim])
        S.op("dve", lambda e: e.tensor_tensor(out=w1[:], in0=Sre[:], in1=Ec[:], op=ALU.mult), reads=[Sre, Ec], writes=[w1])
        S.op("dve", lambda e: e.tensor_tensor(out=w2[:], in0=Sim[:], in1=Es[:], op=ALU.mult), reads=[Sim, Es], writes=[w2])
        S.op("dve", lambda e: e.tensor_tensor(out=Gre[:], in0=w1[:], in1=w2[:], op=ALU.subtract), reads=[w1, w2], writes=[Gre])
        S.op("dve", lambda e: e.tensor_tensor(out=w1[:], in0=Sre[:], in1=Es[:], op=ALU.mult), reads=[Sre, Es], writes=[w1])
        S.op("dve", lambda e: e.tensor_tensor(out=w2[:], in0=Sim[:], in1=Ec[:], op=ALU.mult), reads=[Sim, Ec], writes=[w2])
        S.op("dve", lambda e: e.tensor_tensor(out=Gim[:], in0=w1[:], in1=w2[:], op=ALU.add), reads=[w1, w2], writes=[Gim])
        if gb + 1 < NB:
            emit_cmul(gb + 1)
        for j in range(8):
            S.op("pe", lambda e: e.matmul(py[:, :], lhsT=Mg[:, j, :], rhs=Ug[:, j, 128:256], start=True, stop=False), reads=[Mg, Ug], writes=[py])
            S.op("pe", lambda e: e.matmul(py[:, :], lhsT=Rre[:, j, :], rhs=Gre[:, j, 127:255], start=False, stop=False), reads=[Rre, Gre], writes=[py])
            S.op("pe", lambda e: e.matmul(py[:, :], lhsT=Rim[:, j, :], rhs=Gim[:, j, 127:255], start=False, stop=True), reads=[Rim, Gim], writes=[py])
            Yg = Ygs.next()
            S.op("act", lambda e: e.activation(out=Yg[:, :], in_=py[:, :], func=AF.Copy), reads=[py], writes=[Yg])
            S.op("pe", lambda e: e.transpose(out=ptY[:, :], in_=Yg[:, :], identity=ident[:]), reads=[Yg, ident], writes=[ptY])
            S.op("act", lambda e: e.activation(out=Ytm[:, :, j * 16:(j + 1) * 16], in_=ptY[:, :].rearrange("p (t c) -> p t c", t=8), func=AF.Copy), reads=[ptY], writes=[Ytm])
        S.op("dve", lambda e: e.tensor_tensor(out=Ytmp[:].rearrange("p s (j c) -> p s j c", j=8), in0=Utm2[:, 1, :, :].rearrange("p j (s c) -> p s j c", s=8), in1=dbc[:, :].rearrange("p (j c) -> p j c", j=8).unsqueeze(1).to_broadcast([128, 8, 8, 16]), op=ALU.mult), reads=[Utm2, dbc], writes=[Ytmp])
        S.op("dve", lambda e: e.tensor_tensor(out=Ytmp[:], in0=Ytmp[:], in1=Ytm[:], op=ALU.add), reads=[Ytmp, Ytm], writes=[Ytmp])
        S.dma("sp", lambda e: e.dma_start(out=Yv[:, :, ch0:ch0 + 128], in_=Ytmp[:]), reads=[Ytmp])
    barrier(S)
    A.release(mk)

def host_ssm_tables(lam_re, lam_im, log_dt, b_re, b_im, c_re, c_im, d):
    G = lam_re.shape[0]
    f = lambda a: np.ascontiguousarray(a).astype(np.float32)
    mv = np.concatenate([7 - np.arange(8), -1 - np.arange(8), 1 + np.arange(8)]).astype(np.float32)
    s = np.arange(128) // 16
    cmask = (s[None, :] >= s[:, None]).astype(np.float32)
    return dict(lam_re=f(lam_re.T), lam_im=f(lam_im.T), logdt=f(np.broadcast_to(log_dt[None, :], (64, G))),
                b_re=f(b_re.transpose(1, 0, 2)), b_im=f(b_im.transpose(1, 0, 2)),
                c_re=f(c_re.transpose(2, 0, 1)), c_im=f(c_im.transpose(2, 0, 1)),
                d_bc=f(np.broadcast_to(d[None, :], (128, d.shape[0]))),
                mv24=f(np.broadcast_to(mv[None], (64, 24))), nidx=f(np.broadcast_to(np.arange(256, dtype=np.float32)[None], (64, 256))),
                cmask=cmask)

def transpose_plain(S, src, W, ident, ptpool, dst, dst_tok0, c0=0, eng="dve"):
    nch = W // 128
    for cg in range(0, nch, 4):
        n = min(4, nch - cg)
        pt = ptpool.next()
        for j in range(n):
            c = cg + j
            S.op("pe", lambda e, c=c, j=j: e.transpose(out=pt[:, j * 128:(j + 1) * 128], in_=src[:, c * 128:(c + 1) * 128], identity=ident[:]), reads=[src, ident], writes=[pt])
        S.op("dve", lambda e, cg=cg, n=n: e.tensor_copy(
            out=dst[:, c0 + cg:c0 + cg + n, dst_tok0:dst_tok0 + 128],
            in_=pt[:, 0:n * 128].rearrange("p (a b) -> p a b", a=n)), reads=[pt], writes=[dst])

def phase4(nc, S, A, c, T):
    mk = A.mark()
    D, Da, Ds, NC = c.D, c.Da, c.Ds, c.NC
    NA = Da // 128; NS = Ds // 128
    ident = build_identity(S, A)
    gout = A.sb("gout", [128, NC], F32)
    S.dma("sp", lambda e: e.dma_start(out=gout[:], in_=T.g_out[:, :]), writes=[gout])
    mixT = A.sb("mixT", [128, NC, 1024], BF16)
    mk2 = A.mark()
    wglu = A.sb("wglu", [128, NS, Ds], BF16)
    wg_view = T.w_glu.rearrange("(c p) n -> p c n", p=128)
    for cc in range(0, NS, 4):
        n = min(4, NS - cc)
        S.dma("pool", lambda e: e.dma_start(out=wglu[:, cc:cc + n, :], in_=wg_view[:, cc:cc + n, :]), writes=[wglu])
    yss = Pool([A.sb("ys%d" % i, [128, Ds], F32) for i in range(2)])
    yas = Pool([A.sb("ya%d" % i, [128, Da], F32) for i in range(2)])
    junk = A.sb("junk4", [128, max(Da, Ds)], BF16)
    ygTs = Pool([A.sb("ygT%d" % i, [128, NS, 128], BF16) for i in range(2)])
    sgs = Pool([A.sb("sg%d" % i, [128, 512], F32) for i in range(2)])
    ssq = Pool([A.sb("ssq4%d" % i, [128, 1], F32) for i in range(2)])
    rstd = Pool([A.sb("rstd4%d" % i, [128, 1], F32) for i in range(2)])
    ptp = Pool([A.ps("pt4%d" % i, [128, 512], F32) for i in range(2)])
    pgp = Pool([A.ps("pg4%d" % i, [128, 512], F32) for i in range(3)])
    GB = min(512, Ds)
    for tt in range(8):
        ys = yss.next(); ya = yas.next(); ygT = ygTs.next()
        S.dma("sp", lambda e: e.dma_start(out=ys[:], in_=T.YSSM[tt * 128:(tt + 1) * 128, :]), writes=[ys])
        S.dma("sp", lambda e: e.dma_start(out=ya[:], in_=T.YATT[tt * 128:(tt + 1) * 128, :]), writes=[ya])
        S.op("act", lambda e: e.activation(out=ys[:], in_=ys[:], func=AF.Gelu), reads=[ys], writes=[ys])
        transpose_plain(S, ys, Ds, ident, ptp, ygT, 0)
        for jb in range(Ds // GB):
            pg = pgp.next(); sg = sgs.next()
            for cc in range(NS):
                S.op("pe", lambda e: e.matmul(pg[:, 0:GB], lhsT=ygT[:, cc, :], rhs=wglu[:, cc, jb * GB:(jb + 1) * GB], start=(cc == 0), stop=(cc == NS - 1)), reads=[ygT, wglu], writes=[pg])
            S.op("act", lambda e: e.activation(out=sg[:, 0:GB], in_=pg[:, 0:GB], func=AF.Sigmoid), reads=[pg], writes=[sg])
            S.op("dve", lambda e: e.tensor_tensor(out=ys[:, jb * GB:(jb + 1) * GB], in0=ys[:, jb * GB:(jb + 1) * GB], in1=sg[:, 0:GB], op=ALU.mult), reads=[ys, sg], writes=[ys])
        sq = ssq.next(); rs = rstd.next()
        rms_rstd(S, ys, Ds, junk, sq, rs)
        S.op("act", lambda e: e.activation(out=ys[:], in_=ys[:], func=AF.Copy, scale=rs[:, 0:1]), reads=[ys, rs], writes=[ys])
        transpose_to(S, ys, Ds, ident, ptp, mixT, tt * 128, gout, c0=NA)
        sq = ssq.next(); rs = rstd.next()
        rms_rstd(S, ya, Da, junk, sq, rs)
        S.op("act", lambda e: e.activation(out=ya[:], in_=ya[:], func=AF.Copy, scale=rs[:, 0:1]), reads=[ya, rs], writes=[ya])
        transpose_to(S, ya, Da, ident, ptp, mixT, tt * 128, gout, c0=0)
    barrier(S)
    A.release(mk2)
    OB = min(512, D)
    wos = Pool([A.sb("wo%d" % i, [128, NC, OB], BF16) for i in range(2)])
    xrs = Pool([A.sb("xr%d" % i, [128, OB], F32) for i in range(3)])
    pop = Pool([A.ps("po4%d" % i, [128, 512], F32) for i in range(4)])
    wo_view = T.w_out.rearrange("(c p) n -> p c n", p=128)
    for db in range(D // OB):
        wo = wos.next()
        S.dma("pool", lambda e: e.dma_start(out=wo[:], in_=wo_view[:, :, db * OB:(db + 1) * OB]), writes=[wo])
        for tt in range(8):
            po = pop.next(); xr = xrs.next()
            S.dma("sp", lambda e: e.dma_start(out=xr[:], in_=T.xall[1024 + tt * 128:1024 + (tt + 1) * 128, db * OB:(db + 1) * OB]), writes=[xr])
            for cc in range(NC):
                S.op("pe", lambda e: e.matmul(po[:, 0:OB], lhsT=mixT[:, cc, tt * 128:(tt + 1) * 128], rhs=wo[:, cc, :], start=(cc == 0), stop=(cc == NC - 1)), reads=[mixT, wo], writes=[po])
            S.op("dve", lambda e: e.tensor_tensor(out=xr[:], in0=xr[:], in1=po[:, 0:OB], op=ALU.add), reads=[xr, po], writes=[xr])
            S.dma("sp", lambda e: e.dma_start(out=T.X1[tt * 128:(tt + 1) * 128, db * OB:(db + 1) * OB], in_=xr[:]), reads=[xr])
    barrier(S)
    A.release(mk)

def phase5(nc, S, A, c, T):
    D, NC = c.D, c.NC
    convert_tables(S, T, 1 << 30)
    mk = A.mark()
    ident = build_identity(S, A)
    gT = A.sb("gffnT", [128, NC], F32); gbc = A.sb("gffnbc", [128, D], F32)
    S.dma("sp", lambda e: e.dma_start(out=gT[:], in_=T.g_ffnT[:, :]), writes=[gT])
    S.dma("sp", lambda e: e.dma_start(out=gbc[:], in_=T.g_ffn_bc[:, :]), writes=[gbc])
    hn2T = A.sb("hn2T", [128, NC, 1024], BF16)
    xts = Pool([A.sb("x5_%d" % i, [128, D], F32) for i in range(2)])
    junk = A.sb("junk5", [128, D], BF16)
    ssq = Pool([A.sb("ssq5%d" % i, [128, 1], F32) for i in range(2)])
    rstd = Pool([A.sb("rstd5%d" % i, [128, 1], F32) for i in range(2)])
    ptp = Pool([A.ps("pt5%d" % i, [128, 512], F32) for i in range(2)])
    pmp = Pool([A.ps("pm5%d" % i, [128, 512], F32) for i in range(4)])
    for tt in range(8):
        xt = xts.next(); sq = ssq.next(); rs = rstd.next()
        S.dma("sp", lambda e: e.dma_start(out=xt[:], in_=T.X1[tt * 128:(tt + 1) * 128, :]), writes=[xt])
        rms_rstd(S, xt, D, junk, sq, rs)
        S.op("act", lambda e: e.activation(out=xt[:], in_=xt[:], func=AF.Copy, scale=rs[:, 0:1]), reads=[xt, rs], writes=[xt])
        transpose_to(S, xt, D, ident, ptp, hn2T, tt * 128, gT)
        S.op("dve", lambda e: e.tensor_tensor(out=junk[:], in0=xt[:], in1=gbc[:], op=ALU.mult), reads=[xt, gbc], writes=[junk])
        S.dma("sp", lambda e: e.dma_start(out=T.HN2[tt * 128:(tt + 1) * 128, :], in_=junk[:]), reads=[junk])
    wqs = Pool([A.sb("wq%d" % i, [128, NC, 512], BF16) for i in range(2)])
    stq = Pool([A.sb("stq5%d" % i, [128, 1024], F32) for i in range(2)])
    wq_view = T.w_q.rearrange("(c p) n -> p c n", p=128)
    for cb in range(4):
        wq = wqs.next()
        S.dma("pool", lambda e: e.dma_start(out=wq[:], in_=wq_view[:, :, cb * 512:(cb + 1) * 512]), writes=[wq])
        for j in range(4):
            cq = cb * 4 + j
            st = stq.next()
            for half in range(2):
                pm = pmp.next()
                for cc in range(NC):
                    S.op("pe", lambda e: e.matmul(pm[:, :], lhsT=wq[:, cc, j * 128:(j + 1) * 128], rhs=hn2T[:, cc, half * 512:(half + 1) * 512], start=(cc == 0), stop=(cc == NC - 1)), reads=[wq, hn2T], writes=[pm])
                if half == 0:
                    S.op("act", lambda e: e.activation(out=st[:, 0:512], in_=pm[:, :], func=AF.Copy), reads=[pm], writes=[st])
                else:
                    S.op("dve", lambda e: e.tensor_copy(out=st[:, 512:1024], in_=pm[:, :]), reads=[pm], writes=[st])
            S.dma("sp", lambda e: e.dma_start(out=T.QPT[cq, :, :], in_=st[:, :]), reads=[st])
    barrier(S)
    A.release(mk)
    mk = A.mark()
    identb = build_identity(S, A, BF16, "identb5")
    NDB = D // 512
    pacc = [A.ps("pacc%d" % i, [128, 512], F32) for i in range(8)]
    mk2 = A.mark()
    keysT = A.sb("keysT", [128, 16, 128], F32)
    S.dma("sp", lambda e: e.dma_start(out=keysT[:], in_=T.keysT[:, :, :]), writes=[keysT])
    qts = Pool([A.sb("qt5%d" % i, [128, 16, 128], F32) for i in range(2)])
    scs = Pool([A.sb("scs%d" % i, [128, 16, 128], F32) for i in range(2)])
    QPv = T.QPT.rearrange("c p t -> p c t")
    for tt in range(8):
        r0 = tt * 128
        qt = qts.next(); sct = scs.next()
        S.dma("sp", lambda e: e.dma_start(out=qt[:], in_=QPv[:, :, r0:r0 + 128]), writes=[qt])
        for cq in range(16):
            ps = pacc[(tt % 2) * 4 + cq // 4]
            S.op("pe", lambda e: e.matmul(ps[:, (cq % 4) * 128:(cq % 4) * 128 + 128], lhsT=qt[:, cq, :], rhs=keysT[:, cq, :], start=True, stop=True), reads=[qt, keysT], writes=[ps])
        for b4 in range(4):
            ps = pacc[(tt % 2) * 4 + b4]
            S.op("act", lambda e: e.activation(out=sct[:, b4 * 4:(b4 + 1) * 4, :], in_=ps[:, :].rearrange("p (a b) -> p a b", a=4), func=AF.Copy), reads=[ps], writes=[sct])
        S.dma("sp", lambda e: e.dma_start(out=T.SCD[r0:r0 + 128, :, :], in_=sct[:]), reads=[sct])
    barrier(S)
    A.release(mk2)
    gpool = Pool([A.sb("gth%d" % i, [128, 2 * D], BF16) for i in range(7)])
    prods = Pool([A.sb("prod%d" % i, [128, D], BF16) for i in range(2)])
    junk2 = A.sb("junk5d", [128, D], BF16)
    PUVv = T.PUV.rearrange("e two d -> e (two d)")
    hgs = Pool([A.sb("hg%d" % i, [128, D], BF16) for i in range(1)])
    xb = A.sb("xb5", [128, D], F32); gfb = A.sb("gfb", [128, D], F32)
    accA = xb
    S.dma("sp", lambda e: e.dma_start(out=gfb[:], in_=T.g_fin_bc[:, :]), writes=[gfb])
    junk = prods.bufs[0]
    sc = A.sb("sc", [128, 16, 128], F32)
    scr = A.sb("scr", [128, 128], F32)
    va = A.sb("va", [128, 16], F32); vb = A.sb("vb", [128, 16], F32)
    iu = A.sb("iu", [128, 16], U32); iaf = A.sb("iaf", [128, 16], F32); ibf = A.sb("ibf", [128, 16], F32)
    cand = A.sb("cand", [128, 256], F32); ecand = A.sb("ecand", [128, 256], F32); cscr = A.sb("cscr", [128, 256], F32)
    vbst = A.sb("vbst", [128, 16], F32); nmx = A.sb("nmx", [128, 1], F32); ex = A.sb("ex", [128, 16], F32)
    zz = A.sb("zz", [128, 1], F32)
    ef = A.sb("ef", [128, 128], F32)
    pu = A.sb("pu", [128, 16], U32); pi_ = A.sb("pi_", [128, 16], U32); pj_ = A.sb("pj_", [128, 16], U32)
    pif = A.sb("pif", [128, 16], F32); pjf = A.sb("pjf", [128, 16], F32); eaf = A.sb("eaf", [128, 16], F32); ebf = A.sb("ebf", [128, 16], F32)
    sel3 = A.sb("sel3", [128, 16, 16], F32)
    iota16 = A.sb("iota16", [128, 16], F32)
    S.dma("sp", lambda e: e.dma_start(out=iota16[:], in_=T.iota16[:, :]), writes=[iota16])
    eis = Pool([A.sb("ei%d" % i, [128, 128], I32) for i in range(2)])
    ggs = Pool([A.sb("gg%d" % i, [128, 128], F32) for i in range(2)])
    araws = Pool([A.sb("araw%d" % i, [128, 1], F32) for i in range(8)])
    wgs = Pool([A.sb("wg%d" % i, [128, 1], F32) for i in range(8)])
    dgs = Pool([A.sb("dg%d" % i, [128, 128], BF16) for i in range(6)])
    sq = A.sb("ssq5c", [128, 1], F32); rs = A.sb("rstd5c", [128, 1], F32)

    def topk_ops(tt, ei, gg):
        ops = []
        r0 = tt * 128
        ops.append(lambda: S.dma("sp", lambda e: e.dma_start(out=sc[:], in_=T.SCD[r0:r0 + 128, :, :]), writes=[sc]))
        def top16(src_ap, vals, idxf):
            ops.append(lambda: S.op("dve", lambda e: e.max(out=vals[:, 0:8], in_=src_ap), reads=[sc], writes=[vals]))
            ops.append(lambda: S.op("dve", lambda e: e.max_index(out=iu[:, 0:8], in_max=vals[:, 0:8], in_values=src_ap), reads=[sc, vals], writes=[iu]))
            ops.append(lambda: S.op("dve", lambda e: e.match_replace(out=scr[:, :], in_to_replace=vals[:, 0:8], in_values=src_ap, imm_value=-1e30), reads=[sc, vals], writes=[scr]))
            ops.append(lambda: S.op("dve", lambda e: e.max(out=vals[:, 8:16], in_=scr[:, :]), reads=[scr], writes=[vals]))
            ops.append(lambda: S.op("dve", lambda e: e.max_index(out=iu[:, 8:16], in_max=vals[:, 8:16], in_values=scr[:, :]), reads=[scr, vals], writes=[iu]))
            ops.append(lambda: S.op("dve", lambda e: e.tensor_copy(out=idxf[:, :], in_=iu[:, :]), reads=[iu], writes=[idxf]))
        c3 = [128, 16, 16]; m3 = [128, 16, 256]
        for h in range(8):
            top16(sc[:, 2 * h, :], va, iaf)
            top16(sc[:, 2 * h + 1, :], vb, ibf)
            ops.append(lambda: S.op("dve", lambda e: e.tensor_tensor(out=cand[:, :].rearrange("p (a b) -> p a b", a=16), in0=va[:, :].unsqueeze(2).to_broadcast(c3), in1=vb[:, :].unsqueeze(1).to_broadcast(c3), op=ALU.add), reads=[va, vb], writes=[cand]))
            ops.append(lambda: S.op("dve", lambda e: e.max(out=vbst[:, 0:8], in_=cand[:, :]), reads=[cand], writes=[vbst]))
            ops.append(lambda: S.op("dve", lambda e: e.max_index(out=pu[:, 0:8], in_max=vbst[:, 0:8], in_values=cand[:, :]), reads=[cand, vbst], writes=[pu]))
            ops.append(lambda: S.op("dve", lambda e: e.match_replace(out=cscr[:, :], in_to_replace=vbst[:, 0:8], in_values=cand[:, :], imm_value=-1e30), reads=[cand, vbst], writes=[cscr]))
            ops.append(lambda: S.op("dve", lambda e: e.max(out=vbst[:, 8:16], in_=cscr[:, :]), reads=[cscr], writes=[vbst]))
            ops.append(lambda: S.op("dve", lambda e: e.max_index(out=pu[:, 8:16], in_max=vbst[:, 8:16], in_values=cscr[:, :]), reads=[cscr, vbst], writes=[pu]))
            ops.append(lambda: S.op("dve", lambda e: e.tensor_single_scalar(out=pi_[:, :], in_=pu[:, :], scalar=4, op=ALU.logical_shift_right), reads=[pu], writes=[pi_]))
            ops.append(lambda: S.op("dve", lambda e: e.tensor_single_scalar(out=pj_[:, :], in_=pu[:, :], scalar=15, op=ALU.bitwise_and), reads=[pu], writes=[pj_]))
            ops.append(lambda: S.op("dve", lambda e: e.tensor_copy(out=pif[:, :], in_=pi_[:, :]), reads=[pi_], writes=[pif]))
            ops.append(lambda: S.op("dve", lambda e: e.tensor_copy(out=pjf[:, :], in_=pj_[:, :]), reads=[pj_], writes=[pjf]))
            ops.append(lambda: S.op("dve", lambda e: e.tensor_tensor(out=sel3[:, :, :], in0=pif[:, :].unsqueeze(2).to_broadcast(c3), in1=iota16[:, :].unsqueeze(1).to_broadcast(c3), op=ALU.is_equal), reads=[pif, iota16], writes=[sel3]))
            ops.append(lambda: S.op("dve", lambda e: e.tensor_tensor(out=sel3[:, :, :], in0=sel3[:, :, :], in1=iaf[:, :].unsqueeze(1).to_broadcast(c3), op=ALU.mult), reads=[sel3, iaf], writes=[sel3]))
            ops.append(lambda: S.op("dve", lambda e: e.tensor_reduce(out=eaf[:, :], in_=sel3[:, :, :], axis=AX.X, op=ALU.add), reads=[sel3], writes=[eaf]))
            ops.append(lambda: S.op("dve", lambda e: e.tensor_tensor(out=sel3[:, :, :], in0=pjf[:, :].unsqueeze(2).to_broadcast(c3), in1=iota16[:, :].unsqueeze(1).to_broadcast(c3), op=ALU.is_equal), reads=[pjf, iota16], writes=[sel3]))
            ops.append(lambda: S.op("dve", lambda e: e.tensor_tensor(out=sel3[:, :, :], in0=sel3[:, :, :], in1=ibf[:, :].unsqueeze(1).to_broadcast(c3), op=ALU.mult), reads=[sel3, ibf], writes=[sel3]))
            ops.append(lambda: S.op("dve", lambda e: e.tensor_reduce(out=ebf[:, :], in_=sel3[:, :, :], axis=AX.X, op=ALU.add), reads=[sel3], writes=[ebf]))
            ops.append(lambda h=h: S.op("dve", lambda e: e.scalar_tensor_tensor(out=ef[:, h * 16:(h + 1) * 16], in0=eaf[:, :], scalar=128.0, in1=ebf[:, :], op0=ALU.mult, op1=ALU.add), reads=[eaf, ebf], writes=[ef]))
            ops.append(lambda: S.op("dve", lambda e: e.tensor_scalar(out=nmx[:, :], in0=vbst[:, 0:1], scalar1=-1.0, scalar2=None, op0=ALU.mult), reads=[vbst], writes=[nmx]))
            ops.append(lambda: S.op("act", lambda e: e.activation(out=ex[:, :], in_=vbst[:, :], func=AF.Exp, bias=nmx[:, 0:1], scale=1.0), reads=[vbst, nmx], writes=[ex]))
            ops.append(lambda: S.op("dve", lambda e: e.tensor_reduce(out=zz[:, :], in_=ex[:, :], axis=AX.X, op=ALU.add), reads=[ex], writes=[zz]))
            ops.append(lambda: S.op("dve", lambda e: e.reciprocal(out=zz[:, :], in_=zz[:, :]), reads=[zz], writes=[zz]))
            ops.append(lambda h=h: S.op("dve", lambda e: e.tensor_scalar(out=gg[:, h * 16:(h + 1) * 16], in0=ex[:, :], scalar1=zz[:, 0:1], scalar2=None, op0=ALU.mult), reads=[ex, zz], writes=[gg]))
        ops.append(lambda: S.op("dve", lambda e: e.tensor_scalar(out=ef[:, :], in0=ef[:, :], scalar1=0.0, scalar2=float(c.NE - 1), op0=ALU.max, op1=ALU.min), reads=[ef], writes=[ef]))
        ops.append(lambda: S.op("dve", lambda e: e.tensor_copy(out=ei[:, :], in_=ef[:, :]), reads=[ef], writes=[ei]))
        return ops

    ei = eis.next(); gg = ggs.next()
    for th in topk_ops(0, ei, gg):
        th()
    LAG = 2
    for tt in range(8):
        r0 = tt * 128
        hg = hgs.next()
        S.dma("sp", lambda e: e.dma_start(out=hg[:], in_=T.HN2[r0:r0 + 128, :]), writes=[hg])
        S.dma("sp", lambda e: e.dma_start(out=xb[:], in_=T.X1[r0:r0 + 128, :]), writes=[xb])
        if tt + 1 < 8:
            ei_n = eis.next(); gg_n = ggs.next()
            nxt = topk_ops(tt + 1, ei_n, gg_n)
        else:
            nxt = []
        per = (len(nxt) + 99) // 100 if nxt else 0
        stage = {}
        def emit_dot(hk):
            gb = gpool.next(); ar = araws.next(); wg = wgs.next()
            S.dma("pool", lambda e: e.indirect_dma_start(out=gb[:], out_offset=None, in_=PUVv, in_offset=bass.IndirectOffsetOnAxis(ap=ei[:, hk:hk + 1], axis=0)), reads=[ei], writes=[gb])
            if hk % 3 == 0:
                S.op("dve", lambda e: e.scalar_tensor_tensor(out=junk[:, :], in0=hg[:, :], scalar=1.0, in1=gb[:, 0:D], op0=ALU.mult, op1=ALU.mult, accum_out=ar[:, 0:1]), reads=[hg, gb], writes=[junk, ar])
            else:
                pr = prods.next()
                S.op("dve", lambda e: e.tensor_tensor(out=pr[:, :], in0=hg[:, :], in1=gb[:, 0:D], op=ALU.mult), reads=[hg, gb], writes=[pr])
                S.op("act", lambda e: e.activation(out=junk2[:, :], in_=pr[:, :], func=AF.Copy, accum_out=ar[:, 0:1]), reads=[pr], writes=[junk2, ar])
            S.op("act", lambda e: e.activation(out=wg[:, 0:1], in_=ar[:, 0:1], func=AF.Gelu), reads=[ar], writes=[wg])
            stage[hk] = (gb, wg)
        def emit_acc(hk):
            gb, wg = stage.pop(hk)
            dgk = dgs.next()
            S.op("dve", lambda e: e.tensor_scalar(out=dgk[:, :], in0=identb[:, :], scalar1=wg[:, 0:1], scalar2=gg[:, hk:hk + 1], op0=ALU.mult, op1=ALU.mult), reads=[identb, wg, gg], writes=[dgk])
            for db in range(NDB):
                S.op("pe", lambda e: e.matmul(pacc[db][:, :], lhsT=dgk[:, :], rhs=gb[:, D + db * 512:D + (db + 1) * 512], start=(hk == 0), stop=(hk == 127)), reads=[dgk, gb], writes=[pacc[db]])
        for step in range(128 + LAG):
            if step < 128:
                emit_dot(step)
            if step - LAG >= 0:
                emit_acc(step - LAG)
            for _ in range(per):
                if nxt:
                    nxt.pop(0)()
        while nxt:
            nxt.pop(0)()
        for db in range(NDB):
            S.op("dve", lambda e: e.tensor_tensor(out=accA[:, db * 512:(db + 1) * 512], in0=xb[:, db * 512:(db + 1) * 512], in1=pacc[db][:, :], op=ALU.add), reads=[xb, pacc[db]], writes=[accA])
        rms_rstd(S, accA, D, junk, sq, rs)
        S.op("act", lambda e: e.activation(out=accA[:, :], in_=accA[:, :], func=AF.Copy, scale=rs[:, 0:1]), reads=[accA, rs], writes=[accA])
        S.op("dve", lambda e: e.tensor_tensor(out=accA[:, :], in0=accA[:, :], in1=gfb[:, :], op=ALU.mult), reads=[accA, gfb], writes=[accA])
        S.dma("sp", lambda e: e.dma_start(out=T.OUT[r0:r0 + 128, :], in_=accA[:, :]), reads=[accA])
        if tt + 1 < 8:
            ei = ei_n; gg = gg_n
    barrier(S)
    A.release(mk)

def convert_tables(S, T, n):
    st = T.conv_state
    while n > 0 and st[0] < len(st[1]):
        src, which, r = st[1][st[0]]
        S.dma("pool", lambda e: e.dma_start(out=T.PUV[r:r + 128, which, :], in_=src[r:r + 128, :]))
        st[0] += 1; n -= 1


def build_program(D=4096):
    c = make_cfg(D)
    nc = bass.Bass("TRN2", target_bir_lowering=False)
    T = Ctx()
    G = c.G
    ext_in = dict(xall=[2048, D], g_mix=[128, c.NC], w_in=[D, 2 * D],
                  abias=[c.H, 128, 4, 256], cbias=[128, c.H], eladd=[128, 8, 8], eligp=[128, 8, 8], ownm=[128, 8, 8],
                  lam_re=[64, G], lam_im=[64, G], logdt=[64, G], b_re=[64, G, 16], b_im=[64, G, 16], c_re=[64, G, 16], c_im=[64, G, 16],
                  d_bc=[128, c.Ds], mv24=[64, 24], nidx=[64, 256], cmask=[128, 128],
                  w_glu=[c.Ds, c.Ds], g_out=[128, c.NC], w_out=[D, D],
                  g_ffnT=[128, c.NC], g_ffn_bc=[128, D], w_q=[D, 2048], keysT=[128, 16, 128], peer_u=[16384, D], peer_v=[16384, D], g_fin_bc=[128, D], iota16=[128, 16])
    for k, s in ext_in.items():
        setattr(T, k, nc.dram_tensor(k, s, F32, kind="ExternalInput").ap())
    T.QT = nc.dram_tensor("QT", [c.H, 128, 1024], BF16, kind="Internal").ap()
    T.KT = nc.dram_tensor("KT", [c.H, 128, 2048], BF16, kind="Internal").ap()
    T.V = nc.dram_tensor("V", [2048, c.Da], BF16, kind="Internal").ap()
    T.U = nc.dram_tensor("U", [2048, c.Ds], F32, kind="Internal").ap()
    T.YATT = nc.dram_tensor("YATT", [1024, c.Da], F32, kind="Internal").ap()
    T.YSSM = nc.dram_tensor("YSSM", [1024, c.Ds], F32, kind="Internal").ap()
    T.X1 = nc.dram_tensor("X1", [1024, D], F32, kind="Internal").ap()
    T.HN2 = nc.dram_tensor("HN2", [1024, D], BF16, kind="Internal").ap()
    T.PUV = nc.dram_tensor("PUV", [16384, 2, D], BF16, kind="Internal").ap()
    T.SCD = nc.dram_tensor("SCD", [1024, 16, 128], F32, kind="Internal").ap()
    T.conv_state = [0, [(T.peer_u, 0, r) for r in range(0, 16384, 128)] + [(T.peer_v, 1, r) for r in range(0, 16384, 128)]]
    T.conv_per_step = 2
    T.QPT = nc.dram_tensor("QPT", [16, 128, 1024], F32, kind="Internal").ap()
    T.OUT = nc.dram_tensor("OUT", [1024, D], F32, kind="ExternalOutput").ap()
    S = Sched(nc); A = Alloc(nc)
    for ph in (phase1, phase2, phase3, phase4, phase5):
        ph(nc, S, A, c, T)
    return nc, c


from concourse.bass_utils import run_bass_kernel_spmd

_PROG = {}

def _bc(v, n=128):
    return np.ascontiguousarray(np.broadcast_to(np.asarray(v, np.float32)[None, :], (n, v.shape[0])))

def _fm(v):
    v = np.asarray(v, np.float32)
    return np.ascontiguousarray(v.reshape(-1, 128).T)

def kernel(x, norm_mix_gain, w_in, rel_bias, ssm_lambda_re, ssm_lambda_im, ssm_log_dt,
           ssm_b_re, ssm_b_im, ssm_c_re, ssm_c_im, ssm_d, ssm_w_glu, attn_out_gain,
           ssm_out_gain, w_out, norm_ffn_gain, peer_w_q, peer_keys_a, peer_keys_b,
           peer_u, peer_v, norm_final_gain):
    f32 = lambda a: np.ascontiguousarray(np.asarray(a, dtype=np.float32))
    x = f32(x)
    B, SEQ, D = x.shape
    assert B == 4 and SEQ == 2048
    if D not in _PROG:
        _PROG[D] = build_program(D)
    nc, c = _PROG[D]
    l = 0
    shared = dict(
        g_mix=_fm(norm_mix_gain[l]), w_in=f32(w_in[l]),
        w_glu=f32(ssm_w_glu[l]),
        g_out=_fm(np.concatenate([np.asarray(attn_out_gain[l], np.float32), np.asarray(ssm_out_gain[l], np.float32)])),
        w_out=f32(w_out[l]),
        g_ffnT=_fm(norm_ffn_gain[l]), g_ffn_bc=_bc(np.asarray(norm_ffn_gain[l], np.float32)),
        w_q=f32(peer_w_q[l]), peer_u=f32(peer_u[l]), peer_v=f32(peer_v[l]),
        g_fin_bc=_bc(np.asarray(norm_final_gain, np.float32)),
        iota16=_bc(np.arange(16, dtype=np.float32)),
    )
    ka = np.asarray(peer_keys_a[l], np.float32); kb = np.asarray(peer_keys_b[l], np.float32)
    keysT = np.empty((128, 16, 128), np.float32)
    for h in range(8):
        keysT[:, 2 * h, :] = ka[h].T
        keysT[:, 2 * h + 1, :] = kb[h].T
    shared["keysT"] = keysT
    shared.update(host_ssm_tables(np.asarray(ssm_lambda_re[l], np.float32), np.asarray(ssm_lambda_im[l], np.float32),
                                  np.asarray(ssm_log_dt[l], np.float32), np.asarray(ssm_b_re[l], np.float32),
                                  np.asarray(ssm_b_im[l], np.float32), np.asarray(ssm_c_re[l], np.float32),
                                  np.asarray(ssm_c_im[l], np.float32), np.asarray(ssm_d[l], np.float32)))
    rb = np.asarray(rel_bias, np.float32)
    attn_tabs = [host_attn_tables(rb, half) for half in range(2)]
    in_maps = []
    for core in range(8):
        b, half = core // 2, core % 2
        xall = np.zeros((2048, D), np.float32)
        if half == 1:
            xall[:1024] = x[b, :1024]
        xall[1024:] = x[b, half * 1024:(half + 1) * 1024]
        ab, cbias, eladd, eligp, ownm = attn_tabs[half]
        m = dict(shared)
        m.update(xall=xall, abias=ab, cbias=cbias, eladd=eladd, eligp=eligp, ownm=ownm)
        in_maps.append(m)
    res = run_bass_kernel_spmd(nc, in_maps, core_ids=list(range(8)))
    out = np.empty((B, SEQ, D), np.float32)
    for core in range(8):
        b, half = core // 2, core % 2
        out[b, half * 1024:(half + 1) * 1024] = np.asarray(res.results[core]["OUT"], np.float32)
    return out
```

```python
import numpy as np
import concourse.bass as bass
import concourse.mybir as mybir
F32 = mybir.dt.float32
BF16 = mybir.dt.bfloat16
I32 = mybir.dt.int32
U32 = mybir.dt.uint32
AF = mybir.ActivationFunctionType
ALU = mybir.AluOpType
AX = mybir.AxisListType

class Buf:
    __slots__ = ("t", "w", "r", "name")
    def __init__(self, t, name=""):
        self.t = t; self.w = None; self.r = {}; self.name = name
    def __getitem__(self, k):
        return self.t[k]

class Sched:
    NDMA = 12
    def __init__(self, nc):
        self.nc = nc
        self.eng = {"pe": nc.tensor, "act": nc.scalar, "dve": nc.vector, "pool": nc.gpsimd, "sp": nc.sync}
        self.sem = {}
        self.cnt = {}
        self.waited = {k: {} for k in self.eng}
        self._ctx = []
        for k in self.eng:
            s = nc.semaphore("s_" + k); self.sem[k] = s.__enter__(); self._ctx.append(s); self.cnt[k] = 0
        self.dsem = {}; self.dcnt = {}
        for q in ("sp", "pool", "act"):
            l = []
            for i in range(self.NDMA):
                s = nc.semaphore("d_%s%d" % (q, i)); l.append(s.__enter__()); self._ctx.append(s)
            self.dsem[q] = l; self.dcnt[q] = 0
        self.semid = {}
    def close(self):
        for s in reversed(self._ctx):
            s.__exit__(None, None, None)
    def _wait(self, e, ev):
        if ev is None: return
        sem, val, key = ev
        w = self.waited[e]
        if w.get(key, 0) >= val: return
        w[key] = val
        self.eng[e].wait_ge(sem, val)
    def _deps(self, e, reads, writes, skip_self=False):
        for b in reads:
            if b.w is not None and not (skip_self and b.w[2] == e):
                self._wait(e, b.w)
        for b in writes:
            if b.w is not None and not (skip_self and b.w[2] == e):
                self._wait(e, b.w)
            for ev in b.r.values():
                if not (skip_self and ev[2] == e):
                    self._wait(e, ev)
    def _record(self, ev, reads, writes):
        for b in writes:
            b.w = ev; b.r = {}
        for b in reads:
            b.r[ev[2]] = ev
    def op(self, e, fn, reads=(), writes=()):
        self._deps(e, reads, writes, skip_self=(e == "pe"))
        ins = fn(self.eng[e])
        self.cnt[e] += 1
        ins.then_inc(self.sem[e], 1)
        ev = (self.sem[e], self.cnt[e], e)
        self._record(ev, reads, writes)
        return ev
    def dma(self, q, fn, reads=(), writes=()):
        i = self.dcnt[q]; self.dcnt[q] += 1
        slot = i % self.NDMA; rnd = i // self.NDMA
        sem = self.dsem[q][slot]
        key = "d_%s%d" % (q, slot)
        if rnd > 0:
            self._wait(q, (sem, 16 * rnd, key))
        self._deps(q, reads, writes)
        ins = fn(self.eng[q])
        ins.then_inc(sem, 16)
        ev = (sem, 16 * (rnd + 1), key)
        self._record(ev, reads, writes)
        return ev
    def wait_all(self, e, bufs):
        for b in bufs:
            self._wait(e, b.w)

import numpy as np

class Ctx:
    pass

def make_cfg(D=4096):
    c = Ctx()
    c.D = D; c.Da = D // 2; c.Ds = D // 2; c.H = c.Da // 128; c.G = c.Ds // 16
    c.NC = D // 128; c.TA = 2048; c.TO = 1024
    c.CB = min(512, c.Da)
    c.NE = 16384; c.PH = 8; c.QW = 2048
    return c

class Pool:
    def __init__(self, bufs): self.bufs = bufs; self.i = 0
    def next(self):
        b = self.bufs[self.i % len(self.bufs)]; self.i += 1; return b

class Alloc:
    def __init__(self, nc): self.nc = nc; self.stack = []; self.n = 0
    def sb(self, name, shape, dt):
        self.n += 1; c = self.nc.sbuf_tensor("sb%d_%s" % (self.n, name), shape, dt); t = c.__enter__(); self.stack.append(c); return Buf(t, name)
    def ps(self, name, shape, dt):
        self.n += 1; c = self.nc.psum_tensor("ps%d_%s" % (self.n, name), shape, dt); t = c.__enter__(); self.stack.append(c); return Buf(t, name)
    def mark(self): return len(self.stack)
    def release(self, mark):
        while len(self.stack) > mark:
            self.stack.pop().__exit__(None, None, None)

def barrier(S):
    engs = ["pe", "act", "dve", "pool", "sp"]
    for e in engs:
        for o in engs:
            if o != e and S.cnt[o] > 0:
                S._wait(e, (S.sem[o], S.cnt[o], o))
        for q in S.dsem:
            for i in range(min(S.dcnt[q], S.NDMA)):
                n = (S.dcnt[q] - 1 - i) // S.NDMA + 1
                S._wait(e, (S.dsem[q][i], 16 * n, "d_%s%d" % (q, i)))

def build_identity(S, A, dt=F32, name="ident"):
    ident = A.sb(name, [128, 128], dt)
    S.op("pool", lambda e: e.memset(ident[:], 0.0), writes=[ident])
    S.op("pool", lambda e: e.affine_select(out=ident[:], in_=ident[:], pattern=[[-1, 128]], compare_op=ALU.not_equal, fill=1.0, base=0, channel_multiplier=1), reads=[ident], writes=[ident])
    return ident

def rms_rstd(S, src, W, junk, ssq, rstd, eps=1e-6):
    S.op("act", lambda e: e.activation(out=junk[:, 0:W], in_=src[:, 0:W], func=AF.Square, accum_out=ssq[:, 0:1]), reads=[src], writes=[junk, ssq])
    S.op("dve", lambda e: e.tensor_scalar(out=ssq[:, 0:1], in0=ssq[:, 0:1], scalar1=1.0 / W, scalar2=eps, op0=ALU.mult, op1=ALU.add), reads=[ssq], writes=[ssq])
    S.op("act", lambda e: e.activation(out=ssq[:, 0:1], in_=ssq[:, 0:1], func=AF.Sqrt), reads=[ssq], writes=[ssq])
    S.op("dve", lambda e: e.reciprocal(out=rstd[:, 0:1], in_=ssq[:, 0:1]), reads=[ssq], writes=[rstd])

def transpose_to(S, src, W, ident, ptpool, dst, dst_tok0, gainT, c0=0):
    nch = W // 128
    for cg in range(0, nch, 4):
        n = min(4, nch - cg)
        pt = ptpool.next()
        for j in range(n):
            c = cg + j
            S.op("pe", lambda e, c=c, j=j: e.transpose(out=pt[:, j * 128:(j + 1) * 128], in_=src[:, c * 128:(c + 1) * 128], identity=ident[:]), reads=[src, ident], writes=[pt])
        S.op("dve", lambda e, cg=cg, n=n: e.tensor_tensor(
            out=dst[:, c0 + cg:c0 + cg + n, dst_tok0:dst_tok0 + 128],
            in0=pt[:, 0:n * 128].rearrange("p (a b) -> p a b", a=n),
            in1=gainT[:, c0 + cg:c0 + cg + n].unsqueeze(2).to_broadcast([128, n, 128]), op=ALU.mult),
            reads=[pt, gainT], writes=[dst])

def phase1(nc, S, A, c, T):
    mk = A.mark()
    D, NC, CB = c.D, c.NC, c.CB
    ident = build_identity(S, A)
    gmix = A.sb("gmix", [128, NC], F32)
    S.dma("sp", lambda e: e.dma_start(out=gmix[:], in_=T.g_mix[:, :]), writes=[gmix])
    hnT = A.sb("hnT", [128, NC, 1024], BF16)
    xts = Pool([A.sb("xt%d" % i, [128, D], F32) for i in range(2)])
    junk = A.sb("junk", [128, D], BF16)
    ssq = Pool([A.sb("ssq%d" % i, [128, 1], F32) for i in range(2)])
    rstd = Pool([A.sb("rstd%d" % i, [128, 1], F32) for i in range(2)])
    wts = Pool([A.sb("wt%d" % i, [128, NC, CB], BF16) for i in range(2)])
    ptp = Pool([A.ps("pt%d" % i, [128, 512], F32) for i in range(2)])
    pmp = Pool([A.ps("pm%d" % i, [128, 512], F32) for i in range(4)])
    stq = Pool([A.sb("stq%d" % i, [128, 1024], BF16) for i in range(2)])
    stv = Pool([A.sb("stv%d" % i, [128, CB], BF16) for i in range(2)])
    stu = Pool([A.sb("stu%d" % i, [128, CB], F32) for i in range(2)])
    nqb = c.Da // CB
    hpb = CB // 128
    w_view = T.w_in.rearrange("(c p) n -> p c n", p=128)
    for blk in range(2):
        for tt in range(8):
            xt = xts.next(); sq = ssq.next(); rs = rstd.next()
            r0 = blk * 1024 + tt * 128
            S.dma("sp", lambda e: e.dma_start(out=xt[:], in_=T.xall[r0:r0 + 128, :]), writes=[xt])
            rms_rstd(S, xt, D, junk, sq, rs)
            S.op("act", lambda e: e.activation(out=xt[:], in_=xt[:], func=AF.Copy, scale=rs[:, 0:1]), reads=[xt, rs], writes=[xt])
            transpose_to(S, xt, D, ident, ptp, hnT, tt * 128, gmix)
        for kind in range(4):
            if kind == 0 and blk == 0:
                continue
            for b in range(nqb):
                col0 = kind * c.Da + b * CB
                wt = wts.next()
                S.dma("pool", lambda e: e.dma_start(out=wt[:], in_=w_view[:, :, col0:col0 + CB]), writes=[wt])
                convert_tables(S, T, getattr(T, "conv_p1", 0))
                if kind < 2:
                    for j in range(hpb):
                        h = b * hpb + j
                        st = stq.next()
                        for half in range(2):
                            pm = pmp.next()
                            for cc in range(NC):
                                S.op("pe", lambda e, cc=cc: e.matmul(pm[:, :], lhsT=wt[:, cc, j * 128:(j + 1) * 128], rhs=hnT[:, cc, half * 512:(half + 1) * 512], start=(cc == 0), stop=(cc == NC - 1)), reads=[wt, hnT], writes=[pm])
                            eng = "act" if half == 0 else "dve"
                            if eng == "act":
                                S.op("act", lambda e: e.activation(out=st[:, half * 512:(half + 1) * 512], in_=pm[:, :], func=AF.Copy), reads=[pm], writes=[st])
                            else:
                                S.op("dve", lambda e: e.tensor_copy(out=st[:, half * 512:(half + 1) * 512], in_=pm[:, :]), reads=[pm], writes=[st])
                        if kind == 0:
                            S.dma("sp", lambda e: e.dma_start(out=T.QT[h, :, :], in_=st[:, :]), reads=[st])
                        else:
                            S.dma("sp", lambda e: e.dma_start(out=T.KT[h, :, blk * 1024:(blk + 1) * 1024], in_=st[:, :]), reads=[st])
                else:
                    for tt in range(8):
                        pm = pmp.next()
                        for cc in range(NC):
                            S.op("pe", lambda e, cc=cc: e.matmul(pm[:, 0:CB], lhsT=hnT[:, cc, tt * 128:(tt + 1) * 128], rhs=wt[:, cc, :], start=(cc == 0), stop=(cc == NC - 1)), reads=[wt, hnT], writes=[pm])
                        r0 = blk * 1024 + tt * 128
                        cl = b * CB
                        if kind == 2:
                            st = stv.next()
                            S.op("act", lambda e: e.activation(out=st[:, :], in_=pm[:, 0:CB], func=AF.Copy), reads=[pm], writes=[st])
                            S.dma("sp", lambda e: e.dma_start(out=T.V[r0:r0 + 128, cl:cl + CB], in_=st[:, :]), reads=[st])
                        else:
                            st = stu.next()
                            S.op("dve", lambda e: e.tensor_copy(out=st[:, :], in_=pm[:, 0:CB]), reads=[pm], writes=[st])
                            S.dma("sp", lambda e: e.dma_start(out=T.U[r0:r0 + 128, cl:cl + CB], in_=st[:, :]), reads=[st])
    barrier(S)
    A.release(mk)

def phase2(nc, S, A, c, T):
    mk = A.mark()
    H = c.H
    scale = 128 ** -0.5
    identb = build_identity(S, A, BF16, "identb")
    cb = A.sb("cbias", [128, H], F32)
    eladd = A.sb("eladd", [128, 8, 8], F32); eligp = A.sb("eligp", [128, 8, 8], F32); ownm = A.sb("ownm", [128, 8, 8], F32)
    S.dma("sp", lambda e: e.dma_start(out=cb[:], in_=T.cbias[:, :]), writes=[cb])
    S.dma("sp", lambda e: e.dma_start(out=eladd[:], in_=T.eladd[:, :, :]), writes=[eladd])
    S.dma("sp", lambda e: e.dma_start(out=eligp[:], in_=T.eligp[:, :, :]), writes=[eligp])
    S.dma("sp", lambda e: e.dma_start(out=ownm[:], in_=T.ownm[:, :, :]), writes=[ownm])
    qTs = Pool([A.sb("qT%d" % i, [128, 1024], BF16) for i in range(2)])
    kTs = Pool([A.sb("kT%d" % i, [128, 2048], BF16) for i in range(2)])
    vhs = Pool([A.sb("vh%d" % i, [128, 16, 136], BF16) for i in range(2)])
    for vb_ in vhs.bufs:
        S.op("pool", lambda e: e.memset(vb_[:, :, 128:136], 1.0), writes=[vb_])
    bts = Pool([A.sb("bt%d" % i, [128, 4, 256], F32) for i in range(2)])
    km = A.sb("km", [128, 8], F32); kmb = A.sb("kmb", [128, 8], F32); qf = A.sb("qf", [128, 1024], F32)
    pss = [A.ps("pss%d" % i, [128, 512], F32) for i in range(4)]
    bankA = A.ps("bankA", [128, 512], F32); bankB = A.ps("bankB", [128, 512], F32)
    pg = Buf(bankA[:, 0:8], "pg")
    ptb = Pool([A.ps("ptb%d" % i, [128, 512], BF16) for i in range(2)])
    pop = Pool([Buf(bankA[:, 128:264], "po0"), Buf(bankB[:, 128:264], "po1")])
    gm = A.sb("gm", [128, 8], F32); t8 = A.sb("t8", [128, 8], F32); m1 = A.sb("m1", [128, 8], F32)
    selb = A.sb("selb", [128, 8], F32); selc = A.sb("selc", [128, 8], F32)
    lgs = Pool([A.sb("lg%d" % i, [128, 256], F32) for i in range(2)])
    pexps = Pool([A.sb("pexp%d" % i, [128, 2048], BF16) for i in range(2)])
    rss = Pool([A.sb("rs%d" % i, [128, 8], F32) for i in range(2)])
    rsum = A.sb("rsum", [128, 1], F32); rinv = A.sb("rinv", [128, 1], F32)
    pTs = Pool([A.sb("pT%d" % i, [128, 4, 128], BF16) for i in range(4)])
    ysts = Pool([A.sb("yst%d" % i, [128, 128], F32) for i in range(2)])
    Vv = T.V.rearrange("(kt p) d -> p kt d", p=128)
    heads = {}
    def load_head(h):
        qT = qTs.next(); kT = kTs.next(); vh = vhs.next(); bt = bts.next()
        S.dma("sp", lambda e: e.dma_start(out=qT[:], in_=T.QT[h, :, :]), writes=[qT])
        S.dma("sp", lambda e: e.dma_start(out=kT[:], in_=T.KT[h, :, :]), writes=[kT])
        S.dma("sp", lambda e: e.dma_start(out=vh[:, :, 0:128], in_=Vv[:, :, h * 128:(h + 1) * 128]), writes=[vh])
        S.dma("sp", lambda e: e.dma_start(out=bt[:], in_=T.abias[h, :, :, :]), writes=[bt])
        heads[h] = (qT, kT, vh, bt)
    def stage_a(h, qt):
        if qt == 0:
            if h not in heads:
                load_head(h)
            qT, kT, vh, bt = heads[h]
            S.op("dve", lambda e: e.tensor_reduce(out=km[:, :], in_=kT[:, :].rearrange("p (n k) -> p n k", k=256), axis=AX.X, op=ALU.add), reads=[kT], writes=[km])
            S.op("dve", lambda e: e.tensor_scalar(out=kmb[:, :], in0=km[:, :], scalar1=1.0 / 256, scalar2=None, op0=ALU.mult), reads=[km], writes=[kmb])
            S.op("act", lambda e: e.activation(out=qf[:, :], in_=qT[:, :], func=AF.Copy), reads=[qT], writes=[qf])
        if qt == 1 and h + 1 < H:
            load_head(h + 1)
        qT, kT, vh, bt = heads[h]
        nown = 4 + qt // 2
        nblk = nown + 1
        qs = qT[:, qt * 128:(qt + 1) * 128]
        convert_tables(S, T, T.conv_per_step)
        S.op("pe", lambda e: e.matmul(pg[:, :], lhsT=qf[:, qt * 128:(qt + 1) * 128], rhs=kmb[:, :], start=True, stop=True), reads=[qf, kmb], writes=[pg])
        S.op("dve", lambda e: e.tensor_tensor(out=gm[:, :], in0=pg[:, :], in1=eladd[:, qt, :], op=ALU.add), reads=[pg, eladd], writes=[gm])
        S.op("dve", lambda e: e.max(out=t8[:, :], in_=gm[:, :]), reads=[gm], writes=[t8])
        S.op("dve", lambda e: e.tensor_scalar(out=m1[:, :], in0=gm[:, :], scalar1=t8[:, 2:3], scalar2=None, op0=ALU.is_ge), reads=[gm, t8], writes=[m1])
        S.op("dve", lambda e: e.tensor_tensor(out=m1[:, :], in0=m1[:, :], in1=eligp[:, qt, :], op=ALU.mult), reads=[m1, eligp], writes=[m1])
        S.op("dve", lambda e: e.tensor_tensor(out=m1[:, :], in0=m1[:, :], in1=ownm[:, qt, :], op=ALU.add), reads=[m1, ownm], writes=[m1])
        S.op("dve", lambda e: e.tensor_scalar(out=selb[:, :], in0=m1[:, :], scalar1=1.0, scalar2=1e30, op0=ALU.subtract, op1=ALU.mult), reads=[m1], writes=[selb])
        S.op("dve", lambda e: e.tensor_scalar(out=selc[:, :], in0=selb[:, :], scalar1=cb[:, h:h + 1], scalar2=None, op0=ALU.add), reads=[selb, cb], writes=[selc])
        def bank_of(n):
            if n >= nown - 1:
                return pss[3], (n - (nown - 1)) * 256
            return pss[n // 2], (n % 2) * 256
        for n in range(nblk):
            ps, po_ = bank_of(n)
            S.op("pe", lambda e: e.matmul(ps[:, po_:po_ + 256], lhsT=qs, rhs=kT[:, n * 256:(n + 1) * 256], start=True, stop=True), reads=[qT, kT], writes=[ps])
        pexp = pexps.next()
        for n in range(nblk):
            ps, po_ = bank_of(n)
            pslice = ps[:, po_:po_ + 256]
            if n >= nown - 1:
                v = (0 if n == nown else 2) + (qt % 2)
                lg = lgs.next()
                S.op("dve", lambda e: e.scalar_tensor_tensor(out=lg[:, :], in0=pslice, scalar=scale, in1=bt[:, v, :], op0=ALU.mult, op1=ALU.add), reads=[ps, bt], writes=[lg])
                S.op("act", lambda e: e.activation(out=pexp[:, n * 256:(n + 1) * 256], in_=lg[:, :], func=AF.Exp, bias=selb[:, n:n + 1], scale=1.0), reads=[lg, selb], writes=[pexp])
            else:
                S.op("act", lambda e: e.activation(out=pexp[:, n * 256:(n + 1) * 256], in_=pslice, func=AF.Exp, bias=selc[:, n:n + 1], scale=scale), reads=[ps, selc], writes=[pexp])
        return pexp, nblk
    def stage_b(h, qt, pexp, nblk):
        qT, kT, vh, bt = heads[h]
        po = pop.next()
        nkt = 2 * nblk
        groups = list(range(0, nkt, 4))
        tiles = {}
        def emit_t(gi):
            k0 = groups[gi]
            pt = ptb.next(); pT = pTs.next()
            nj = min(4, nkt - k0)
            for j in range(nj):
                kt = k0 + j
                S.op("pe", lambda e: e.transpose(out=pt[:, j * 128:(j + 1) * 128], in_=pexp[:, kt * 128:(kt + 1) * 128], identity=identb[:]), reads=[pexp, identb], writes=[pt])
            if gi % 2 == 0:
                S.op("act", lambda e: e.activation(out=pT[:, 0:nj, :], in_=pt[:, 0:nj * 128].rearrange("p (a b) -> p a b", a=nj), func=AF.Copy), reads=[pt], writes=[pT])
            else:
                S.op("dve", lambda e: e.tensor_copy(out=pT[:, 0:nj, :], in_=pt[:, 0:nj * 128].rearrange("p (a b) -> p a b", a=nj)), reads=[pt], writes=[pT])
            tiles[gi] = (pT, nj, k0)
        def emit_pv(gi):
            pT, nj, k0 = tiles.pop(gi)
            for j in range(nj):
                kt = k0 + j
                S.op("pe", lambda e: e.matmul(po[:, 0:129], lhsT=pT[:, j, :], rhs=vh[:, kt, 0:129], start=(kt == 0), stop=(kt == nkt - 1)), reads=[pT, vh], writes=[po])
        emit_t(0)
        for gi in range(len(groups)):
            if gi + 1 < len(groups):
                emit_t(gi + 1)
            emit_pv(gi)
        yst = ysts.next()
        S.op("dve", lambda e: e.reciprocal(out=rinv[:, :], in_=po[:, 128:129]), reads=[po], writes=[rinv])
        S.op("dve", lambda e: e.tensor_scalar(out=yst[:, :], in0=po[:, 0:128], scalar1=rinv[:, 0:1], scalar2=None, op0=ALU.mult), reads=[po, rinv], writes=[yst])
        S.dma("sp", lambda e: e.dma_start(out=T.YATT[qt * 128:(qt + 1) * 128, h * 128:(h + 1) * 128], in_=yst[:, :]), reads=[yst])
    units = [(h, qt) for h in range(H) for qt in range(8)]
    prev = None
    for (h, qt) in units:
        cur = stage_a(h, qt)
        if prev is not None:
            stage_b(*prev)
        prev = (h, qt) + cur
    stage_b(*prev)
    barrier(S)
    A.release(mk)

def t5_bucket_np(n):
    import math
    n = np.maximum(n, 0)
    r = np.log(np.maximum(n, 1).astype(np.float32) / np.float32(16)) / np.float32(math.log(128 / 16))
    large = 16 + (r.astype(np.float32) * np.float32(16)).astype(np.int32)
    large = np.minimum(large, 31)
    return np.where(n < 16, n, large)

def host_attn_tables(rel_bias, half):
    H = rel_bias.shape[1]
    i = np.arange(128)[:, None]; j = np.arange(256)[None, :]
    ab = np.empty((H, 128, 4, 256), np.float32)
    for v in range(4):
        qoff = (v % 2) * 128
        dist = qoff + i - j + (256 if v >= 2 else 0)
        bk = t5_bucket_np(dist)
        btl = rel_bias[bk, :]
        btl = np.where((dist >= 0)[:, :, None], btl, np.float32(-1e30))
        ab[:, :, v, :] = btl.transpose(2, 0, 1)
    cbias = np.ascontiguousarray(np.broadcast_to(rel_bias[31, :][None, :], (128, H))).astype(np.float32)
    eligp = np.zeros((8, 8), np.float32); ownm = np.zeros((8, 8), np.float32)
    for qt in range(8):
        own = 4 + qt // 2
        ownm[qt, own] = 1.0
        lo = 0 if half == 1 else 4
        eligp[qt, lo:own] = 1.0
    eladd = ((eligp - 1.0) * np.float32(1e30)).astype(np.float32)
    bc = lambda a: np.ascontiguousarray(np.broadcast_to(a[None], (128, 8, 8))).astype(np.float32)
    return ab, cbias, bc(eladd), bc(eligp), bc(ownm)

TWO_PI = 6.283185307179586
MAGIC = 12582912.0

def sincos(S, ang, shape, tmpa, tmpb, out_sin, out_cos, defer_act=False):
    a, ta, tb, osn, ocs = ang, tmpa, tmpb, out_sin, out_cos
    S.op("dve", lambda e: e.tensor_scalar(out=ta[1](ta[0]), in0=a[1](a[0]), scalar1=1.0 / TWO_PI, scalar2=MAGIC, op0=ALU.mult, op1=ALU.add), reads=[a[0]], writes=[ta[0]])
    S.op("dve", lambda e: e.tensor_scalar(out=ta[1](ta[0]), in0=ta[1](ta[0]), scalar1=MAGIC, scalar2=None, op0=ALU.subtract), reads=[ta[0]], writes=[ta[0]])
    S.op("dve", lambda e: e.scalar_tensor_tensor(out=tb[1](tb[0]), in0=ta[1](ta[0]), scalar=-TWO_PI, in1=a[1](a[0]), op0=ALU.mult, op1=ALU.add), reads=[ta[0], a[0]], writes=[tb[0]])
    S.op("dve", lambda e: e.tensor_scalar(out=tb[1](tb[0]), in0=tb[1](tb[0]), scalar1=-3.1415925, scalar2=3.1415925, op0=ALU.max, op1=ALU.min), reads=[tb[0]], writes=[tb[0]])
    late = []
    sin1 = lambda: S.op("act", lambda e: e.activation(out=osn[1](osn[0]), in_=tb[1](tb[0]), func=AF.Sin), reads=[tb[0]], writes=[osn[0]])
    if defer_act:
        late.append(sin1)
    else:
        sin1()
    S.op("dve", lambda e: e.tensor_scalar(out=ta[1](ta[0]), in0=tb[1](tb[0]), scalar1=1.5707963, scalar2=-TWO_PI, op0=ALU.is_gt, op1=ALU.mult), reads=[tb[0]], writes=[ta[0]])
    S.op("dve", lambda e: e.scalar_tensor_tensor(out=ta[1](ta[0]), in0=tb[1](tb[0]), scalar=1.5707963, in1=ta[1](ta[0]), op0=ALU.add, op1=ALU.add), reads=[tb[0], ta[0]], writes=[ta[0]])
    S.op("dve", lambda e: e.tensor_scalar(out=ta[1](ta[0]), in0=ta[1](ta[0]), scalar1=-3.1415925, scalar2=3.1415925, op0=ALU.max, op1=ALU.min), reads=[ta[0]], writes=[ta[0]])
    sin2 = lambda: S.op("act", lambda e: e.activation(out=ocs[1](ocs[0]), in_=ta[1](ta[0]), func=AF.Sin), reads=[ta[0]], writes=[ocs[0]])
    if defer_act:
        late.append(sin2)
    else:
        sin2()
    return late

def phase3(nc, S, A, c, T):
    mk = A.mark()
    G = c.G
    full = lambda b: b[:]
    ident = build_identity(S, A)
    def load(name, shape, src):
        b = A.sb(name, shape, F32)
        S.dma("sp", lambda e: e.dma_start(out=b[:], in_=src), writes=[b])
        return b
    sh3 = [64, G, 24]; sB = [64, G, 16]
    Are = A.sb("Are", sh3, F32); Aim = A.sb("Aim", sh3, F32)
    Bre = A.sb("Bre", sB, F32); Bim = A.sb("Bim", sB, F32)
    th8 = A.sb("th8", [64, G], F32); r8 = A.sb("r8", [64, G], F32)
    cre = load("cre", [64, G, 16], T.c_re[:, :, :]); cim = load("cim", [64, G, 16], T.c_im[:, :, :])
    dbcs = Pool([A.sb("dbc%d" % i, [128, 128], F32) for i in range(2)])
    nidx = load("nidx", [64, 256], T.nidx[:, :]); cmask = load("cmask", [128, 128], T.cmask[:, :])
    mk_tmp = A.mark()
    lre = load("lre", [64, G], T.lam_re[:, :]); lim = load("lim", [64, G], T.lam_im[:, :]); ldt = load("ldt", [64, G], T.logdt[:, :])
    bre = load("bre", [64, G, 16], T.b_re[:, :, :]); bim = load("bim", [64, G, 16], T.b_im[:, :, :])
    mv = load("mv", [64, 24], T.mv24[:, :])
    dt = A.sb("dt", [64, G], F32); lrd = A.sb("lrd", [64, G], F32); lid = A.sb("lid", [64, G], F32)
    S.op("act", lambda e: e.activation(out=dt[:], in_=ldt[:], func=AF.Exp), reads=[ldt], writes=[dt])
    S.op("dve", lambda e: e.tensor_tensor(out=lrd[:], in0=lre[:], in1=dt[:], op=ALU.mult), reads=[lre, dt], writes=[lrd])
    S.op("dve", lambda e: e.tensor_tensor(out=lid[:], in0=lim[:], in1=dt[:], op=ALU.mult), reads=[lim, dt], writes=[lid])
    arg = A.sb("arg", sh3, F32); mag = A.sb("mag", sh3, F32); ang = A.sb("ang", sh3, F32)
    ta = A.sb("ta", sh3, F32); tb = A.sb("tb", sh3, F32)
    mvb = mv[:, :].unsqueeze(1).to_broadcast(sh3)
    S.op("dve", lambda e: e.tensor_tensor(out=arg[:], in0=lrd[:, :].unsqueeze(2).to_broadcast(sh3), in1=mvb, op=ALU.mult), reads=[lrd, mv], writes=[arg])
    S.op("act", lambda e: e.activation(out=mag[:], in_=arg[:], func=AF.Exp), reads=[arg], writes=[mag])
    S.op("dve", lambda e: e.tensor_tensor(out=ang[:], in0=lid[:, :].unsqueeze(2).to_broadcast(sh3), in1=mvb, op=ALU.mult), reads=[lid, mv], writes=[ang])
    sincos(S, (ang, full), sh3, (ta, full), (tb, full), (Aim, full), (Are, full))
    S.op("dve", lambda e: e.tensor_copy(out=th8[:], in_=tb[:, :, 23]), reads=[tb], writes=[th8])
    S.op("dve", lambda e: e.tensor_copy(out=r8[:], in_=mag[:, :, 23]), reads=[mag], writes=[r8])
    S.op("dve", lambda e: e.tensor_tensor(out=Are[:], in0=Are[:], in1=mag[:], op=ALU.mult), reads=[Are, mag], writes=[Are])
    S.op("dve", lambda e: e.tensor_tensor(out=Aim[:], in0=Aim[:], in1=mag[:], op=ALU.mult), reads=[Aim, mag], writes=[Aim])
    nr = A.sb("nr", [64, G], F32); t1 = A.sb("t1", [64, G], F32); t2 = A.sb("t2", [64, G], F32); den = A.sb("den", [64, G], F32)
    cfr = A.sb("cfr", [64, G], F32); cfi = A.sb("cfi", [64, G], F32)
    S.op("dve", lambda e: e.tensor_scalar(out=nr[:], in0=Are[:, :, 16], scalar1=-1.0, scalar2=None, op0=ALU.add), reads=[Are], writes=[nr])
    S.op("dve", lambda e: e.tensor_tensor(out=t1[:], in0=lre[:], in1=lre[:], op=ALU.mult), reads=[lre], writes=[t1])
    S.op("dve", lambda e: e.tensor_tensor(out=t2[:], in0=lim[:], in1=lim[:], op=ALU.mult), reads=[lim], writes=[t2])
    S.op("dve", lambda e: e.tensor_tensor(out=den[:], in0=t1[:], in1=t2[:], op=ALU.add), reads=[t1, t2], writes=[den])
    S.op("dve", lambda e: e.reciprocal(out=den[:], in_=den[:]), reads=[den], writes=[den])
    S.op("dve", lambda e: e.tensor_tensor(out=t1[:], in0=nr[:], in1=lre[:], op=ALU.mult), reads=[nr, lre], writes=[t1])
    S.op("dve", lambda e: e.tensor_tensor(out=t2[:], in0=Aim[:, :, 16], in1=lim[:], op=ALU.mult), reads=[Aim, lim], writes=[t2])
    S.op("dve", lambda e: e.tensor_tensor(out=t1[:], in0=t1[:], in1=t2[:], op=ALU.add), reads=[t1, t2], writes=[t1])
    S.op("dve", lambda e: e.tensor_tensor(out=cfr[:], in0=t1[:], in1=den[:], op=ALU.mult), reads=[t1, den], writes=[cfr])
    S.op("dve", lambda e: e.tensor_tensor(out=t1[:], in0=Aim[:, :, 16], in1=lre[:], op=ALU.mult), reads=[Aim, lre], writes=[t1])
    S.op("dve", lambda e: e.tensor_tensor(out=t2[:], in0=nr[:], in1=lim[:], op=ALU.mult), reads=[nr, lim], writes=[t2])
    S.op("dve", lambda e: e.tensor_tensor(out=t1[:], in0=t1[:], in1=t2[:], op=ALU.subtract), reads=[t1, t2], writes=[t1])
    S.op("dve", lambda e: e.tensor_tensor(out=cfi[:], in0=t1[:], in1=den[:], op=ALU.mult), reads=[t1, den], writes=[cfi])
    tB1 = A.sb("tB1", sB, F32); tB2 = A.sb("tB2", sB, F32)
    cfrb = cfr[:, :].unsqueeze(2).to_broadcast(sB); cfib = cfi[:, :].unsqueeze(2).to_broadcast(sB)
    S.op("dve", lambda e: e.tensor_tensor(out=tB1[:], in0=bre[:], in1=cfrb, op=ALU.mult), reads=[bre, cfr], writes=[tB1])
    S.op("dve", lambda e: e.tensor_tensor(out=tB2[:], in0=bim[:], in1=cfib, op=ALU.mult), reads=[bim, cfi], writes=[tB2])
    S.op("dve", lambda e: e.tensor_tensor(out=Bre[:], in0=tB1[:], in1=tB2[:], op=ALU.subtract), reads=[tB1, tB2], writes=[Bre])
    S.op("dve", lambda e: e.tensor_tensor(out=tB1[:], in0=bim[:], in1=cfrb, op=ALU.mult), reads=[bim, cfr], writes=[tB1])
    S.op("dve", lambda e: e.tensor_tensor(out=tB2[:], in0=bre[:], in1=cfib, op=ALU.mult), reads=[bre, cfi], writes=[tB2])
    S.op("dve", lambda e: e.tensor_tensor(out=Bim[:], in0=tB1[:], in1=tB2[:], op=ALU.add), reads=[tB1, tB2], writes=[Bim])
    barrier(S)
    A.release(mk_tmp)
    s4 = [64, 8, 8, 16]
    s4f = [64, 8, 128]
    NTre = A.sb("NTre", s4f, F32); NTim = A.sb("NTim", s4f, F32); NPre = A.sb("NPre", s4f, F32); NPim = A.sb("NPim", s4f, F32)
    Rs = [(A.sb("Rre%d" % i, s4f, F32), A.sb("Rim%d" % i, s4f, F32)) for i in range(2)]
    q1 = A.sb("q1", s4, F32); q2 = A.sb("q2", s4, F32)
    v4 = lambda b: b[:].rearrange("p g (s c) -> p g s c", s=8)
    Utm2 = A.sb("Utm2", [128, 2, 8, 128], F32)
    Utms = Pool([A.sb("Utm%d" % i, [128, 2, 8, 128], F32) for i in range(1)])
    Ytm = A.sb("Ytm", [128, 8, 128], F32); Ytmp = A.sb("Ytmp", [128, 8, 128], F32)
    Ug = A.sb("Ug", [128, 8, 256], F32); Mg = A.sb("Mg", [128, 8, 128], F32)
    Ngs = Pool([A.sb("Ng%d" % i, [128, 128], F32) for i in range(2)])
    s3 = [64, 8, 256]
    Xre = A.sb("Xre", s3, F32); Xim = A.sb("Xim", s3, F32); Ec = A.sb("Ec", s3, F32); Es = A.sb("Es", s3, F32)
    w1 = A.sb("w1", s3, F32); w2 = A.sb("w2", s3, F32); Gre = A.sb("Gre", s3, F32); Gim = A.sb("Gim", s3, F32)
    Sre = Xre; Sim = Xim
    Ygs = Pool([A.sb("Yg%d" % i, [128, 128], F32) for i in range(2)])
    def bank(name, p, w):
        b = A.ps(name, [128, 512], F32)
        return Buf(b[0:p, 0:w], name)
    ptU = Pool([bank("ptU%d" % i, 128, 256) for i in range(2)])
    pN = bank("pN", 128, 128); px = bank("px", 64, 512); pmgs = [bank("pmgA", 128, 512), bank("pmgB", 128, 512)]
    py = bank("py", 128, 128); ptY = bank("ptY", 128, 128)
    Uv = T.U.rearrange("(b n s) ch -> n b s ch", b=2, n=128, s=8)
    Yv = T.YSSM.rearrange("(n t) ch -> n t ch", t=8)
    def cmul(outre, outim, are, aim, xre, xim, rd, neg_im=False):
        S.op("dve", lambda e: e.tensor_tensor(out=q1[:], in0=are, in1=xre, op=ALU.mult), reads=rd, writes=[q1])
        S.op("dve", lambda e: e.tensor_tensor(out=q2[:], in0=aim, in1=xim, op=ALU.mult), reads=rd, writes=[q2])
        S.op("dve", lambda e: e.tensor_tensor(out=v4(outre), in0=q1[:], in1=q2[:], op=ALU.subtract), reads=[q1, q2], writes=[outre])
        S.op("dve", lambda e: e.tensor_tensor(out=q1[:], in0=are, in1=xim, op=ALU.mult), reads=rd, writes=[q1])
        S.op("dve", lambda e: e.tensor_tensor(out=q2[:], in0=aim, in1=xre, op=ALU.mult), reads=rd, writes=[q2])
        if neg_im:
            S.op("dve", lambda e: e.scalar_tensor_tensor(out=v4(outim), in0=q1[:], scalar=-1.0, in1=q2[:], op0=ALU.mult, op1=ALU.subtract), reads=[q1, q2], writes=[outim])
        else:
            S.op("dve", lambda e: e.tensor_tensor(out=v4(outim), in0=q1[:], in1=q2[:], op=ALU.add), reads=[q1, q2], writes=[outim])
    stop = getattr(T, "stop", 0)
    NB = G // 8
    def apw(tab, g0, m0):
        return tab[:, g0:g0 + 8, m0:m0 + 8].unsqueeze(3).to_broadcast(s4)
    def apv(tab, g0):
        return tab[:, g0:g0 + 8, :].unsqueeze(2).to_broadcast(s4)
    def emit_cmul(gb):
        g0 = gb * 8
        Rre, Rim = Rs[gb % 2]
        cmul(NTre, NTim, apw(Are, g0, 0), apw(Aim, g0, 0), apv(Bre, g0), apv(Bim, g0), [Are, Aim, Bre, Bim])
        cmul(NPre, NPim, apw(Are, g0, 8), apw(Aim, g0, 8), apv(Bre, g0), apv(Bim, g0), [Are, Aim, Bre, Bim])
        cmul(Rre, Rim, apw(Are, g0, 16), apw(Aim, g0, 16), apv(cre, g0), apv(cim, g0), [Are, Aim, cre, cim], neg_im=True)
    emit_cmul(0)
    def load_u(gb_):
        ch_ = gb_ * 128
        Utm_ = Utms.next(); dbc_ = dbcs.next()
        for blk_ in range(2):
            S.dma("sp", lambda e: e.dma_start(out=Utm_[:, blk_, :, :], in_=Uv[:, blk_, :, ch_:ch_ + 128]), writes=[Utm_])
        S.dma("sp", lambda e: e.dma_start(out=dbc_[:], in_=T.d_bc[:, ch_:ch_ + 128]), writes=[dbc_])
        return Utm_, dbc_
    nxt_u = load_u(0)
    for gb in range(NB):
        g0 = gb * 8; ch0 = gb * 128
        Rre, Rim = Rs[gb % 2]
        Utm, dbc = nxt_u
        for blk in range(2):
            S.op("dve", lambda e: e.tensor_copy(out=Utm2[:, blk, :, :].rearrange("p j (s c) -> p j s c", s=8), in_=Utm[:, blk, :, :].rearrange("p s (j c) -> p j s c", j=8)), reads=[Utm], writes=[Utm2])
        if gb + 1 < NB:
            nxt_u = load_u(gb + 1)
        th = th8[:, g0:g0 + 8].unsqueeze(2).to_broadcast(s3)
        S.op("dve", lambda e: e.tensor_tensor(out=w1[:], in0=th, in1=nidx[:, :].unsqueeze(1).to_broadcast(s3), op=ALU.mult), reads=[th8, nidx], writes=[w1])
        late = sincos(S, (w1, full), s3, (w2, full), (Gre, full), (Es, full), (Ec, full), defer_act=True)
        for j in range(8):
            g = g0 + j
            pt = ptU.next()
            for blk in range(2):
                S.op("pe", lambda e: e.transpose(out=pt[:, blk * 128:(blk + 1) * 128], in_=Utm2[:, blk, j, :], identity=ident[:]), reads=[Utm2, ident], writes=[pt])
            S.op("act", lambda e: e.activation(out=Ug[:, j, :], in_=pt[:, :], func=AF.Copy), reads=[pt], writes=[Ug])
            S.op("pe", lambda e: e.transpose(out=pN[:, 0:64], in_=NTre[:, j, :], identity=ident[0:64, 0:64]), reads=[NTre, ident], writes=[pN])
            S.op("pe", lambda e: e.transpose(out=pN[:, 64:128], in_=NTim[:, j, :], identity=ident[0:64, 0:64]), reads=[NTim, ident], writes=[pN])
            Ng = Ngs.next()
            S.op("act", lambda e: e.activation(out=Ng[:, :], in_=pN[:, :], func=AF.Copy), reads=[pN], writes=[Ng])
            S.op("pe", lambda e: e.matmul(px[:, 0:256], lhsT=Ng[:, 0:64], rhs=Ug[:, j, :], start=True, stop=True), reads=[Ng, Ug], writes=[px])
            S.op("pe", lambda e: e.matmul(px[:, 256:512], lhsT=Ng[:, 64:128], rhs=Ug[:, j, :], start=True, stop=True), reads=[Ng, Ug], writes=[px])
            S.op("act", lambda e: e.activation(out=Xre[:, j, :], in_=px[:, 0:256], func=AF.Copy), reads=[px], writes=[Xre])
            S.op("act", lambda e: e.activation(out=Xim[:, j, :], in_=px[:, 256:512], func=AF.Copy), reads=[px], writes=[Xim])
            pm_ = pmgs[j // 4]; c0_ = (j % 4) * 128
            S.op("pe", lambda e: e.matmul(pm_[:, c0_:c0_ + 128], lhsT=NPre[:, j, :], rhs=Rre[:, j, :], start=True, stop=False), reads=[NPre, Rre], writes=[pm_])
            S.op("pe", lambda e: e.matmul(pm_[:, c0_:c0_ + 128], lhsT=NPim[:, j, :], rhs=Rim[:, j, :], start=False, stop=True), reads=[NPim, Rim], writes=[pm_])
        for th_ in late:
            th_()
        for hb_ in range(2):
            S.op("dve", lambda e: e.tensor_tensor(out=Mg[:, hb_ * 4:(hb_ + 1) * 4, :], in0=pmgs[hb_][:, :].rearrange("p (a b) -> p a b", a=4), in1=cmask[:, :].unsqueeze(1).to_broadcast([128, 4, 128]), op=ALU.mult), reads=[pmgs[hb_], cmask], writes=[Mg])
        S.op("dve", lambda e: e.tensor_tensor(out=w1[:], in0=Xre[:], in1=Ec[:], op=ALU.mult), reads=[Xre, Ec], writes=[w1])
        S.op("dve", lambda e: e.tensor_tensor(out=w2[:], in0=Xim[:], in1=Es[:], op=ALU.mult), reads=[Xim, Es], writes=[w2])
        S.op("dve", lambda e: e.tensor_tensor(out=Gre[:], in0=w1[:], in1=w2[:], op=ALU.add), reads=[w1, w2], writes=[Gre])
        S.op("dve", lambda e: e.tensor_tensor(out=w1[:], in0=Xim[:], in1=Ec[:], op=ALU.mult), reads=[Xim, Ec], writes=[w1])
        S.op("dve", lambda e: e.tensor_tensor(out=w2[:], in0=Xre[:], in1=Es[:], op=ALU.mult), reads=[Xre, Es], writes=[w2])
        S.op("dve", lambda e: e.tensor_tensor(out=Gim[:], in0=w1[:], in1=w2[:], op=ALU.subtract), reads=[w1, w2], writes=[Gim])
        for j in range(8):
            g = g0 + j
            coef = r8[:, g:g + 1].to_broadcast([64, 256])
            S.op("dve", lambda e: e.tensor_tensor_scan(out=Sre[:, j, :], data0=coef, data1=Gre[:, j, :], initial=0.0, op0=ALU.mult, op1=ALU.add), reads=[r8, Gre], writes=[Sre])
            S.op("dve", lambda e: e.tensor_tensor_scan(out=Sim[:, j, :], data0=coef, data1=Gim[:, j, :], initial=0.0, op0=ALU.mult, op1=ALU.add), reads=[r8, Gim], writes=[Sim])
        S.op("dve", lambda e: e.tensor_tensor(out=w1[:], in0=Sre[:], in1=Ec[:], op=ALU.mult), reads=[Sre, Ec], writes=[w1])
        S.op("dve", lambda e: e.tensor_tensor(out=w2[:], in0=Sim[:], in1=Es[:], op=ALU.mult), reads=[Sim, Es], writes=[w2])
        S.op("dve", lambda e: e.tensor_tensor(out=Gre[:], in0=w1[:], in1=w2[:], op=ALU.subtract), reads=[w1, w2], writes=[Gre])
        S.op("dve", lambda e: e.tensor_tensor(out=w1[:], in0=Sre[:], in1=Es[:], op=ALU.mult), reads=[Sre, Es], writes=[w1])
        S.op("dve", lambda e: e.tensor_tensor(out=w2[:], in0=Sim[:], in1=Ec[:], op=ALU.mult), reads=[Sim, Ec], writes=[w2])
        S.op("dve", lambda e: e.tensor_tensor(out=Gim[:], in0=w1[:], in1=w2[:], op=ALU.add), reads=[w1, w2], writes=[Gim])
        if gb + 1 < NB:
            emit_cmul(gb + 1)
        for j in range(8):
            S.op("pe", lambda e: e.matmul(py[:, :], lhsT=Mg[:, j, :], rhs=Ug[:, j, 128:256], start=True, stop=False), reads=[Mg, Ug], writes=[py])
            S.op("pe", lambda e: e.matmul(py[:, :], lhsT=Rre[:, j, :], rhs=Gre[:, j, 127:255], start=False, stop=False), reads=[Rre, Gre], writes=[py])
            S.op("pe", lambda e: e.matmul(py[:, :], lhsT=Rim[:, j, :], rhs=Gim[:, j, 127:255], start=False, stop=True), reads=[Rim, Gim], writes=[py])
            Yg = Ygs.next()
            S.op("act", lambda e: e.activation(out=Yg[:, :], in_=py[:, :], func=AF.Copy), reads=[py], writes=[Yg])
            S.op("pe", lambda e: e.transpose(out=ptY[:, :], in_=Yg[:, :], identity=ident[:]), reads=[Yg, ident], writes=[ptY])
            S.op("act", lambda e: e.activation(out=Ytm[:, :, j * 16:(j + 1) * 16], in_=ptY[:, :].rearrange("p (t c) -> p t c", t=8), func=AF.Copy), reads=[ptY], writes=[Ytm])
        S.op("dve", lambda e: e.tensor_tensor(out=Ytmp[:].rearrange("p s (j c) -> p s j c", j=8), in0=Utm2[:, 1, :, :].rearrange("p j (s c) -> p s j c", s=8), in1=dbc[:, :].rearrange("p (j c) -> p j c", j=8).unsqueeze(1).to_broadcast([128, 8, 8, 16]), op=ALU.mult), reads=[Utm2, dbc], writes=[Ytmp])
        S.op("dve", lambda e: e.tensor_tensor(out=Ytmp[:], in0=Ytmp[:], in1=Ytm[:], op=ALU.add), reads=[Ytmp, Ytm], writes=[Ytmp])
        S.dma("sp", lambda e: e.dma_start(out=Yv[:, :, ch0:ch0 + 128], in_=Ytmp[:]), reads=[Ytmp])
    barrier(S)
    A.release(mk)

def host_ssm_tables(lam_re, lam_im, log_dt, b_re, b_im, c_re, c_im, d):
    G = lam_re.shape[0]
    f = lambda a: np.ascontiguousarray(a).astype(np.float32)
    mv = np.concatenate([7 - np.arange(8), -1 - np.arange(8), 1 + np.arange(8)]).astype(np.float32)
    s = np.arange(128) // 16
    cmask = (s[None, :] >= s[:, None]).astype(np.float32)
    return dict(lam_re=f(lam_re.T), lam_im=f(lam_im.T), logdt=f(np.broadcast_to(log_dt[None, :], (64, G))),
                b_re=f(b_re.transpose(1, 0, 2)), b_im=f(b_im.transpose(1, 0, 2)),
                c_re=f(c_re.transpose(2, 0, 1)), c_im=f(c_im.transpose(2, 0, 1)),
                d_bc=f(np.broadcast_to(d[None, :], (128, d.shape[0]))),
                mv24=f(np.broadcast_to(mv[None], (64, 24))), nidx=f(np.broadcast_to(np.arange(256, dtype=np.float32)[None], (64, 256))),
                cmask=cmask)

def transpose_plain(S, src, W, ident, ptpool, dst, dst_tok0, c0=0, eng="dve"):
    nch = W // 128
    for cg in range(0, nch, 4):
        n = min(4, nch - cg)
        pt = ptpool.next()
        for j in range(n):
            c = cg + j
            S.op("pe", lambda e, c=c, j=j: e.transpose(out=pt[:, j * 128:(j + 1) * 128], in_=src[:, c * 128:(c + 1) * 128], identity=ident[:]), reads=[src, ident], writes=[pt])
        S.op("dve", lambda e, cg=cg, n=n: e.tensor_copy(
            out=dst[:, c0 + cg:c0 + cg + n, dst_tok0:dst_tok0 + 128],
            in_=pt[:, 0:n * 128].rearrange("p (a b) -> p a b", a=n)), reads=[pt], writes=[dst])

def phase4(nc, S, A, c, T):
    mk = A.mark()
    D, Da, Ds, NC = c.D, c.Da, c.Ds, c.NC
    NA = Da // 128; NS = Ds // 128
    ident = build_identity(S, A)
    gout = A.sb("gout", [128, NC], F32)
    S.dma("sp", lambda e: e.dma_start(out=gout[:], in_=T.g_out[:, :]), writes=[gout])
    mixT = A.sb("mixT", [128, NC, 1024], BF16)
    mk2 = A.mark()
    wglu = A.sb("wglu", [128, NS, Ds], BF16)
    wg_view = T.w_glu.rearrange("(c p) n -> p c n", p=128)
    for cc in range(0, NS, 4):
        n = min(4, NS - cc)
        S.dma("pool", lambda e: e.dma_start(out=wglu[:, cc:cc + n, :], in_=wg_view[:, cc:cc + n, :]), writes=[wglu])
    yss = Pool([A.sb("ys%d" % i, [128, Ds], F32) for i in range(2)])
    yas = Pool([A.sb("ya%d" % i, [128, Da], F32) for i in range(2)])
    junk = A.sb("junk4", [128, max(Da, Ds)], BF16)
    ygTs = Pool([A.sb("ygT%d" % i, [128, NS, 128], BF16) for i in range(2)])
    sgs = Pool([A.sb("sg%d" % i, [128, 512], F32) for i in range(2)])
    ssq = Pool([A.sb("ssq4%d" % i, [128, 1], F32) for i in range(2)])
    rstd = Pool([A.sb("rstd4%d" % i, [128, 1], F32) for i in range(2)])
    ptp = Pool([A.ps("pt4%d" % i, [128, 512], F32) for i in range(2)])
    pgp = Pool([A.ps("pg4%d" % i, [128, 512], F32) for i in range(3)])
    GB = min(512, Ds)
    for tt in range(8):
        ys = yss.next(); ya = yas.next(); ygT = ygTs.next()
        S.dma("sp", lambda e: e.dma_start(out=ys[:], in_=T.YSSM[tt * 128:(tt + 1) * 128, :]), writes=[ys])
        S.dma("sp", lambda e: e.dma_start(out=ya[:], in_=T.YATT[tt * 128:(tt + 1) * 128, :]), writes=[ya])
        S.op("act", lambda e: e.activation(out=ys[:], in_=ys[:], func=AF.Gelu), reads=[ys], writes=[ys])
        transpose_plain(S, ys, Ds, ident, ptp, ygT, 0)
        for jb in range(Ds // GB):
            pg = pgp.next(); sg = sgs.next()
            for cc in range(NS):
                S.op("pe", lambda e: e.matmul(pg[:, 0:GB], lhsT=ygT[:, cc, :], rhs=wglu[:, cc, jb * GB:(jb + 1) * GB], start=(cc == 0), stop=(cc == NS - 1)), reads=[ygT, wglu], writes=[pg])
            S.op("act", lambda e: e.activation(out=sg[:, 0:GB], in_=pg[:, 0:GB], func=AF.Sigmoid), reads=[pg], writes=[sg])
            S.op("dve", lambda e: e.tensor_tensor(out=ys[:, jb * GB:(jb + 1) * GB], in0=ys[:, jb * GB:(jb + 1) * GB], in1=sg[:, 0:GB], op=ALU.mult), reads=[ys, sg], writes=[ys])
        sq = ssq.next(); rs = rstd.next()
        rms_rstd(S, ys, Ds, junk, sq, rs)
        S.op("act", lambda e: e.activation(out=ys[:], in_=ys[:], func=AF.Copy, scale=rs[:, 0:1]), reads=[ys, rs], writes=[ys])
        transpose_to(S, ys, Ds, ident, ptp, mixT, tt * 128, gout, c0=NA)
        sq = ssq.next(); rs = rstd.next()
        rms_rstd(S, ya, Da, junk, sq, rs)
        S.op("act", lambda e: e.activation(out=ya[:], in_=ya[:], func=AF.Copy, scale=rs[:, 0:1]), reads=[ya, rs], writes=[ya])
        transpose_to(S, ya, Da, ident, ptp, mixT, tt * 128, gout, c0=0)
    barrier(S)
    A.release(mk2)
    OB = min(512, D)
    wos = Pool([A.sb("wo%d" % i, [128, NC, OB], BF16) for i in range(2)])
    xrs = Pool([A.sb("xr%d" % i, [128, OB], F32) for i in range(3)])
    pop = Pool([A.ps("po4%d" % i, [128, 512], F32) for i in range(4)])
    wo_view = T.w_out.rearrange("(c p) n -> p c n", p=128)
    for db in range(D // OB):
        wo = wos.next()
        S.dma("pool", lambda e: e.dma_start(out=wo[:], in_=wo_view[:, :, db * OB:(db + 1) * OB]), writes=[wo])
        for tt in range(8):
            po = pop.next(); xr = xrs.next()
            S.dma("sp", lambda e: e.dma_start(out=xr[:], in_=T.xall[1024 + tt * 128:1024 + (tt + 1) * 128, db * OB:(db + 1) * OB]), writes=[xr])
            for cc in range(NC):
                S.op("pe", lambda e: e.matmul(po[:, 0:OB], lhsT=mixT[:, cc, tt * 128:(tt + 1) * 128], rhs=wo[:, cc, :], start=(cc == 0), stop=(cc == NC - 1)), reads=[mixT, wo], writes=[po])
            S.op("dve", lambda e: e.tensor_tensor(out=xr[:], in0=xr[:], in1=po[:, 0:OB], op=ALU.add), reads=[xr, po], writes=[xr])
            S.dma("sp", lambda e: e.dma_start(out=T.X1[tt * 128:(tt + 1) * 128, db * OB:(db + 1) * OB], in_=xr[:]), reads=[xr])
    barrier(S)
    A.release(mk)

def phase5(nc, S, A, c, T):
    D, NC = c.D, c.NC
    convert_tables(S, T, 1 << 30)
    mk = A.mark()
    ident = build_identity(S, A)
    gT = A.sb("gffnT", [128, NC], F32); gbc = A.sb("gffnbc", [128, D], F32)
    S.dma("sp", lambda e: e.dma_start(out=gT[:], in_=T.g_ffnT[:, :]), writes=[gT])
    S.dma("sp", lambda e: e.dma_start(out=gbc[:], in_=T.g_ffn_bc[:, :]), writes=[gbc])
    hn2T = A.sb("hn2T", [128, NC, 1024], BF16)
    xts = Pool([A.sb("x5_%d" % i, [128, D], F32) for i in range(2)])
    junk = A.sb("junk5", [128, D], BF16)
    ssq = Pool([A.sb("ssq5%d" % i, [128, 1], F32) for i in range(2)])
    rstd = Pool([A.sb("rstd5%d" % i, [128, 1], F32) for i in range(2)])
    ptp = Pool([A.ps("pt5%d" % i, [128, 512], F32) for i in range(2)])
    pmp = Pool([A.ps("pm5%d" % i, [128, 512], F32) for i in range(4)])
    for tt in range(8):
        xt = xts.next(); sq = ssq.next(); rs = rstd.next()
        S.dma("sp", lambda e: e.dma_start(out=xt[:], in_=T.X1[tt * 128:(tt + 1) * 128, :]), writes=[xt])
        rms_rstd(S, xt, D, junk, sq, rs)
        S.op("act", lambda e: e.activation(out=xt[:], in_=xt[:], func=AF.Copy, scale=rs[:, 0:1]), reads=[xt, rs], writes=[xt])
        transpose_to(S, xt, D, ident, ptp, hn2T, tt * 128, gT)
        S.op("dve", lambda e: e.tensor_tensor(out=junk[:], in0=xt[:], in1=gbc[:], op=ALU.mult), reads=[xt, gbc], writes=[junk])
        S.dma("sp", lambda e: e.dma_start(out=T.HN2[tt * 128:(tt + 1) * 128, :], in_=junk[:]), reads=[junk])
    wqs = Pool([A.sb("wq%d" % i, [128, NC, 512], BF16) for i in range(2)])
    stq = Pool([A.sb("stq5%d" % i, [128, 1024], F32) for i in range(2)])
    wq_view = T.w_q.rearrange("(c p) n -> p c n", p=128)
    for cb in range(4):
        wq = wqs.next()
        S.dma("pool", lambda e: e.dma_start(out=wq[:], in_=wq_view[:, :, cb * 512:(cb + 1) * 512]), writes=[wq])
        for j in range(4):
            cq = cb * 4 + j
            st = stq.next()
            for half in range(2):
                pm = pmp.next()
                for cc in range(NC):
                    S.op("pe", lambda e: e.matmul(pm[:, :], lhsT=wq[:, cc, j * 128:(j + 1) * 128], rhs=hn2T[:, cc, half * 512:(half + 1) * 512], start=(cc == 0), stop=(cc == NC - 1)), reads=[wq, hn2T], writes=[pm])
                if half == 0:
                    S.op("act", lambda e: e.activation(out=st[:, 0:512], in_=pm[:, :], func=AF.Copy), reads=[pm], writes=[st])
                else:
                    S.op("dve", lambda e: e.tensor_copy(out=st[:, 512:1024], in_=pm[:, :]), reads=[pm], writes=[st])
            S.dma("sp", lambda e: e.dma_start(out=T.QPT[cq, :, :], in_=st[:, :]), reads=[st])
    barrier(S)
    A.release(mk)
    mk = A.mark()
    identb = build_identity(S, A, BF16, "identb5")
    NDB = D // 512
    pacc = [A.ps("pacc%d" % i, [128, 512], F32) for i in range(8)]
    mk2 = A.mark()
    keysT = A.sb("keysT", [128, 16, 128], F32)
    S.dma("sp", lambda e: e.dma_start(out=keysT[:], in_=T.keysT[:, :, :]), writes=[keysT])
    qts = Pool([A.sb("qt5%d" % i, [128, 16, 128], F32) for i in range(2)])
    scs = Pool([A.sb("scs%d" % i, [128, 16, 128], F32) for i in range(2)])
    QPv = T.QPT.rearrange("c p t -> p c t")
    for tt in range(8):
        r0 = tt * 128
        qt = qts.next(); sct = scs.next()
        S.dma("sp", lambda e: e.dma_start(out=qt[:], in_=QPv[:, :, r0:r0 + 128]), writes=[qt])
        for cq in range(16):
            ps = pacc[(tt % 2) * 4 + cq // 4]
            S.op("pe", lambda e: e.matmul(ps[:, (cq % 4) * 128:(cq % 4) * 128 + 128], lhsT=qt[:, cq, :], rhs=keysT[:, cq, :], start=True, stop=True), reads=[qt, keysT], writes=[ps])
        for b4 in range(4):
            ps = pacc[(tt % 2) * 4 + b4]
            S.op("act", lambda e: e.activation(out=sct[:, b4 * 4:(b4 + 1) * 4, :], in_=ps[:, :].rearrange("p (a b) -> p a b", a=4), func=AF.Copy), reads=[ps], writes=[sct])
        S.dma("sp", lambda e: e.dma_start(out=T.SCD[r0:r0 + 128, :, :], in_=sct[:]), reads=[sct])
    barrier(S)
    A.release(mk2)
    gpool = Pool([A.sb("gth%d" % i, [128, 2 * D], BF16) for i in range(7)])
    prods = Pool([A.sb("prod%d" % i, [128, D], BF16) for i in range(2)])
    junk2 = A.sb("junk5d", [128, D], BF16)
    PUVv = T.PUV.rearrange("e two d -> e (two d)")
    hgs = Pool([A.sb("hg%d" % i, [128, D], BF16) for i in range(1)])
    xb = A.sb("xb5", [128, D], F32); gfb = A.sb("gfb", [128, D], F32)
    accA = xb
    S.dma("sp", lambda e: e.dma_start(out=gfb[:], in_=T.g_fin_bc[:, :]), writes=[gfb])
    junk = prods.bufs[0]
    sc = A.sb("sc", [128, 16, 128], F32)
    scr = A.sb("scr", [128, 128], F32)
    va = A.sb("va", [128, 16], F32); vb = A.sb("vb", [128, 16], F32)
    iu = A.sb("iu", [128, 16], U32); iaf = A.sb("iaf", [128, 16], F32); ibf = A.sb("ibf", [128, 16], F32)
    cand = A.sb("cand", [128, 256], F32); ecand = A.sb("ecand", [128, 256], F32); cscr = A.sb("cscr", [128, 256], F32)
    vbst = A.sb("vbst", [128, 16], F32); nmx = A.sb("nmx", [128, 1], F32); ex = A.sb("ex", [128, 16], F32)
    zz = A.sb("zz", [128, 1], F32)
    ef = A.sb("ef", [128, 128], F32)
    pu = A.sb("pu", [128, 16], U32); pi_ = A.sb("pi_", [128, 16], U32); pj_ = A.sb("pj_", [128, 16], U32)
    pif = A.sb("pif", [128, 16], F32); pjf = A.sb("pjf", [128, 16], F32); eaf = A.sb("eaf", [128, 16], F32); ebf = A.sb("ebf", [128, 16], F32)
    sel3 = A.sb("sel3", [128, 16, 16], F32)
    iota16 = A.sb("iota16", [128, 16], F32)
    S.dma("sp", lambda e: e.dma_start(out=iota16[:], in_=T.iota16[:, :]), writes=[iota16])
    eis = Pool([A.sb("ei%d" % i, [128, 128], I32) for i in range(2)])
    ggs = Pool([A.sb("gg%d" % i, [128, 128], F32) for i in range(2)])
    araws = Pool([A.sb("araw%d" % i, [128, 1], F32) for i in range(8)])
    wgs = Pool([A.sb("wg%d" % i, [128, 1], F32) for i in range(8)])
    dgs = Pool([A.sb("dg%d" % i, [128, 128], BF16) for i in range(6)])
    sq = A.sb("ssq5c", [128, 1], F32); rs = A.sb("rstd5c", [128, 1], F32)

    def topk_ops(tt, ei, gg):
        ops = []
        r0 = tt * 128
        ops.append(lambda: S.dma("sp", lambda e: e.dma_start(out=sc[:], in_=T.SCD[r0:r0 + 128, :, :]), writes=[sc]))
        def top16(src_ap, vals, idxf):
            ops.append(lambda: S.op("dve", lambda e: e.max(out=vals[:, 0:8], in_=src_ap), reads=[sc], writes=[vals]))
            ops.append(lambda: S.op("dve", lambda e: e.max_index(out=iu[:, 0:8], in_max=vals[:, 0:8], in_values=src_ap), reads=[sc, vals], writes=[iu]))
            ops.append(lambda: S.op("dve", lambda e: e.match_replace(out=scr[:, :], in_to_replace=vals[:, 0:8], in_values=src_ap, imm_value=-1e30), reads=[sc, vals], writes=[scr]))
            ops.append(lambda: S.op("dve", lambda e: e.max(out=vals[:, 8:16], in_=scr[:, :]), reads=[scr], writes=[vals]))
            ops.append(lambda: S.op("dve", lambda e: e.max_index(out=iu[:, 8:16], in_max=vals[:, 8:16], in_values=scr[:, :]), reads=[scr, vals], writes=[iu]))
            ops.append(lambda: S.op("dve", lambda e: e.tensor_copy(out=idxf[:, :], in_=iu[:, :]), reads=[iu], writes=[idxf]))
        c3 = [128, 16, 16]; m3 = [128, 16, 256]
        for h in range(8):
            top16(sc[:, 2 * h, :], va, iaf)
            top16(sc[:, 2 * h + 1, :], vb, ibf)
            ops.append(lambda: S.op("dve", lambda e: e.tensor_tensor(out=cand[:, :].rearrange("p (a b) -> p a b", a=16), in0=va[:, :].unsqueeze(2).to_broadcast(c3), in1=vb[:, :].unsqueeze(1).to_broadcast(c3), op=ALU.add), reads=[va, vb], writes=[cand]))
            ops.append(lambda: S.op("dve", lambda e: e.max(out=vbst[:, 0:8], in_=cand[:, :]), reads=[cand], writes=[vbst]))
            ops.append(lambda: S.op("dve", lambda e: e.max_index(out=pu[:, 0:8], in_max=vbst[:, 0:8], in_values=cand[:, :]), reads=[cand, vbst], writes=[pu]))
            ops.append(lambda: S.op("dve", lambda e: e.match_replace(out=cscr[:, :], in_to_replace=vbst[:, 0:8], in_values=cand[:, :], imm_value=-1e30), reads=[cand, vbst], writes=[cscr]))
            ops.append(lambda: S.op("dve", lambda e: e.max(out=vbst[:, 8:16], in_=cscr[:, :]), reads=[cscr], writes=[vbst]))
            ops.append(lambda: S.op("dve", lambda e: e.max_index(out=pu[:, 8:16], in_max=vbst[:, 8:16], in_values=cscr[:, :]), reads=[cscr, vbst], writes=[pu]))
            ops.append(lambda: S.op("dve", lambda e: e.tensor_single_scalar(out=pi_[:, :], in_=pu[:, :], scalar=4, op=ALU.logical_shift_right), reads=[pu], writes=[pi_]))
            ops.append(lambda: S.op("dve", lambda e: e.tensor_single_scalar(out=pj_[:, :], in_=pu[:, :], scalar=15, op=ALU.bitwise_and), reads=[pu], writes=[pj_]))
            ops.append(lambda: S.op("dve", lambda e: e.tensor_copy(out=pif[:, :], in_=pi_[:, :]), reads=[pi_], writes=[pif]))
            ops.append(lambda: S.op("dve", lambda e: e.tensor_copy(out=pjf[:, :], in_=pj_[:, :]), reads=[pj_], writes=[pjf]))
            ops.append(lambda: S.op("dve", lambda e: e.tensor_tensor(out=sel3[:, :, :], in0=pif[:, :].unsqueeze(2).to_broadcast(c3), in1=iota16[:, :].unsqueeze(1).to_broadcast(c3), op=ALU.is_equal), reads=[pif, iota16], writes=[sel3]))
            ops.append(lambda: S.op("dve", lambda e: e.tensor_tensor(out=sel3[:, :, :], in0=sel3[:, :, :], in1=iaf[:, :].unsqueeze(1).to_broadcast(c3), op=ALU.mult), reads=[sel3, iaf], writes=[sel3]))
            ops.append(lambda: S.op("dve", lambda e: e.tensor_reduce(out=eaf[:, :], in_=sel3[:, :, :], axis=AX.X, op=ALU.add), reads=[sel3], writes=[eaf]))
            ops.append(lambda: S.op("dve", lambda e: e.tensor_tensor(out=sel3[:, :, :], in0=pjf[:, :].unsqueeze(2).to_broadcast(c3), in1=iota16[:, :].unsqueeze(1).to_broadcast(c3), op=ALU.is_equal), reads=[pjf, iota16], writes=[sel3]))
            ops.append(lambda: S.op("dve", lambda e: e.tensor_tensor(out=sel3[:, :, :], in0=sel3[:, :, :], in1=ibf[:, :].unsqueeze(1).to_broadcast(c3), op=ALU.mult), reads=[sel3, ibf], writes=[sel3]))
            ops.append(lambda: S.op("dve", lambda e: e.tensor_reduce(out=ebf[:, :], in_=sel3[:, :, :], axis=AX.X, op=ALU.add), reads=[sel3], writes=[ebf]))
            ops.append(lambda h=h: S.op("dve", lambda e: e.scalar_tensor_tensor(out=ef[:, h * 16:(h + 1) * 16], in0=eaf[:, :], scalar=128.0, in1=ebf[:, :], op0=ALU.mult, op1=ALU.add), reads=[eaf, ebf], writes=[ef]))
            ops.append(lambda: S.op("dve", lambda e: e.tensor_scalar(out=nmx[:, :], in0=vbst[:, 0:1], scalar1=-1.0, scalar2=None, op0=ALU.mult), reads=[vbst], writes=[nmx]))
            ops.append(lambda: S.op("act", lambda e: e.activation(out=ex[:, :], in_=vbst[:, :], func=AF.Exp, bias=nmx[:, 0:1], scale=1.0), reads=[vbst, nmx], writes=[ex]))
            ops.append(lambda: S.op("dve", lambda e: e.tensor_reduce(out=zz[:, :], in_=ex[:, :], axis=AX.X, op=ALU.add), reads=[ex], writes=[zz]))
            ops.append(lambda: S.op("dve", lambda e: e.reciprocal(out=zz[:, :], in_=zz[:, :]), reads=[zz], writes=[zz]))
            ops.append(lambda h=h: S.op("dve", lambda e: e.tensor_scalar(out=gg[:, h * 16:(h + 1) * 16], in0=ex[:, :], scalar1=zz[:, 0:1], scalar2=None, op0=ALU.mult), reads=[ex, zz], writes=[gg]))
        ops.append(lambda: S.op("dve", lambda e: e.tensor_scalar(out=ef[:, :], in0=ef[:, :], scalar1=0.0, scalar2=float(c.NE - 1), op0=ALU.max, op1=ALU.min), reads=[ef], writes=[ef]))
        ops.append(lambda: S.op("dve", lambda e: e.tensor_copy(out=ei[:, :], in_=ef[:, :]), reads=[ef], writes=[ei]))
        return ops

    ei = eis.next(); gg = ggs.next()
    for th in topk_ops(0, ei, gg):
        th()
    LAG = 2
    for tt in range(8):
        r0 = tt * 128
        hg = hgs.next()
        S.dma("sp", lambda e: e.dma_start(out=hg[:], in_=T.HN2[r0:r0 + 128, :]), writes=[hg])
        S.dma("sp", lambda e: e.dma_start(out=xb[:], in_=T.X1[r0:r0 + 128, :]), writes=[xb])
        if tt + 1 < 8:
            ei_n = eis.next(); gg_n = ggs.next()
            nxt = topk_ops(tt + 1, ei_n, gg_n)
        else:
            nxt = []
        per = (len(nxt) + 99) // 100 if nxt else 0
        stage = {}
        def emit_dot(hk):
            gb = gpool.next(); ar = araws.next(); wg = wgs.next()
            S.dma("pool", lambda e: e.indirect_dma_start(out=gb[:], out_offset=None, in_=PUVv, in_offset=bass.IndirectOffsetOnAxis(ap=ei[:, hk:hk + 1], axis=0)), reads=[ei], writes=[gb])
            if hk % 3 == 0:
                S.op("dve", lambda e: e.scalar_tensor_tensor(out=junk[:, :], in0=hg[:, :], scalar=1.0, in1=gb[:, 0:D], op0=ALU.mult, op1=ALU.mult, accum_out=ar[:, 0:1]), reads=[hg, gb], writes=[junk, ar])
            else:
                pr = prods.next()
                S.op("dve", lambda e: e.tensor_tensor(out=pr[:, :], in0=hg[:, :], in1=gb[:, 0:D], op=ALU.mult), reads=[hg, gb], writes=[pr])
                S.op("act", lambda e: e.activation(out=junk2[:, :], in_=pr[:, :], func=AF.Copy, accum_out=ar[:, 0:1]), reads=[pr], writes=[junk2, ar])
            S.op("act", lambda e: e.activation(out=wg[:, 0:1], in_=ar[:, 0:1], func=AF.Gelu), reads=[ar], writes=[wg])
            stage[hk] = (gb, wg)
        def emit_acc(hk):
            gb, wg = stage.pop(hk)
            dgk = dgs.next()
            S.op("dve", lambda e: e.tensor_scalar(out=dgk[:, :], in0=identb[:, :], scalar1=wg[:, 0:1], scalar2=gg[:, hk:hk + 1], op0=ALU.mult, op1=ALU.mult), reads=[identb, wg, gg], writes=[dgk])
            for db in range(NDB):
                S.op("pe", lambda e: e.matmul(pacc[db][:, :], lhsT=dgk[:, :], rhs=gb[:, D + db * 512:D + (db + 1) * 512], start=(hk == 0), stop=(hk == 127)), reads=[dgk, gb], writes=[pacc[db]])
        for step in range(128 + LAG):
            if step < 128:
                emit_dot(step)
            if step - LAG >= 0:
                emit_acc(step - LAG)
            for _ in range(per):
                if nxt:
                    nxt.pop(0)()
        while nxt:
            nxt.pop(0)()
        for db in range(NDB):
            S.op("dve", lambda e: e.tensor_tensor(out=accA[:, db * 512:(db + 1) * 512], in0=xb[:, db * 512:(db + 1) * 512], in1=pacc[db][:, :], op=ALU.add), reads=[xb, pacc[db]], writes=[accA])
        rms_rstd(S, accA, D, junk, sq, rs)
        S.op("act", lambda e: e.activation(out=accA[:, :], in_=accA[:, :], func=AF.Copy, scale=rs[:, 0:1]), reads=[accA, rs], writes=[accA])
        S.op("dve", lambda e: e.tensor_tensor(out=accA[:, :], in0=accA[:, :], in1=gfb[:, :], op=ALU.mult), reads=[accA, gfb], writes=[accA])
        S.dma("sp", lambda e: e.dma_start(out=T.OUT[r0:r0 + 128, :], in_=accA[:, :]), reads=[accA])
        if tt + 1 < 8:
            ei = ei_n; gg = gg_n
    barrier(S)
    A.release(mk)

def convert_tables(S, T, n):
    st = T.conv_state
    while n > 0 and st[0] < len(st[1]):
        src, which, r = st[1][st[0]]
        S.dma("pool", lambda e: e.dma_start(out=T.PUV[r:r + 128, which, :], in_=src[r:r + 128, :]))
        st[0] += 1; n -= 1


def build_program(D=4096):
    c = make_cfg(D)
    nc = bass.Bass("TRN2", target_bir_lowering=False)
    T = Ctx()
    G = c.G
    ext_in = dict(xall=[2048, D], g_mix=[128, c.NC], w_in=[D, 2 * D],
                  abias=[c.H, 128, 4, 256], cbias=[128, c.H], eladd=[128, 8, 8], eligp=[128, 8, 8], ownm=[128, 8, 8],
                  lam_re=[64, G], lam_im=[64, G], logdt=[64, G], b_re=[64, G, 16], b_im=[64, G, 16], c_re=[64, G, 16], c_im=[64, G, 16],
                  d_bc=[128, c.Ds], mv24=[64, 24], nidx=[64, 256], cmask=[128, 128],
                  w_glu=[c.Ds, c.Ds], g_out=[128, c.NC], w_out=[D, D],
                  g_ffnT=[128, c.NC], g_ffn_bc=[128, D], w_q=[D, 2048], keysT=[128, 16, 128], peer_u=[16384, D], peer_v=[16384, D], g_fin_bc=[128, D], iota16=[128, 16])
    for k, s in ext_in.items():
        setattr(T, k, nc.dram_tensor(k, s, F32, kind="ExternalInput").ap())
    T.QT = nc.dram_tensor("QT", [c.H, 128, 1024], BF16, kind="Internal").ap()
    T.KT = nc.dram_tensor("KT", [c.H, 128, 2048], BF16, kind="Internal").ap()
    T.V = nc.dram_tensor("V", [2048, c.Da], BF16, kind="Internal").ap()
    T.U = nc.dram_tensor("U", [2048, c.Ds], F32, kind="Internal").ap()
    T.YATT = nc.dram_tensor("YATT", [1024, c.Da], F32, kind="Internal").ap()
    T.YSSM = nc.dram_tensor("YSSM", [1024, c.Ds], F32, kind="Internal").ap()
    T.X1 = nc.dram_tensor("X1", [1024, D], F32, kind="Internal").ap()
    T.HN2 = nc.dram_tensor("HN2", [1024, D], BF16, kind="Internal").ap()
    T.PUV = nc.dram_tensor("PUV", [16384, 2, D], BF16, kind="Internal").ap()
    T.SCD = nc.dram_tensor("SCD", [1024, 16, 128], F32, kind="Internal").ap()
    T.conv_state = [0, [(T.peer_u, 0, r) for r in range(0, 16384, 128)] + [(T.peer_v, 1, r) for r in range(0, 16384, 128)]]
    T.conv_per_step = 3
    T.conv_p1 = 2
    T.QPT = nc.dram_tensor("QPT", [16, 128, 1024], F32, kind="Internal").ap()
    T.OUT = nc.dram_tensor("OUT", [1024, D], F32, kind="ExternalOutput").ap()
    S = Sched(nc); A = Alloc(nc)
    for ph in (phase1, phase2, phase3, phase4, phase5):
        ph(nc, S, A, c, T)
    return nc, c


from concourse.bass_utils import run_bass_kernel_spmd

_PROG = {}

def _bc(v, n=128):
    return np.ascontiguousarray(np.broadcast_to(np.asarray(v, np.float32)[None, :], (n, v.shape[0])))

def _fm(v):
    v = np.asarray(v, np.float32)
    return np.ascontiguousarray(v.reshape(-1, 128).T)

def kernel(x, norm_mix_gain, w_in, rel_bias, ssm_lambda_re, ssm_lambda_im, ssm_log_dt,
           ssm_b_re, ssm_b_im, ssm_c_re, ssm_c_im, ssm_d, ssm_w_glu, attn_out_gain,
           ssm_out_gain, w_out, norm_ffn_gain, peer_w_q, peer_keys_a, peer_keys_b,
           peer_u, peer_v, norm_final_gain):
    f32 = lambda a: np.ascontiguousarray(np.asarray(a, dtype=np.float32))
    x = f32(x)
    B, SEQ, D = x.shape
    assert B == 4 and SEQ == 2048
    if D not in _PROG:
        _PROG[D] = build_program(D)
    nc, c = _PROG[D]
    l = 0
    shared = dict(
        g_mix=_fm(norm_mix_gain[l]), w_in=f32(w_in[l]),
        w_glu=f32(ssm_w_glu[l]),
        g_out=_fm(np.concatenate([np.asarray(attn_out_gain[l], np.float32), np.asarray(ssm_out_gain[l], np.float32)])),
        w_out=f32(w_out[l]),
        g_ffnT=_fm(norm_ffn_gain[l]), g_ffn_bc=_bc(np.asarray(norm_ffn_gain[l], np.float32)),
        w_q=f32(peer_w_q[l]), peer_u=f32(peer_u[l]), peer_v=f32(peer_v[l]),
        g_fin_bc=_bc(np.asarray(norm_final_gain, np.float32)),
        iota16=_bc(np.arange(16, dtype=np.float32)),
    )
    ka = np.asarray(peer_keys_a[l], np.float32); kb = np.asarray(peer_keys_b[l], np.float32)
    keysT = np.empty((128, 16, 128), np.float32)
    for h in range(8):
        keysT[:, 2 * h, :] = ka[h].T
        keysT[:, 2 * h + 1, :] = kb[h].T
    shared["keysT"] = keysT
    shared.update(host_ssm_tables(np.asarray(ssm_lambda_re[l], np.float32), np.asarray(ssm_lambda_im[l], np.float32),
                                  np.asarray(ssm_log_dt[l], np.float32), np.asarray(ssm_b_re[l], np.float32),
                                  np.asarray(ssm_b_im[l], np.float32), np.asarray(ssm_c_re[l], np.float32),
                                  np.asarray(ssm_c_im[l], np.float32), np.asarray(ssm_d[l], np.float32)))
    rb = np.asarray(rel_bias, np.float32)
    attn_tabs = [host_attn_tables(rb, half) for half in range(2)]
    in_maps = []
    for core in range(8):
        b, half = core // 2, core % 2
        xall = np.zeros((2048, D), np.float32)
        if half == 1:
            xall[:1024] = x[b, :1024]
        xall[1024:] = x[b, half * 1024:(half + 1) * 1024]
        ab, cbias, eladd, eligp, ownm = attn_tabs[half]
        m = dict(shared)
        m.update(xall=xall, abias=ab, cbias=cbias, eladd=eladd, eligp=eligp, ownm=ownm)
        in_maps.append(m)
    res = run_bass_kernel_spmd(nc, in_maps, core_ids=list(range(8)))
    out = np.empty((B, SEQ, D), np.float32)
    for core in range(8):
        b, half = core // 2, core % 2
        out[b, half * 1024:(half + 1) * 1024] = np.asarray(res.results[core]["OUT"], np.float32)
    return out
```

```python
import numpy as np
import concourse.bass as bass
import concourse.mybir as mybir
F32 = mybir.dt.float32
BF16 = mybir.dt.bfloat16
I32 = mybir.dt.int32
U32 = mybir.dt.uint32
AF = mybir.ActivationFunctionType
ALU = mybir.AluOpType
AX = mybir.AxisListType

class Buf:
    __slots__ = ("t", "w", "r", "name")
    def __init__(self, t, name=""):
        self.t = t; self.w = None; self.r = {}; self.name = name
    def __getitem__(self, k):
        return self.t[k]

class Sched:
    NDMA = 12
    def __init__(self, nc):
        self.nc = nc
        self.eng = {"pe": nc.tensor, "act": nc.scalar, "dve": nc.vector, "pool": nc.gpsimd, "sp": nc.sync}
        self.sem = {}
        self.cnt = {}
        self.waited = {k: {} for k in self.eng}
        self._ctx = []
        for k in self.eng:
            s = nc.semaphore("s_" + k); self.sem[k] = s.__enter__(); self._ctx.append(s); self.cnt[k] = 0
        self.dsem = {}; self.dcnt = {}
        self.q_engine = {"sp": "sp", "pool": "pool", "act": "act", "conv": "pool"}
        for q in ("sp", "pool", "act", "conv"):
            l = []
            for i in range(self.NDMA):
                s = nc.semaphore("d_%s%d" % (q, i)); l.append(s.__enter__()); self._ctx.append(s)
            self.dsem[q] = l; self.dcnt[q] = 0
        self.semid = {}
    def close(self):
        for s in reversed(self._ctx):
            s.__exit__(None, None, None)
    def _wait(self, e, ev):
        if ev is None: return
        sem, val, key = ev
        w = self.waited[e]
        if w.get(key, 0) >= val: return
        w[key] = val
        self.eng[e].wait_ge(sem, val)
    def _deps(self, e, reads, writes, skip_self=False):
        for b in reads:
            if b.w is not None and not (skip_self and b.w[2] == e):
                self._wait(e, b.w)
        for b in writes:
            if b.w is not None and not (skip_self and b.w[2] == e):
                self._wait(e, b.w)
            for ev in b.r.values():
                if not (skip_self and ev[2] == e):
                    self._wait(e, ev)
    def _record(self, ev, reads, writes):
        for b in writes:
            b.w = ev; b.r = {}
        for b in reads:
            b.r[ev[2]] = ev
    def op(self, e, fn, reads=(), writes=()):
        self._deps(e, reads, writes, skip_self=(e == "pe"))
        ins = fn(self.eng[e])
        self.cnt[e] += 1
        ins.then_inc(self.sem[e], 1)
        ev = (self.sem[e], self.cnt[e], e)
        self._record(ev, reads, writes)
        return ev
    def dma(self, q, fn, reads=(), writes=()):
        i = self.dcnt[q]; self.dcnt[q] += 1
        slot = i % self.NDMA; rnd = i // self.NDMA
        sem = self.dsem[q][slot]
        key = "d_%s%d" % (q, slot)
        q = self.q_engine[q]
        if rnd > 0:
            self._wait(q, (sem, 16 * rnd, key))
        self._deps(q, reads, writes)
        ins = fn(self.eng[q])
        ins.then_inc(sem, 16)
        ev = (sem, 16 * (rnd + 1), key)
        self._record(ev, reads, writes)
        return ev
    def wait_all(self, e, bufs):
        for b in bufs:
            self._wait(e, b.w)

import numpy as np

class Ctx:
    pass

def make_cfg(D=4096):
    c = Ctx()
    c.D = D; c.Da = D // 2; c.Ds = D // 2; c.H = c.Da // 128; c.G = c.Ds // 16
    c.NC = D // 128; c.TA = 2048; c.TO = 1024
    c.CB = min(512, c.Da)
    c.NE = 16384; c.PH = 8; c.QW = 2048
    return c

class Pool:
    def __init__(self, bufs): self.bufs = bufs; self.i = 0
    def next(self):
        b = self.bufs[self.i % len(self.bufs)]; self.i += 1; return b

class Alloc:
    def __init__(self, nc): self.nc = nc; self.stack = []; self.n = 0
    def sb(self, name, shape, dt):
        self.n += 1; c = self.nc.sbuf_tensor("sb%d_%s" % (self.n, name), shape, dt); t = c.__enter__(); self.stack.append(c); return Buf(t, name)
    def ps(self, name, shape, dt):
        self.n += 1; c = self.nc.psum_tensor("ps%d_%s" % (self.n, name), shape, dt); t = c.__enter__(); self.stack.append(c); return Buf(t, name)
    def mark(self): return len(self.stack)
    def release(self, mark):
        while len(self.stack) > mark:
            self.stack.pop().__exit__(None, None, None)

def barrier(S, include_conv=False):
    engs = ["pe", "act", "dve", "pool", "sp"]
    for e in engs:
        for o in engs:
            if o != e and S.cnt[o] > 0:
                S._wait(e, (S.sem[o], S.cnt[o], o))
        for q in S.dsem:
            if q == "conv" and not include_conv:
                continue
            for i in range(min(S.dcnt[q], S.NDMA)):
                n = (S.dcnt[q] - 1 - i) // S.NDMA + 1
                S._wait(e, (S.dsem[q][i], 16 * n, "d_%s%d" % (q, i)))

def build_identity(S, A, dt=F32, name="ident"):
    ident = A.sb(name, [128, 128], dt)
    S.op("pool", lambda e: e.memset(ident[:], 0.0), writes=[ident])
    S.op("pool", lambda e: e.affine_select(out=ident[:], in_=ident[:], pattern=[[-1, 128]], compare_op=ALU.not_equal, fill=1.0, base=0, channel_multiplier=1), reads=[ident], writes=[ident])
    return ident

def rms_rstd(S, src, W, junk, ssq, rstd, eps=1e-6):
    S.op("act", lambda e: e.activation(out=junk[:, 0:W], in_=src[:, 0:W], func=AF.Square, accum_out=ssq[:, 0:1]), reads=[src], writes=[junk, ssq])
    S.op("dve", lambda e: e.tensor_scalar(out=ssq[:, 0:1], in0=ssq[:, 0:1], scalar1=1.0 / W, scalar2=eps, op0=ALU.mult, op1=ALU.add), reads=[ssq], writes=[ssq])
    S.op("act", lambda e: e.activation(out=ssq[:, 0:1], in_=ssq[:, 0:1], func=AF.Sqrt), reads=[ssq], writes=[ssq])
    S.op("dve", lambda e: e.reciprocal(out=rstd[:, 0:1], in_=ssq[:, 0:1]), reads=[ssq], writes=[rstd])

def transpose_to(S, src, W, ident, ptpool, dst, dst_tok0, gainT, c0=0):
    nch = W // 128
    for cg in range(0, nch, 4):
        n = min(4, nch - cg)
        pt = ptpool.next()
        for j in range(n):
            c = cg + j
            S.op("pe", lambda e, c=c, j=j: e.transpose(out=pt[:, j * 128:(j + 1) * 128], in_=src[:, c * 128:(c + 1) * 128], identity=ident[:]), reads=[src, ident], writes=[pt])
        S.op("dve", lambda e, cg=cg, n=n: e.tensor_tensor(
            out=dst[:, c0 + cg:c0 + cg + n, dst_tok0:dst_tok0 + 128],
            in0=pt[:, 0:n * 128].rearrange("p (a b) -> p a b", a=n),
            in1=gainT[:, c0 + cg:c0 + cg + n].unsqueeze(2).to_broadcast([128, n, 128]), op=ALU.mult),
            reads=[pt, gainT], writes=[dst])

def phase1(nc, S, A, c, T):
    mk = A.mark()
    D, NC, CB = c.D, c.NC, c.CB
    ident = build_identity(S, A)
    gmix = A.sb("gmix", [128, NC], F32)
    S.dma("sp", lambda e: e.dma_start(out=gmix[:], in_=T.g_mix[:, :]), writes=[gmix])
    hnT = A.sb("hnT", [128, NC, 1024], BF16)
    xts = Pool([A.sb("xt%d" % i, [128, D], F32) for i in range(2)])
    junk = A.sb("junk", [128, D], BF16)
    ssq = Pool([A.sb("ssq%d" % i, [128, 1], F32) for i in range(2)])
    rstd = Pool([A.sb("rstd%d" % i, [128, 1], F32) for i in range(2)])
    wts = Pool([A.sb("wt%d" % i, [128, NC, CB], BF16) for i in range(2)])
    ptp = Pool([A.ps("pt%d" % i, [128, 512], F32) for i in range(2)])
    pmp = Pool([A.ps("pm%d" % i, [128, 512], F32) for i in range(4)])
    stq = Pool([A.sb("stq%d" % i, [128, 1024], BF16) for i in range(2)])
    stv = Pool([A.sb("stv%d" % i, [128, CB], BF16) for i in range(2)])
    stu = Pool([A.sb("stu%d" % i, [128, CB], F32) for i in range(2)])
    nqb = c.Da // CB
    hpb = CB // 128
    w_view = T.w_in.rearrange("(c p) n -> p c n", p=128)
    for blk in range(2):
        for tt in range(8):
            xt = xts.next(); sq = ssq.next(); rs = rstd.next()
            r0 = blk * 1024 + tt * 128
            S.dma("sp", lambda e: e.dma_start(out=xt[:], in_=T.xall[r0:r0 + 128, :]), writes=[xt])
            rms_rstd(S, xt, D, junk, sq, rs)
            S.op("act", lambda e: e.activation(out=xt[:], in_=xt[:], func=AF.Copy, scale=rs[:, 0:1]), reads=[xt, rs], writes=[xt])
            transpose_to(S, xt, D, ident, ptp, hnT, tt * 128, gmix)
        for kind in range(4):
            if kind == 0 and blk == 0:
                continue
            for b in range(nqb):
                col0 = kind * c.Da + b * CB
                wt = wts.next()
                S.dma("pool", lambda e: e.dma_start(out=wt[:], in_=w_view[:, :, col0:col0 + CB]), writes=[wt])
                convert_tables(S, T, getattr(T, "conv_p1", 0))
                if kind < 2:
                    for j in range(hpb):
                        h = b * hpb + j
                        st = stq.next()
                        for half in range(2):
                            pm = pmp.next()
                            for cc in range(NC):
                                S.op("pe", lambda e, cc=cc: e.matmul(pm[:, :], lhsT=wt[:, cc, j * 128:(j + 1) * 128], rhs=hnT[:, cc, half * 512:(half + 1) * 512], start=(cc == 0), stop=(cc == NC - 1)), reads=[wt, hnT], writes=[pm])
                            eng = "act" if half == 0 else "dve"
                            if eng == "act":
                                S.op("act", lambda e: e.activation(out=st[:, half * 512:(half + 1) * 512], in_=pm[:, :], func=AF.Copy), reads=[pm], writes=[st])
                            else:
                                S.op("dve", lambda e: e.tensor_copy(out=st[:, half * 512:(half + 1) * 512], in_=pm[:, :]), reads=[pm], writes=[st])
                        if kind == 0:
                            S.dma("sp", lambda e: e.dma_start(out=T.QT[h, :, :], in_=st[:, :]), reads=[st])
                        else:
                            S.dma("sp", lambda e: e.dma_start(out=T.KT[h, :, blk * 1024:(blk + 1) * 1024], in_=st[:, :]), reads=[st])
                else:
                    for tt in range(8):
                        pm = pmp.next()
                        for cc in range(NC):
                            S.op("pe", lambda e, cc=cc: e.matmul(pm[:, 0:CB], lhsT=hnT[:, cc, tt * 128:(tt + 1) * 128], rhs=wt[:, cc, :], start=(cc == 0), stop=(cc == NC - 1)), reads=[wt, hnT], writes=[pm])
                        r0 = blk * 1024 + tt * 128
                        cl = b * CB
                        if kind == 2:
                            st = stv.next()
                            S.op("act", lambda e: e.activation(out=st[:, :], in_=pm[:, 0:CB], func=AF.Copy), reads=[pm], writes=[st])
                            S.dma("sp", lambda e: e.dma_start(out=T.V[r0:r0 + 128, cl:cl + CB], in_=st[:, :]), reads=[st])
                        else:
                            st = stu.next()
                            S.op("dve", lambda e: e.tensor_copy(out=st[:, :], in_=pm[:, 0:CB]), reads=[pm], writes=[st])
                            S.dma("sp", lambda e: e.dma_start(out=T.U[r0:r0 + 128, cl:cl + CB], in_=st[:, :]), reads=[st])
    barrier(S)
    A.release(mk)

def phase2(nc, S, A, c, T):
    mk = A.mark()
    H = c.H
    scale = 128 ** -0.5
    identb = build_identity(S, A, BF16, "identb")
    cb = A.sb("cbias", [128, H], F32)
    eladd = A.sb("eladd", [128, 8, 8], F32); eligp = A.sb("eligp", [128, 8, 8], F32); ownm = A.sb("ownm", [128, 8, 8], F32)
    S.dma("sp", lambda e: e.dma_start(out=cb[:], in_=T.cbias[:, :]), writes=[cb])
    S.dma("sp", lambda e: e.dma_start(out=eladd[:], in_=T.eladd[:, :, :]), writes=[eladd])
    S.dma("sp", lambda e: e.dma_start(out=eligp[:], in_=T.eligp[:, :, :]), writes=[eligp])
    S.dma("sp", lambda e: e.dma_start(out=ownm[:], in_=T.ownm[:, :, :]), writes=[ownm])
    qTs = Pool([A.sb("qT%d" % i, [128, 1024], BF16) for i in range(2)])
    kTs = Pool([A.sb("kT%d" % i, [128, 2048], BF16) for i in range(2)])
    vhs = Pool([A.sb("vh%d" % i, [128, 16, 136], BF16) for i in range(2)])
    for vb_ in vhs.bufs:
        S.op("pool", lambda e: e.memset(vb_[:, :, 128:136], 1.0), writes=[vb_])
    bts = Pool([A.sb("bt%d" % i, [128, 4, 256], F32) for i in range(2)])
    km = A.sb("km", [128, 8], F32); kmb = A.sb("kmb", [128, 8], F32); qf = A.sb("qf", [128, 1024], F32)
    pss = [A.ps("pss%d" % i, [128, 512], F32) for i in range(4)]
    bankA = A.ps("bankA", [128, 512], F32); bankB = A.ps("bankB", [128, 512], F32)
    pg = Buf(bankA[:, 0:8], "pg")
    ptb = Pool([A.ps("ptb%d" % i, [128, 512], BF16) for i in range(2)])
    pop = Pool([Buf(bankA[:, 128:264], "po0"), Buf(bankB[:, 128:264], "po1")])
    gm = A.sb("gm", [128, 8], F32); t8 = A.sb("t8", [128, 8], F32); m1 = A.sb("m1", [128, 8], F32)
    selb = A.sb("selb", [128, 8], F32); selc = A.sb("selc", [128, 8], F32)
    lgs = Pool([A.sb("lg%d" % i, [128, 256], F32) for i in range(2)])
    pexps = Pool([A.sb("pexp%d" % i, [128, 2048], BF16) for i in range(2)])
    rss = Pool([A.sb("rs%d" % i, [128, 8], F32) for i in range(2)])
    rsum = A.sb("rsum", [128, 1], F32); rinv = A.sb("rinv", [128, 1], F32)
    pTs = Pool([A.sb("pT%d" % i, [128, 4, 128], BF16) for i in range(4)])
    ysts = Pool([A.sb("yst%d" % i, [128, 128], F32) for i in range(2)])
    Vv = T.V.rearrange("(kt p) d -> p kt d", p=128)
    heads = {}
    def load_head(h):
        qT = qTs.next(); kT = kTs.next(); vh = vhs.next(); bt = bts.next()
        S.dma("sp", lambda e: e.dma_start(out=qT[:], in_=T.QT[h, :, :]), writes=[qT])
        S.dma("sp", lambda e: e.dma_start(out=kT[:], in_=T.KT[h, :, :]), writes=[kT])
        S.dma("sp", lambda e: e.dma_start(out=vh[:, :, 0:128], in_=Vv[:, :, h * 128:(h + 1) * 128]), writes=[vh])
        S.dma("sp", lambda e: e.dma_start(out=bt[:], in_=T.abias[h, :, :, :]), writes=[bt])
        heads[h] = (qT, kT, vh, bt)
    def stage_a(h, qt):
        if qt == 0:
            if h not in heads:
                load_head(h)
            qT, kT, vh, bt = heads[h]
            S.op("dve", lambda e: e.tensor_reduce(out=km[:, :], in_=kT[:, :].rearrange("p (n k) -> p n k", k=256), axis=AX.X, op=ALU.add), reads=[kT], writes=[km])
            S.op("dve", lambda e: e.tensor_scalar(out=kmb[:, :], in0=km[:, :], scalar1=1.0 / 256, scalar2=None, op0=ALU.mult), reads=[km], writes=[kmb])
            S.op("act", lambda e: e.activation(out=qf[:, :], in_=qT[:, :], func=AF.Copy), reads=[qT], writes=[qf])
        if qt == 1 and h + 1 < H:
            load_head(h + 1)
        qT, kT, vh, bt = heads[h]
        nown = 4 + qt // 2
        nblk = nown + 1
        qs = qT[:, qt * 128:(qt + 1) * 128]
        convert_tables(S, T, T.conv_per_step)
        S.op("pe", lambda e: e.matmul(pg[:, :], lhsT=qf[:, qt * 128:(qt + 1) * 128], rhs=kmb[:, :], start=True, stop=True), reads=[qf, kmb], writes=[pg])
        S.op("dve", lambda e: e.tensor_tensor(out=gm[:, :], in0=pg[:, :], in1=eladd[:, qt, :], op=ALU.add), reads=[pg, eladd], writes=[gm])
        S.op("dve", lambda e: e.max(out=t8[:, :], in_=gm[:, :]), reads=[gm], writes=[t8])
        S.op("dve", lambda e: e.tensor_scalar(out=m1[:, :], in0=gm[:, :], scalar1=t8[:, 2:3], scalar2=None, op0=ALU.is_ge), reads=[gm, t8], writes=[m1])
        S.op("dve", lambda e: e.tensor_tensor(out=m1[:, :], in0=m1[:, :], in1=eligp[:, qt, :], op=ALU.mult), reads=[m1, eligp], writes=[m1])
        S.op("dve", lambda e: e.tensor_tensor(out=m1[:, :], in0=m1[:, :], in1=ownm[:, qt, :], op=ALU.add), reads=[m1, ownm], writes=[m1])
        S.op("dve", lambda e: e.tensor_scalar(out=selb[:, :], in0=m1[:, :], scalar1=1.0, scalar2=1e30, op0=ALU.subtract, op1=ALU.mult), reads=[m1], writes=[selb])
        S.op("dve", lambda e: e.tensor_scalar(out=selc[:, :], in0=selb[:, :], scalar1=cb[:, h:h + 1], scalar2=None, op0=ALU.add), reads=[selb, cb], writes=[selc])
        def bank_of(n):
            if n >= nown - 1:
                return pss[3], (n - (nown - 1)) * 256
            return pss[n // 2], (n % 2) * 256
        for n in range(nblk):
            ps, po_ = bank_of(n)
            S.op("pe", lambda e: e.matmul(ps[:, po_:po_ + 256], lhsT=qs, rhs=kT[:, n * 256:(n + 1) * 256], start=True, stop=True), reads=[qT, kT], writes=[ps])
        pexp = pexps.next()
        for n in range(nblk):
            ps, po_ = bank_of(n)
            pslice = ps[:, po_:po_ + 256]
            if n >= nown - 1:
                v = (0 if n == nown else 2) + (qt % 2)
                lg = lgs.next()
                S.op("dve", lambda e: e.scalar_tensor_tensor(out=lg[:, :], in0=pslice, scalar=scale, in1=bt[:, v, :], op0=ALU.mult, op1=ALU.add), reads=[ps, bt], writes=[lg])
                S.op("act", lambda e: e.activation(out=pexp[:, n * 256:(n + 1) * 256], in_=lg[:, :], func=AF.Exp, bias=selb[:, n:n + 1], scale=1.0), reads=[lg, selb], writes=[pexp])
            else:
                S.op("act", lambda e: e.activation(out=pexp[:, n * 256:(n + 1) * 256], in_=pslice, func=AF.Exp, bias=selc[:, n:n + 1], scale=scale), reads=[ps, selc], writes=[pexp])
        return pexp, nblk
    def stage_b(h, qt, pexp, nblk):
        qT, kT, vh, bt = heads[h]
        po = pop.next()
        nkt = 2 * nblk
        groups = list(range(0, nkt, 4))
        tiles = {}
        def emit_t(gi):
            k0 = groups[gi]
            pt = ptb.next(); pT = pTs.next()
            nj = min(4, nkt - k0)
            for j in range(nj):
                kt = k0 + j
                S.op("pe", lambda e: e.transpose(out=pt[:, j * 128:(j + 1) * 128], in_=pexp[:, kt * 128:(kt + 1) * 128], identity=identb[:]), reads=[pexp, identb], writes=[pt])
            if gi % 2 == 0:
                S.op("act", lambda e: e.activation(out=pT[:, 0:nj, :], in_=pt[:, 0:nj * 128].rearrange("p (a b) -> p a b", a=nj), func=AF.Copy), reads=[pt], writes=[pT])
            else:
                S.op("dve", lambda e: e.tensor_copy(out=pT[:, 0:nj, :], in_=pt[:, 0:nj * 128].rearrange("p (a b) -> p a b", a=nj)), reads=[pt], writes=[pT])
            tiles[gi] = (pT, nj, k0)
        def emit_pv(gi):
            pT, nj, k0 = tiles.pop(gi)
            for j in range(nj):
                kt = k0 + j
                S.op("pe", lambda e: e.matmul(po[:, 0:129], lhsT=pT[:, j, :], rhs=vh[:, kt, 0:129], start=(kt == 0), stop=(kt == nkt - 1)), reads=[pT, vh], writes=[po])
        emit_t(0)
        for gi in range(len(groups)):
            if gi + 1 < len(groups):
                emit_t(gi + 1)
            emit_pv(gi)
        yst = ysts.next()
        S.op("dve", lambda e: e.reciprocal(out=rinv[:, :], in_=po[:, 128:129]), reads=[po], writes=[rinv])
        S.op("dve", lambda e: e.tensor_scalar(out=yst[:, :], in0=po[:, 0:128], scalar1=rinv[:, 0:1], scalar2=None, op0=ALU.mult), reads=[po, rinv], writes=[yst])
        S.dma("sp", lambda e: e.dma_start(out=T.YATT[qt * 128:(qt + 1) * 128, h * 128:(h + 1) * 128], in_=yst[:, :]), reads=[yst])
    units = [(h, qt) for h in range(H) for qt in range(8)]
    prev = None
    for (h, qt) in units:
        cur = stage_a(h, qt)
        if prev is not None:
            stage_b(*prev)
        prev = (h, qt) + cur
    stage_b(*prev)
    barrier(S)
    A.release(mk)

def t5_bucket_np(n):
    import math
    n = np.maximum(n, 0)
    r = np.log(np.maximum(n, 1).astype(np.float32) / np.float32(16)) / np.float32(math.log(128 / 16))
    large = 16 + (r.astype(np.float32) * np.float32(16)).astype(np.int32)
    large = np.minimum(large, 31)
    return np.where(n < 16, n, large)

def host_attn_tables(rel_bias, half):
    H = rel_bias.shape[1]
    i = np.arange(128)[:, None]; j = np.arange(256)[None, :]
    ab = np.empty((H, 128, 4, 256), np.float32)
    for v in range(4):
        qoff = (v % 2) * 128
        dist = qoff + i - j + (256 if v >= 2 else 0)
        bk = t5_bucket_np(dist)
        btl = rel_bias[bk, :]
        btl = np.where((dist >= 0)[:, :, None], btl, np.float32(-1e30))
        ab[:, :, v, :] = btl.transpose(2, 0, 1)
    cbias = np.ascontiguousarray(np.broadcast_to(rel_bias[31, :][None, :], (128, H))).astype(np.float32)
    eligp = np.zeros((8, 8), np.float32); ownm = np.zeros((8, 8), np.float32)
    for qt in range(8):
        own = 4 + qt // 2
        ownm[qt, own] = 1.0
        lo = 0 if half == 1 else 4
        eligp[qt, lo:own] = 1.0
    eladd = ((eligp - 1.0) * np.float32(1e30)).astype(np.float32)
    bc = lambda a: np.ascontiguousarray(np.broadcast_to(a[None], (128, 8, 8))).astype(np.float32)
    return ab, cbias, bc(eladd), bc(eligp), bc(ownm)

TWO_PI = 6.283185307179586
MAGIC = 12582912.0

def sincos(S, ang, shape, tmpa, tmpb, out_sin, out_cos, defer_act=False):
    a, ta, tb, osn, ocs = ang, tmpa, tmpb, out_sin, out_cos
    S.op("dve", lambda e: e.tensor_scalar(out=ta[1](ta[0]), in0=a[1](a[0]), scalar1=1.0 / TWO_PI, scalar2=MAGIC, op0=ALU.mult, op1=ALU.add), reads=[a[0]], writes=[ta[0]])
    S.op("dve", lambda e: e.tensor_scalar(out=ta[1](ta[0]), in0=ta[1](ta[0]), scalar1=MAGIC, scalar2=None, op0=ALU.subtract), reads=[ta[0]], writes=[ta[0]])
    S.op("dve", lambda e: e.scalar_tensor_tensor(out=tb[1](tb[0]), in0=ta[1](ta[0]), scalar=-TWO_PI, in1=a[1](a[0]), op0=ALU.mult, op1=ALU.add), reads=[ta[0], a[0]], writes=[tb[0]])
    S.op("dve", lambda e: e.tensor_scalar(out=tb[1](tb[0]), in0=tb[1](tb[0]), scalar1=-3.1415925, scalar2=3.1415925, op0=ALU.max, op1=ALU.min), reads=[tb[0]], writes=[tb[0]])
    late = []
    sin1 = lambda: S.op("act", lambda e: e.activation(out=osn[1](osn[0]), in_=tb[1](tb[0]), func=AF.Sin), reads=[tb[0]], writes=[osn[0]])
    if defer_act:
        late.append(sin1)
    else:
        sin1()
    S.op("dve", lambda e: e.tensor_scalar(out=ta[1](ta[0]), in0=tb[1](tb[0]), scalar1=1.5707963, scalar2=-TWO_PI, op0=ALU.is_gt, op1=ALU.mult), reads=[tb[0]], writes=[ta[0]])
    S.op("dve", lambda e: e.scalar_tensor_tensor(out=ta[1](ta[0]), in0=tb[1](tb[0]), scalar=1.5707963, in1=ta[1](ta[0]), op0=ALU.add, op1=ALU.add), reads=[tb[0], ta[0]], writes=[ta[0]])
    S.op("dve", lambda e: e.tensor_scalar(out=ta[1](ta[0]), in0=ta[1](ta[0]), scalar1=-3.1415925, scalar2=3.1415925, op0=ALU.max, op1=ALU.min), reads=[ta[0]], writes=[ta[0]])
    sin2 = lambda: S.op("act", lambda e: e.activation(out=ocs[1](ocs[0]), in_=ta[1](ta[0]), func=AF.Sin), reads=[ta[0]], writes=[ocs[0]])
    if defer_act:
        late.append(sin2)
    else:
        sin2()
    return late

def phase3(nc, S, A, c, T):
    mk = A.mark()
    G = c.G
    full = lambda b: b[:]
    ident = build_identity(S, A)
    def load(name, shape, src):
        b = A.sb(name, shape, F32)
        S.dma("sp", lambda e: e.dma_start(out=b[:], in_=src), writes=[b])
        return b
    sh3 = [64, G, 24]; sB = [64, G, 16]
    Are = A.sb("Are", sh3, F32); Aim = A.sb("Aim", sh3, F32)
    Bre = A.sb("Bre", sB, F32); Bim = A.sb("Bim", sB, F32)
    th8 = A.sb("th8", [64, G], F32); r8 = A.sb("r8", [64, G], F32)
    cre = load("cre", [64, G, 16], T.c_re[:, :, :]); cim = load("cim", [64, G, 16], T.c_im[:, :, :])
    dbcs = Pool([A.sb("dbc%d" % i, [128, 128], F32) for i in range(2)])
    nidx = load("nidx", [64, 256], T.nidx[:, :]); cmask = load("cmask", [128, 128], T.cmask[:, :])
    mk_tmp = A.mark()
    lre = load("lre", [64, G], T.lam_re[:, :]); lim = load("lim", [64, G], T.lam_im[:, :]); ldt = load("ldt", [64, G], T.logdt[:, :])
    bre = load("bre", [64, G, 16], T.b_re[:, :, :]); bim = load("bim", [64, G, 16], T.b_im[:, :, :])
    mv = load("mv", [64, 24], T.mv24[:, :])
    dt = A.sb("dt", [64, G], F32); lrd = A.sb("lrd", [64, G], F32); lid = A.sb("lid", [64, G], F32)
    S.op("act", lambda e: e.activation(out=dt[:], in_=ldt[:], func=AF.Exp), reads=[ldt], writes=[dt])
    S.op("dve", lambda e: e.tensor_tensor(out=lrd[:], in0=lre[:], in1=dt[:], op=ALU.mult), reads=[lre, dt], writes=[lrd])
    S.op("dve", lambda e: e.tensor_tensor(out=lid[:], in0=lim[:], in1=dt[:], op=ALU.mult), reads=[lim, dt], writes=[lid])
    arg = A.sb("arg", sh3, F32); mag = A.sb("mag", sh3, F32); ang = A.sb("ang", sh3, F32)
    ta = A.sb("ta", sh3, F32); tb = A.sb("tb", sh3, F32)
    mvb = mv[:, :].unsqueeze(1).to_broadcast(sh3)
    S.op("dve", lambda e: e.tensor_tensor(out=arg[:], in0=lrd[:, :].unsqueeze(2).to_broadcast(sh3), in1=mvb, op=ALU.mult), reads=[lrd, mv], writes=[arg])
    S.op("act", lambda e: e.activation(out=mag[:], in_=arg[:], func=AF.Exp), reads=[arg], writes=[mag])
    S.op("dve", lambda e: e.tensor_tensor(out=ang[:], in0=lid[:, :].unsqueeze(2).to_broadcast(sh3), in1=mvb, op=ALU.mult), reads=[lid, mv], writes=[ang])
    sincos(S, (ang, full), sh3, (ta, full), (tb, full), (Aim, full), (Are, full))
    S.op("dve", lambda e: e.tensor_copy(out=th8[:], in_=tb[:, :, 23]), reads=[tb], writes=[th8])
    S.op("dve", lambda e: e.tensor_copy(out=r8[:], in_=mag[:, :, 23]), reads=[mag], writes=[r8])
    S.op("dve", lambda e: e.tensor_tensor(out=Are[:], in0=Are[:], in1=mag[:], op=ALU.mult), reads=[Are, mag], writes=[Are])
    S.op("dve", lambda e: e.tensor_tensor(out=Aim[:], in0=Aim[:], in1=mag[:], op=ALU.mult), reads=[Aim, mag], writes=[Aim])
    nr = A.sb("nr", [64, G], F32); t1 = A.sb("t1", [64, G], F32); t2 = A.sb("t2", [64, G], F32); den = A.sb("den", [64, G], F32)
    cfr = A.sb("cfr", [64, G], F32); cfi = A.sb("cfi", [64, G], F32)
    S.op("dve", lambda e: e.tensor_scalar(out=nr[:], in0=Are[:, :, 16], scalar1=-1.0, scalar2=None, op0=ALU.add), reads=[Are], writes=[nr])
    S.op("dve", lambda e: e.tensor_tensor(out=t1[:], in0=lre[:], in1=lre[:], op=ALU.mult), reads=[lre], writes=[t1])
    S.op("dve", lambda e: e.tensor_tensor(out=t2[:], in0=lim[:], in1=lim[:], op=ALU.mult), reads=[lim], writes=[t2])
    S.op("dve", lambda e: e.tensor_tensor(out=den[:], in0=t1[:], in1=t2[:], op=ALU.add), reads=[t1, t2], writes=[den])
    S.op("dve", lambda e: e.reciprocal(out=den[:], in_=den[:]), reads=[den], writes=[den])
    S.op("dve", lambda e: e.tensor_tensor(out=t1[:], in0=nr[:], in1=lre[:], op=ALU.mult), reads=[nr, lre], writes=[t1])
    S.op("dve", lambda e: e.tensor_tensor(out=t2[:], in0=Aim[:, :, 16], in1=lim[:], op=ALU.mult), reads=[Aim, lim], writes=[t2])
    S.op("dve", lambda e: e.tensor_tensor(out=t1[:], in0=t1[:], in1=t2[:], op=ALU.add), reads=[t1, t2], writes=[t1])
    S.op("dve", lambda e: e.tensor_tensor(out=cfr[:], in0=t1[:], in1=den[:], op=ALU.mult), reads=[t1, den], writes=[cfr])
    S.op("dve", lambda e: e.tensor_tensor(out=t1[:], in0=Aim[:, :, 16], in1=lre[:], op=ALU.mult), reads=[Aim, lre], writes=[t1])
    S.op("dve", lambda e: e.tensor_tensor(out=t2[:], in0=nr[:], in1=lim[:], op=ALU.mult), reads=[nr, lim], writes=[t2])
    S.op("dve", lambda e: e.tensor_tensor(out=t1[:], in0=t1[:], in1=t2[:], op=ALU.subtract), reads=[t1, t2], writes=[t1])
    S.op("dve", lambda e: e.tensor_tensor(out=cfi[:], in0=t1[:], in1=den[:], op=ALU.mult), reads=[t1, den], writes=[cfi])
    tB1 = A.sb("tB1", sB, F32); tB2 = A.sb("tB2", sB, F32)
    cfrb = cfr[:, :].unsqueeze(2).to_broadcast(sB); cfib = cfi[:, :].unsqueeze(2).to_broadcast(sB)
    S.op("dve", lambda e: e.tensor_tensor(out=tB1[:], in0=bre[:], in1=cfrb, op=ALU.mult), reads=[bre, cfr], writes=[tB1])
    S.op("dve", lambda e: e.tensor_tensor(out=tB2[:], in0=bim[:], in1=cfib, op=ALU.mult), reads=[bim, cfi], writes=[tB2])
    S.op("dve", lambda e: e.tensor_tensor(out=Bre[:], in0=tB1[:], in1=tB2[:], op=ALU.subtract), reads=[tB1, tB2], writes=[Bre])
    S.op("dve", lambda e: e.tensor_tensor(out=tB1[:], in0=bim[:], in1=cfrb, op=ALU.mult), reads=[bim, cfr], writes=[tB1])
    S.op("dve", lambda e: e.tensor_tensor(out=tB2[:], in0=bre[:], in1=cfib, op=ALU.mult), reads=[bre, cfi], writes=[tB2])
    S.op("dve", lambda e: e.tensor_tensor(out=Bim[:], in0=tB1[:], in1=tB2[:], op=ALU.add), reads=[tB1, tB2], writes=[Bim])
    barrier(S)
    A.release(mk_tmp)
    s4 = [64, 8, 8, 16]
    s4f = [64, 8, 128]
    NTre = A.sb("NTre", s4f, F32); NTim = A.sb("NTim", s4f, F32); NPre = A.sb("NPre", s4f, F32); NPim = A.sb("NPim", s4f, F32)
    Rs = [(A.sb("Rre%d" % i, s4f, F32), A.sb("Rim%d" % i, s4f, F32)) for i in range(2)]
    q1 = A.sb("q1", s4, F32); q2 = A.sb("q2", s4, F32)
    v4 = lambda b: b[:].rearrange("p g (s c) -> p g s c", s=8)
    Utm2 = A.sb("Utm2", [128, 2, 8, 128], F32)
    Utms = Pool([A.sb("Utm%d" % i, [128, 2, 8, 128], F32) for i in range(1)])
    Ytm = A.sb("Ytm", [128, 8, 128], F32); Ytmp = A.sb("Ytmp", [128, 8, 128], F32)
    Ug = A.sb("Ug", [128, 8, 256], F32); Mg = A.sb("Mg", [128, 8, 128], F32)
    Ngs = Pool([A.sb("Ng%d" % i, [128, 128], F32) for i in range(2)])
    s3 = [64, 8, 256]
    Xre = A.sb("Xre", s3, F32); Xim = A.sb("Xim", s3, F32); Ec = A.sb("Ec", s3, F32); Es = A.sb("Es", s3, F32)
    w1 = A.sb("w1", s3, F32); w2 = A.sb("w2", s3, F32); Gre = A.sb("Gre", s3, F32); Gim = A.sb("Gim", s3, F32)
    Sre = Xre; Sim = Xim
    Ygs = Pool([A.sb("Yg%d" % i, [128, 128], F32) for i in range(2)])
    def bank(name, p, w):
        b = A.ps(name, [128, 512], F32)
        return Buf(b[0:p, 0:w], name)
    ptU = Pool([bank("ptU%d" % i, 128, 256) for i in range(2)])
    pN = bank("pN", 128, 128); px = bank("px", 64, 512); pmgs = [bank("pmgA", 128, 512), bank("pmgB", 128, 512)]
    py = bank("py", 128, 128); ptY = bank("ptY", 128, 128)
    Uv = T.U.rearrange("(b n s) ch -> n b s ch", b=2, n=128, s=8)
    Yv = T.YSSM.rearrange("(n t) ch -> n t ch", t=8)
    def cmul(outre, outim, are, aim, xre, xim, rd, neg_im=False):
        S.op("dve", lambda e: e.tensor_tensor(out=q1[:], in0=are, in1=xre, op=ALU.mult), reads=rd, writes=[q1])
        S.op("dve", lambda e: e.tensor_tensor(out=q2[:], in0=aim, in1=xim, op=ALU.mult), reads=rd, writes=[q2])
        S.op("dve", lambda e: e.tensor_tensor(out=v4(outre), in0=q1[:], in1=q2[:], op=ALU.subtract), reads=[q1, q2], writes=[outre])
        S.op("dve", lambda e: e.tensor_tensor(out=q1[:], in0=are, in1=xim, op=ALU.mult), reads=rd, writes=[q1])
        S.op("dve", lambda e: e.tensor_tensor(out=q2[:], in0=aim, in1=xre, op=ALU.mult), reads=rd, writes=[q2])
        if neg_im:
            S.op("dve", lambda e: e.scalar_tensor_tensor(out=v4(outim), in0=q1[:], scalar=-1.0, in1=q2[:], op0=ALU.mult, op1=ALU.subtract), reads=[q1, q2], writes=[outim])
        else:
            S.op("dve", lambda e: e.tensor_tensor(out=v4(outim), in0=q1[:], in1=q2[:], op=ALU.add), reads=[q1, q2], writes=[outim])
    stop = getattr(T, "stop", 0)
    NB = G // 8
    def apw(tab, g0, m0):
        return tab[:, g0:g0 + 8, m0:m0 + 8].unsqueeze(3).to_broadcast(s4)
    def apv(tab, g0):
        return tab[:, g0:g0 + 8, :].unsqueeze(2).to_broadcast(s4)
    def emit_cmul(gb):
        g0 = gb * 8
        Rre, Rim = Rs[gb % 2]
        cmul(NTre, NTim, apw(Are, g0, 0), apw(Aim, g0, 0), apv(Bre, g0), apv(Bim, g0), [Are, Aim, Bre, Bim])
        cmul(NPre, NPim, apw(Are, g0, 8), apw(Aim, g0, 8), apv(Bre, g0), apv(Bim, g0), [Are, Aim, Bre, Bim])
        cmul(Rre, Rim, apw(Are, g0, 16), apw(Aim, g0, 16), apv(cre, g0), apv(cim, g0), [Are, Aim, cre, cim], neg_im=True)
    emit_cmul(0)
    def load_u(gb_):
        ch_ = gb_ * 128
        Utm_ = Utms.next(); dbc_ = dbcs.next()
        for blk_ in range(2):
            S.dma("sp", lambda e: e.dma_start(out=Utm_[:, blk_, :, :], in_=Uv[:, blk_, :, ch_:ch_ + 128]), writes=[Utm_])
        S.dma("sp", lambda e: e.dma_start(out=dbc_[:], in_=T.d_bc[:, ch_:ch_ + 128]), writes=[dbc_])
        return Utm_, dbc_
    nxt_u = load_u(0)
    for gb in range(NB):
        g0 = gb * 8; ch0 = gb * 128
        Rre, Rim = Rs[gb % 2]
        Utm, dbc = nxt_u
        for blk in range(2):
            S.op("dve", lambda e: e.tensor_copy(out=Utm2[:, blk, :, :].rearrange("p j (s c) -> p j s c", s=8), in_=Utm[:, blk, :, :].rearrange("p s (j c) -> p j s c", j=8)), reads=[Utm], writes=[Utm2])
        if gb + 1 < NB:
            nxt_u = load_u(gb + 1)
        convert_tables(S, T, getattr(T, "conv_p3", 0))
        th = th8[:, g0:g0 + 8].unsqueeze(2).to_broadcast(s3)
        S.op("dve", lambda e: e.tensor_tensor(out=w1[:], in0=th, in1=nidx[:, :].unsqueeze(1).to_broadcast(s3), op=ALU.mult), reads=[th8, nidx], writes=[w1])
        late = sincos(S, (w1, full), s3, (w2, full), (Gre, full), (Es, full), (Ec, full), defer_act=True)
        for j in range(8):
            g = g0 + j
            pt = ptU.next()
            for blk in range(2):
                S.op("pe", lambda e: e.transpose(out=pt[:, blk * 128:(blk + 1) * 128], in_=Utm2[:, blk, j, :], identity=ident[:]), reads=[Utm2, ident], writes=[pt])
            S.op("act", lambda e: e.activation(out=Ug[:, j, :], in_=pt[:, :], func=AF.Copy), reads=[pt], writes=[Ug])
            S.op("pe", lambda e: e.transpose(out=pN[:, 0:64], in_=NTre[:, j, :], identity=ident[0:64, 0:64]), reads=[NTre, ident], writes=[pN])
            S.op("pe", lambda e: e.transpose(out=pN[:, 64:128], in_=NTim[:, j, :], identity=ident[0:64, 0:64]), reads=[NTim, ident], writes=[pN])
            Ng = Ngs.next()
            S.op("act", lambda e: e.activation(out=Ng[:, :], in_=pN[:, :], func=AF.Copy), reads=[pN], writes=[Ng])
            S.op("pe", lambda e: e.matmul(px[:, 0:256], lhsT=Ng[:, 0:64], rhs=Ug[:, j, :], start=True, stop=True), reads=[Ng, Ug], writes=[px])
            S.op("pe", lambda e: e.matmul(px[:, 256:512], lhsT=Ng[:, 64:128], rhs=Ug[:, j, :], start=True, stop=True), reads=[Ng, Ug], writes=[px])
            S.op("act", lambda e: e.activation(out=Xre[:, j, :], in_=px[:, 0:256], func=AF.Copy), reads=[px], writes=[Xre])
            S.op("act", lambda e: e.activation(out=Xim[:, j, :], in_=px[:, 256:512], func=AF.Copy), reads=[px], writes=[Xim])
            pm_ = pmgs[j // 4]; c0_ = (j % 4) * 128
            S.op("pe", lambda e: e.matmul(pm_[:, c0_:c0_ + 128], lhsT=NPre[:, j, :], rhs=Rre[:, j, :], start=True, stop=False), reads=[NPre, Rre], writes=[pm_])
            S.op("pe", lambda e: e.matmul(pm_[:, c0_:c0_ + 128], lhsT=NPim[:, j, :], rhs=Rim[:, j, :], start=False, stop=True), reads=[NPim, Rim], writes=[pm_])
        for th_ in late:
            th_()
        for hb_ in range(2):
            S.op("dve", lambda e: e.tensor_tensor(out=Mg[:, hb_ * 4:(hb_ + 1) * 4, :], in0=pmgs[hb_][:, :].rearrange("p (a b) -> p a b", a=4), in1=cmask[:, :].unsqueeze(1).to_broadcast([128, 4, 128]), op=ALU.mult), reads=[pmgs[hb_], cmask], writes=[Mg])
        S.op("dve", lambda e: e.tensor_tensor(out=w1[:], in0=Xre[:], in1=Ec[:], op=ALU.mult), reads=[Xre, Ec], writes=[w1])
        S.op("dve", lambda e: e.tensor_tensor(out=w2[:], in0=Xim[:], in1=Es[:], op=ALU.mult), reads=[Xim, Es], writes=[w2])
        S.op("dve", lambda e: e.tensor_tensor(out=Gre[:], in0=w1[:], in1=w2[:], op=ALU.add), reads=[w1, w2], writes=[Gre])
        S.op("dve", lambda e: e.tensor_tensor(out=w1[:], in0=Xim[:], in1=Ec[:], op=ALU.mult), reads=[Xim, Ec], writes=[w1])
        S.op("dve", lambda e: e.tensor_tensor(out=w2[:], in0=Xre[:], in1=Es[:], op=ALU.mult), reads=[Xre, Es], writes=[w2])
        S.op("dve", lambda e: e.tensor_tensor(out=Gim[:], in0=w1[:], in1=w2[:], op=ALU.subtract), reads=[w1, w2], writes=[Gim])
        for j in range(8):
            g = g0 + j
            coef = r8[:, g:g + 1].to_broadcast([64, 256])
            S.op("dve", lambda e: e.tensor_tensor_scan(out=Sre[:, j, :], data0=coef, data1=Gre[:, j, :], initial=0.0, op0=ALU.mult, op1=ALU.add), reads=[r8, Gre], writes=[Sre])
            S.op("dve", lambda e: e.tensor_tensor_scan(out=Sim[:, j, :], data0=coef, data1=Gim[:, j, :], initial=0.0, op0=ALU.mult, op1=ALU.add), reads=[r8, Gim], writes=[Sim])
        S.op("dve", lambda e: e.tensor_tensor(out=w1[:], in0=Sre[:], in1=Ec[:], op=ALU.mult), reads=[Sre, Ec], writes=[w1])
        S.op("dve", lambda e: e.tensor_tensor(out=w2[:], in0=Sim[:], in1=Es[:], op=ALU.mult), reads=[Sim, Es], writes=[w2])
        S.op("dve", lambda e: e.tensor_tensor(out=Gre[:], in0=w1[:], in1=w2[:], op=ALU.subtract), reads=[w1, w2], writes=[Gre])
        S.op("dve", lambda e: e.tensor_tensor(out=w1[:], in0=Sre[:], in1=Es[:], op=ALU.mult), reads=[Sre, Es], writes=[w1])
        S.op("dve", lambda e: e.tensor_tensor(out=w2[:], in0=Sim[:], in1=Ec[:], op=ALU.mult), reads=[Sim, Ec], writes=[w2])
        S.op("dve", lambda e: e.tensor_tensor(out=Gim[:], in0=w1[:], in1=w2[:], op=ALU.add), reads=[w1, w2], writes=[Gim])
        if gb + 1 < NB:
            emit_cmul(gb + 1)
        for j in range(8):
            S.op("pe", lambda e: e.matmul(py[:, :], lhsT=Mg[:, j, :], rhs=Ug[:, j, 128:256], start=True, stop=False), reads=[Mg, Ug], writes=[py])
            S.op("pe", lambda e: e.matmul(py[:, :], lhsT=Rre[:, j, :], rhs=Gre[:, j, 127:255], start=False, stop=False), reads=[Rre, Gre], writes=[py])
            S.op("pe", lambda e: e.matmul(py[:, :], lhsT=Rim[:, j, :], rhs=Gim[:, j, 127:255], start=False, stop=True), reads=[Rim, Gim], writes=[py])
            Yg = Ygs.next()
            S.op("act", lambda e: e.activation(out=Yg[:, :], in_=py[:, :], func=AF.Copy), reads=[py], writes=[Yg])
            S.op("pe", lambda e: e.transpose(out=ptY[:, :], in_=Yg[:, :], identity=ident[:]), reads=[Yg, ident], writes=[ptY])
            S.op("act", lambda e: e.activation(out=Ytm[:, :, j * 16:(j + 1) * 16], in_=ptY[:, :].rearrange("p (t c) -> p t c", t=8), func=AF.Copy), reads=[ptY], writes=[Ytm])
        S.op("dve", lambda e: e.tensor_tensor(out=Ytmp[:].rearrange("p s (j c) -> p s j c", j=8), in0=Utm2[:, 1, :, :].rearrange("p j (s c) -> p s j c", s=8), in1=dbc[:, :].rearrange("p (j c) -> p j c", j=8).unsqueeze(1).to_broadcast([128, 8, 8, 16]), op=ALU.mult), reads=[Utm2, dbc], writes=[Ytmp])
        S.op("dve", lambda e: e.tensor_tensor(out=Ytmp[:], in0=Ytmp[:], in1=Ytm[:], op=ALU.add), reads=[Ytmp, Ytm], writes=[Ytmp])
        S.dma("sp", lambda e: e.dma_start(out=Yv[:, :, ch0:ch0 + 128], in_=Ytmp[:]), reads=[Ytmp])
    barrier(S)
    A.release(mk)

def host_ssm_tables(lam_re, lam_im, log_dt, b_re, b_im, c_re, c_im, d):
    G = lam_re.shape[0]
    f = lambda a: np.ascontiguousarray(a).astype(np.float32)
    mv = np.concatenate([7 - np.arange(8), -1 - np.arange(8), 1 + np.arange(8)]).astype(np.float32)
    s = np.arange(128) // 16
    cmask = (s[None, :] >= s[:, None]).astype(np.float32)
    return dict(lam_re=f(lam_re.T), lam_im=f(lam_im.T), logdt=f(np.broadcast_to(log_dt[None, :], (64, G))),
                b_re=f(b_re.transpose(1, 0, 2)), b_im=f(b_im.transpose(1, 0, 2)),
                c_re=f(c_re.transpose(2, 0, 1)), c_im=f(c_im.transpose(2, 0, 1)),
                d_bc=f(np.broadcast_to(d[None, :], (128, d.shape[0]))),
                mv24=f(np.broadcast_to(mv[None], (64, 24))), nidx=f(np.broadcast_to(np.arange(256, dtype=np.float32)[None], (64, 256))),
                cmask=cmask)

def transpose_plain(S, src, W, ident, ptpool, dst, dst_tok0, c0=0, eng="dve"):
    nch = W // 128
    for cg in range(0, nch, 4):
        n = min(4, nch - cg)
        pt = ptpool.next()
        for j in range(n):
            c = cg + j
            S.op("pe", lambda e, c=c, j=j: e.transpose(out=pt[:, j * 128:(j + 1) * 128], in_=src[:, c * 128:(c + 1) * 128], identity=ident[:]), reads=[src, ident], writes=[pt])
        S.op("dve", lambda e, cg=cg, n=n: e.tensor_copy(
            out=dst[:, c0 + cg:c0 + cg + n, dst_tok0:dst_tok0 + 128],
            in_=pt[:, 0:n * 128].rearrange("p (a b) -> p a b", a=n)), reads=[pt], writes=[dst])

def phase4(nc, S, A, c, T):
    mk = A.mark()
    D, Da, Ds, NC = c.D, c.Da, c.Ds, c.NC
    NA = Da // 128; NS = Ds // 128
    ident = build_identity(S, A)
    gout = A.sb("gout", [128, NC], F32)
    S.dma("sp", lambda e: e.dma_start(out=gout[:], in_=T.g_out[:, :]), writes=[gout])
    mixT = A.sb("mixT", [128, NC, 1024], BF16)
    mk2 = A.mark()
    wglu = A.sb("wglu", [128, NS, Ds], BF16)
    wg_view = T.w_glu.rearrange("(c p) n -> p c n", p=128)
    for cc in range(0, NS, 4):
        n = min(4, NS - cc)
        S.dma("pool", lambda e: e.dma_start(out=wglu[:, cc:cc + n, :], in_=wg_view[:, cc:cc + n, :]), writes=[wglu])
    yss = Pool([A.sb("ys%d" % i, [128, Ds], F32) for i in range(2)])
    yas = Pool([A.sb("ya%d" % i, [128, Da], F32) for i in range(2)])
    junk = A.sb("junk4", [128, max(Da, Ds)], BF16)
    ygTs = Pool([A.sb("ygT%d" % i, [128, NS, 128], BF16) for i in range(2)])
    sgs = Pool([A.sb("sg%d" % i, [128, 512], F32) for i in range(2)])
    ssq = Pool([A.sb("ssq4%d" % i, [128, 1], F32) for i in range(2)])
    rstd = Pool([A.sb("rstd4%d" % i, [128, 1], F32) for i in range(2)])
    ptp = Pool([A.ps("pt4%d" % i, [128, 512], F32) for i in range(2)])
    pgp = Pool([A.ps("pg4%d" % i, [128, 512], F32) for i in range(3)])
    GB = min(512, Ds)
    for tt in range(8):
        ys = yss.next(); ya = yas.next(); ygT = ygTs.next()
        S.dma("sp", lambda e: e.dma_start(out=ys[:], in_=T.YSSM[tt * 128:(tt + 1) * 128, :]), writes=[ys])
        S.dma("sp", lambda e: e.dma_start(out=ya[:], in_=T.YATT[tt * 128:(tt + 1) * 128, :]), writes=[ya])
        S.op("act", lambda e: e.activation(out=ys[:], in_=ys[:], func=AF.Gelu), reads=[ys], writes=[ys])
        transpose_plain(S, ys, Ds, ident, ptp, ygT, 0)
        for jb in range(Ds // GB):
            pg = pgp.next(); sg = sgs.next()
            for cc in range(NS):
                S.op("pe", lambda e: e.matmul(pg[:, 0:GB], lhsT=ygT[:, cc, :], rhs=wglu[:, cc, jb * GB:(jb + 1) * GB], start=(cc == 0), stop=(cc == NS - 1)), reads=[ygT, wglu], writes=[pg])
            S.op("act", lambda e: e.activation(out=sg[:, 0:GB], in_=pg[:, 0:GB], func=AF.Sigmoid), reads=[pg], writes=[sg])
            S.op("dve", lambda e: e.tensor_tensor(out=ys[:, jb * GB:(jb + 1) * GB], in0=ys[:, jb * GB:(jb + 1) * GB], in1=sg[:, 0:GB], op=ALU.mult), reads=[ys, sg], writes=[ys])
        sq = ssq.next(); rs = rstd.next()
        rms_rstd(S, ys, Ds, junk, sq, rs)
        S.op("act", lambda e: e.activation(out=ys[:], in_=ys[:], func=AF.Copy, scale=rs[:, 0:1]), reads=[ys, rs], writes=[ys])
        transpose_to(S, ys, Ds, ident, ptp, mixT, tt * 128, gout, c0=NA)
        sq = ssq.next(); rs = rstd.next()
        rms_rstd(S, ya, Da, junk, sq, rs)
        S.op("act", lambda e: e.activation(out=ya[:], in_=ya[:], func=AF.Copy, scale=rs[:, 0:1]), reads=[ya, rs], writes=[ya])
        transpose_to(S, ya, Da, ident, ptp, mixT, tt * 128, gout, c0=0)
    barrier(S)
    A.release(mk2)
    OB = min(512, D)
    wos = Pool([A.sb("wo%d" % i, [128, NC, OB], BF16) for i in range(2)])
    xrs = Pool([A.sb("xr%d" % i, [128, OB], F32) for i in range(3)])
    pop = Pool([A.ps("po4%d" % i, [128, 512], F32) for i in range(4)])
    wo_view = T.w_out.rearrange("(c p) n -> p c n", p=128)
    for db in range(D // OB):
        wo = wos.next()
        S.dma("pool", lambda e: e.dma_start(out=wo[:], in_=wo_view[:, :, db * OB:(db + 1) * OB]), writes=[wo])
        for tt in range(8):
            po = pop.next(); xr = xrs.next()
            S.dma("sp", lambda e: e.dma_start(out=xr[:], in_=T.xall[1024 + tt * 128:1024 + (tt + 1) * 128, db * OB:(db + 1) * OB]), writes=[xr])
            for cc in range(NC):
                S.op("pe", lambda e: e.matmul(po[:, 0:OB], lhsT=mixT[:, cc, tt * 128:(tt + 1) * 128], rhs=wo[:, cc, :], start=(cc == 0), stop=(cc == NC - 1)), reads=[mixT, wo], writes=[po])
            S.op("dve", lambda e: e.tensor_tensor(out=xr[:], in0=xr[:], in1=po[:, 0:OB], op=ALU.add), reads=[xr, po], writes=[xr])
            S.dma("sp", lambda e: e.dma_start(out=T.X1[tt * 128:(tt + 1) * 128, db * OB:(db + 1) * OB], in_=xr[:]), reads=[xr])
    barrier(S)
    A.release(mk)

def phase5(nc, S, A, c, T):
    D, NC = c.D, c.NC
    mk = A.mark()
    ident = build_identity(S, A)
    gT = A.sb("gffnT", [128, NC], F32); gbc = A.sb("gffnbc", [128, D], F32)
    S.dma("sp", lambda e: e.dma_start(out=gT[:], in_=T.g_ffnT[:, :]), writes=[gT])
    S.dma("sp", lambda e: e.dma_start(out=gbc[:], in_=T.g_ffn_bc[:, :]), writes=[gbc])
    hn2T = A.sb("hn2T", [128, NC, 1024], BF16)
    xts = Pool([A.sb("x5_%d" % i, [128, D], F32) for i in range(2)])
    junk = A.sb("junk5", [128, D], BF16)
    ssq = Pool([A.sb("ssq5%d" % i, [128, 1], F32) for i in range(2)])
    rstd = Pool([A.sb("rstd5%d" % i, [128, 1], F32) for i in range(2)])
    ptp = Pool([A.ps("pt5%d" % i, [128, 512], F32) for i in range(2)])
    pmp = Pool([A.ps("pm5%d" % i, [128, 512], F32) for i in range(4)])
    for tt in range(8):
        xt = xts.next(); sq = ssq.next(); rs = rstd.next()
        S.dma("sp", lambda e: e.dma_start(out=xt[:], in_=T.X1[tt * 128:(tt + 1) * 128, :]), writes=[xt])
        rms_rstd(S, xt, D, junk, sq, rs)
        S.op("act", lambda e: e.activation(out=xt[:], in_=xt[:], func=AF.Copy, scale=rs[:, 0:1]), reads=[xt, rs], writes=[xt])
        transpose_to(S, xt, D, ident, ptp, hn2T, tt * 128, gT)
        S.op("dve", lambda e: e.tensor_tensor(out=junk[:], in0=xt[:], in1=gbc[:], op=ALU.mult), reads=[xt, gbc], writes=[junk])
        S.dma("sp", lambda e: e.dma_start(out=T.HN2[tt * 128:(tt + 1) * 128, :], in_=junk[:]), reads=[junk])
    wqs = Pool([A.sb("wq%d" % i, [128, NC, 512], BF16) for i in range(2)])
    stq = Pool([A.sb("stq5%d" % i, [128, 1024], F32) for i in range(2)])
    wq_view = T.w_q.rearrange("(c p) n -> p c n", p=128)
    for cb in range(4):
        wq = wqs.next()
        S.dma("pool", lambda e: e.dma_start(out=wq[:], in_=wq_view[:, :, cb * 512:(cb + 1) * 512]), writes=[wq])
        for j in range(4):
            cq = cb * 4 + j
            st = stq.next()
            for half in range(2):
                pm = pmp.next()
                for cc in range(NC):
                    S.op("pe", lambda e: e.matmul(pm[:, :], lhsT=wq[:, cc, j * 128:(j + 1) * 128], rhs=hn2T[:, cc, half * 512:(half + 1) * 512], start=(cc == 0), stop=(cc == NC - 1)), reads=[wq, hn2T], writes=[pm])
                if half == 0:
                    S.op("act", lambda e: e.activation(out=st[:, 0:512], in_=pm[:, :], func=AF.Copy), reads=[pm], writes=[st])
                else:
                    S.op("dve", lambda e: e.tensor_copy(out=st[:, 512:1024], in_=pm[:, :]), reads=[pm], writes=[st])
            S.dma("sp", lambda e: e.dma_start(out=T.QPT[cq, :, :], in_=st[:, :]), reads=[st])
    convert_tables(S, T, 1 << 30)
    barrier(S, include_conv=True)
    A.release(mk)
    mk = A.mark()
    identb = build_identity(S, A, BF16, "identb5")
    NDB = D // 512
    pacc = [A.ps("pacc%d" % i, [128, 512], F32) for i in range(8)]
    mk2 = A.mark()
    keysT = A.sb("keysT", [128, 16, 128], F32)
    S.dma("sp", lambda e: e.dma_start(out=keysT[:], in_=T.keysT[:, :, :]), writes=[keysT])
    qts = Pool([A.sb("qt5%d" % i, [128, 16, 128], F32) for i in range(2)])
    scs = Pool([A.sb("scs%d" % i, [128, 16, 128], F32) for i in range(2)])
    QPv = T.QPT.rearrange("c p t -> p c t")
    for tt in range(8):
        r0 = tt * 128
        qt = qts.next(); sct = scs.next()
        S.dma("sp", lambda e: e.dma_start(out=qt[:], in_=QPv[:, :, r0:r0 + 128]), writes=[qt])
        for cq in range(16):
            ps = pacc[(tt % 2) * 4 + cq // 4]
            S.op("pe", lambda e: e.matmul(ps[:, (cq % 4) * 128:(cq % 4) * 128 + 128], lhsT=qt[:, cq, :], rhs=keysT[:, cq, :], start=True, stop=True), reads=[qt, keysT], writes=[ps])
        for b4 in range(4):
            ps = pacc[(tt % 2) * 4 + b4]
            S.op("act", lambda e: e.activation(out=sct[:, b4 * 4:(b4 + 1) * 4, :], in_=ps[:, :].rearrange("p (a b) -> p a b", a=4), func=AF.Copy), reads=[ps], writes=[sct])
        S.dma("sp", lambda e: e.dma_start(out=T.SCD[r0:r0 + 128, :, :], in_=sct[:]), reads=[sct])
    barrier(S)
    A.release(mk2)
    gpool = Pool([A.sb("gth%d" % i, [128, 2 * D], BF16) for i in range(7)])
    prods = Pool([A.sb("prod%d" % i, [128, D], BF16) for i in range(2)])
    junk2 = A.sb("junk5d", [128, D], BF16)
    PUVv = T.PUV.rearrange("e two d -> e (two d)")
    hgs = Pool([A.sb("hg%d" % i, [128, D], BF16) for i in range(1)])
    xb = A.sb("xb5", [128, D], F32); gfb = A.sb("gfb", [128, D], F32)
    accA = xb
    S.dma("sp", lambda e: e.dma_start(out=gfb[:], in_=T.g_fin_bc[:, :]), writes=[gfb])
    junk = prods.bufs[0]
    sc = A.sb("sc", [128, 16, 128], F32)
    scr = A.sb("scr", [128, 128], F32)
    va = A.sb("va", [128, 16], F32); vb = A.sb("vb", [128, 16], F32)
    iu = A.sb("iu", [128, 16], U32); iaf = A.sb("iaf", [128, 16], F32); ibf = A.sb("ibf", [128, 16], F32)
    cand = A.sb("cand", [128, 256], F32); ecand = A.sb("ecand", [128, 256], F32); cscr = A.sb("cscr", [128, 256], F32)
    vbst = A.sb("vbst", [128, 16], F32); nmx = A.sb("nmx", [128, 1], F32); ex = A.sb("ex", [128, 16], F32)
    zz = A.sb("zz", [128, 1], F32)
    ef = A.sb("ef", [128, 128], F32)
    pu = A.sb("pu", [128, 16], U32); pi_ = A.sb("pi_", [128, 16], U32); pj_ = A.sb("pj_", [128, 16], U32)
    pif = A.sb("pif", [128, 16], F32); pjf = A.sb("pjf", [128, 16], F32); eaf = A.sb("eaf", [128, 16], F32); ebf = A.sb("ebf", [128, 16], F32)
    sel3 = A.sb("sel3", [128, 16, 16], F32)
    iota16 = A.sb("iota16", [128, 16], F32)
    S.dma("sp", lambda e: e.dma_start(out=iota16[:], in_=T.iota16[:, :]), writes=[iota16])
    eis = Pool([A.sb("ei%d" % i, [128, 128], I32) for i in range(2)])
    ggs = Pool([A.sb("gg%d" % i, [128, 128], F32) for i in range(2)])
    araws = Pool([A.sb("araw%d" % i, [128, 1], F32) for i in range(8)])
    wgs = Pool([A.sb("wg%d" % i, [128, 1], F32) for i in range(8)])
    dgs = Pool([A.sb("dg%d" % i, [128, 128], BF16) for i in range(6)])
    sq = A.sb("ssq5c", [128, 1], F32); rs = A.sb("rstd5c", [128, 1], F32)

    def topk_ops(tt, ei, gg):
        ops = []
        r0 = tt * 128
        ops.append(lambda: S.dma("sp", lambda e: e.dma_start(out=sc[:], in_=T.SCD[r0:r0 + 128, :, :]), writes=[sc]))
        def top16(src_ap, vals, idxf):
            ops.append(lambda: S.op("dve", lambda e: e.max(out=vals[:, 0:8], in_=src_ap), reads=[sc], writes=[vals]))
            ops.append(lambda: S.op("dve", lambda e: e.max_index(out=iu[:, 0:8], in_max=vals[:, 0:8], in_values=src_ap), reads=[sc, vals], writes=[iu]))
            ops.append(lambda: S.op("dve", lambda e: e.match_replace(out=scr[:, :], in_to_replace=vals[:, 0:8], in_values=src_ap, imm_value=-1e30), reads=[sc, vals], writes=[scr]))
            ops.append(lambda: S.op("dve", lambda e: e.max(out=vals[:, 8:16], in_=scr[:, :]), reads=[scr], writes=[vals]))
            ops.append(lambda: S.op("dve", lambda e: e.max_index(out=iu[:, 8:16], in_max=vals[:, 8:16], in_values=scr[:, :]), reads=[scr, vals], writes=[iu]))
            ops.append(lambda: S.op("dve", lambda e: e.tensor_copy(out=idxf[:, :], in_=iu[:, :]), reads=[iu], writes=[idxf]))
        c3 = [128, 16, 16]; m3 = [128, 16, 256]
        for h in range(8):
            top16(sc[:, 2 * h, :], va, iaf)
            top16(sc[:, 2 * h + 1, :], vb, ibf)
            ops.append(lambda: S.op("dve", lambda e: e.tensor_tensor(out=cand[:, :].rearrange("p (a b) -> p a b", a=16), in0=va[:, :].unsqueeze(2).to_broadcast(c3), in1=vb[:, :].unsqueeze(1).to_broadcast(c3), op=ALU.add), reads=[va, vb], writes=[cand]))
            ops.append(lambda: S.op("dve", lambda e: e.max(out=vbst[:, 0:8], in_=cand[:, :]), reads=[cand], writes=[vbst]))
            ops.append(lambda: S.op("dve", lambda e: e.max_index(out=pu[:, 0:8], in_max=vbst[:, 0:8], in_values=cand[:, :]), reads=[cand, vbst], writes=[pu]))
            ops.append(lambda: S.op("dve", lambda e: e.match_replace(out=cscr[:, :], in_to_replace=vbst[:, 0:8], in_values=cand[:, :], imm_value=-1e30), reads=[cand, vbst], writes=[cscr]))
            ops.append(lambda: S.op("dve", lambda e: e.max(out=vbst[:, 8:16], in_=cscr[:, :]), reads=[cscr], writes=[vbst]))
            ops.append(lambda: S.op("dve", lambda e: e.max_index(out=pu[:, 8:16], in_max=vbst[:, 8:16], in_values=cscr[:, :]), reads=[cscr, vbst], writes=[pu]))
            ops.append(lambda: S.op("dve", lambda e: e.tensor_single_scalar(out=pi_[:, :], in_=pu[:, :], scalar=4, op=ALU.logical_shift_right), reads=[pu], writes=[pi_]))
            ops.append(lambda: S.op("dve", lambda e: e.tensor_single_scalar(out=pj_[:, :], in_=pu[:, :], scalar=15, op=ALU.bitwise_and), reads=[pu], writes=[pj_]))
            ops.append(lambda: S.op("dve", lambda e: e.tensor_copy(out=pif[:, :], in_=pi_[:, :]), reads=[pi_], writes=[pif]))
            ops.append(lambda: S.op("dve", lambda e: e.tensor_copy(out=pjf[:, :], in_=pj_[:, :]), reads=[pj_], writes=[pjf]))
            ops.append(lambda: S.op("dve", lambda e: e.tensor_tensor(out=sel3[:, :, :], in0=pif[:, :].unsqueeze(2).to_broadcast(c3), in1=iota16[:, :].unsqueeze(1).to_broadcast(c3), op=ALU.is_equal), reads=[pif, iota16], writes=[sel3]))
            ops.append(lambda: S.op("dve", lambda e: e.tensor_tensor(out=sel3[:, :, :], in0=sel3[:, :, :], in1=iaf[:, :].unsqueeze(1).to_broadcast(c3), op=ALU.mult), reads=[sel3, iaf], writes=[sel3]))
            ops.append(lambda: S.op("dve", lambda e: e.tensor_reduce(out=eaf[:, :], in_=sel3[:, :, :], axis=AX.X, op=ALU.add), reads=[sel3], writes=[eaf]))
            ops.append(lambda: S.op("dve", lambda e: e.tensor_tensor(out=sel3[:, :, :], in0=pjf[:, :].unsqueeze(2).to_broadcast(c3), in1=iota16[:, :].unsqueeze(1).to_broadcast(c3), op=ALU.is_equal), reads=[pjf, iota16], writes=[sel3]))
            ops.append(lambda: S.op("dve", lambda e: e.tensor_tensor(out=sel3[:, :, :], in0=sel3[:, :, :], in1=ibf[:, :].unsqueeze(1).to_broadcast(c3), op=ALU.mult), reads=[sel3, ibf], writes=[sel3]))
            ops.append(lambda: S.op("dve", lambda e: e.tensor_reduce(out=ebf[:, :], in_=sel3[:, :, :], axis=AX.X, op=ALU.add), reads=[sel3], writes=[ebf]))
            ops.append(lambda h=h: S.op("dve", lambda e: e.scalar_tensor_tensor(out=ef[:, h * 16:(h + 1) * 16], in0=eaf[:, :], scalar=128.0, in1=ebf[:, :], op0=ALU.mult, op1=ALU.add), reads=[eaf, ebf], writes=[ef]))
            ops.append(lambda: S.op("dve", lambda e: e.tensor_scalar(out=nmx[:, :], in0=vbst[:, 0:1], scalar1=-1.0, scalar2=None, op0=ALU.mult), reads=[vbst], writes=[nmx]))
            ops.append(lambda: S.op("act", lambda e: e.activation(out=ex[:, :], in_=vbst[:, :], func=AF.Exp, bias=nmx[:, 0:1], scale=1.0), reads=[vbst, nmx], writes=[ex]))
            ops.append(lambda: S.op("dve", lambda e: e.tensor_reduce(out=zz[:, :], in_=ex[:, :], axis=AX.X, op=ALU.add), reads=[ex], writes=[zz]))
            ops.append(lambda: S.op("dve", lambda e: e.reciprocal(out=zz[:, :], in_=zz[:, :]), reads=[zz], writes=[zz]))
            ops.append(lambda h=h: S.op("dve", lambda e: e.tensor_scalar(out=gg[:, h * 16:(h + 1) * 16], in0=ex[:, :], scalar1=zz[:, 0:1], scalar2=None, op0=ALU.mult), reads=[ex, zz], writes=[gg]))
        ops.append(lambda: S.op("dve", lambda e: e.tensor_scalar(out=ef[:, :], in0=ef[:, :], scalar1=0.0, scalar2=float(c.NE - 1), op0=ALU.max, op1=ALU.min), reads=[ef], writes=[ef]))
        ops.append(lambda: S.op("dve", lambda e: e.tensor_copy(out=ei[:, :], in_=ef[:, :]), reads=[ef], writes=[ei]))
        return ops

    ei = eis.next(); gg = ggs.next()
    for th in topk_ops(0, ei, gg):
        th()
    LAG = 2
    for tt in range(8):
        r0 = tt * 128
        hg = hgs.next()
        S.dma("sp", lambda e: e.dma_start(out=hg[:], in_=T.HN2[r0:r0 + 128, :]), writes=[hg])
        S.dma("sp", lambda e: e.dma_start(out=xb[:], in_=T.X1[r0:r0 + 128, :]), writes=[xb])
        if tt + 1 < 8:
            ei_n = eis.next(); gg_n = ggs.next()
            nxt = topk_ops(tt + 1, ei_n, gg_n)
        else:
            nxt = []
        per = (len(nxt) + 99) // 100 if nxt else 0
        stage = {}
        def emit_dot(hk):
            gb = gpool.next(); ar = araws.next(); wg = wgs.next()
            S.dma("pool", lambda e: e.indirect_dma_start(out=gb[:], out_offset=None, in_=PUVv, in_offset=bass.IndirectOffsetOnAxis(ap=ei[:, hk:hk + 1], axis=0)), reads=[ei], writes=[gb])
            if hk % 3 == 0:
                S.op("dve", lambda e: e.scalar_tensor_tensor(out=junk[:, :], in0=hg[:, :], scalar=1.0, in1=gb[:, 0:D], op0=ALU.mult, op1=ALU.mult, accum_out=ar[:, 0:1]), reads=[hg, gb], writes=[junk, ar])
            else:
                pr = prods.next()
                S.op("dve", lambda e: e.tensor_tensor(out=pr[:, :], in0=hg[:, :], in1=gb[:, 0:D], op=ALU.mult), reads=[hg, gb], writes=[pr])
                S.op("act", lambda e: e.activation(out=junk2[:, :], in_=pr[:, :], func=AF.Copy, accum_out=ar[:, 0:1]), reads=[pr], writes=[junk2, ar])
            S.op("act", lambda e: e.activation(out=wg[:, 0:1], in_=ar[:, 0:1], func=AF.Gelu), reads=[ar], writes=[wg])
            stage[hk] = (gb, wg)
        def emit_acc(hk):
            gb, wg = stage.pop(hk)
            dgk = dgs.next()
            S.op("dve", lambda e: e.tensor_scalar(out=dgk[:, :], in0=identb[:, :], scalar1=wg[:, 0:1], scalar2=gg[:, hk:hk + 1], op0=ALU.mult, op1=ALU.mult), reads=[identb, wg, gg], writes=[dgk])
            for db in range(NDB):
                S.op("pe", lambda e: e.matmul(pacc[db][:, :], lhsT=dgk[:, :], rhs=gb[:, D + db * 512:D + (db + 1) * 512], start=(hk == 0), stop=(hk == 127)), reads=[dgk, gb], writes=[pacc[db]])
        for step in range(128 + LAG):
            if step < 128:
                emit_dot(step)
            if step - LAG >= 0:
                emit_acc(step - LAG)
            for _ in range(per):
                if nxt:
                    nxt.pop(0)()
        while nxt:
            nxt.pop(0)()
        for db in range(NDB):
            S.op("dve", lambda e: e.tensor_tensor(out=accA[:, db * 512:(db + 1) * 512], in0=xb[:, db * 512:(db + 1) * 512], in1=pacc[db][:, :], op=ALU.add), reads=[xb, pacc[db]], writes=[accA])
        rms_rstd(S, accA, D, junk, sq, rs)
        S.op("act", lambda e: e.activation(out=accA[:, :], in_=accA[:, :], func=AF.Copy, scale=rs[:, 0:1]), reads=[accA, rs], writes=[accA])
        S.op("dve", lambda e: e.tensor_tensor(out=accA[:, :], in0=accA[:, :], in1=gfb[:, :], op=ALU.mult), reads=[accA, gfb], writes=[accA])
        S.dma("sp", lambda e: e.dma_start(out=T.OUT[r0:r0 + 128, :], in_=accA[:, :]), reads=[accA])
        if tt + 1 < 8:
            ei = ei_n; gg = gg_n
    barrier(S)
    A.release(mk)

def convert_tables(S, T, n):
    st = T.conv_state
    while n > 0 and st[0] < len(st[1]):
        src, which, r = st[1][st[0]]
        S.dma("conv", lambda e: e.dma_start(out=T.PUV[r:r + 128, which, :], in_=src[r:r + 128, :]))
        st[0] += 1; n -= 1


def build_program(D=4096):
    c = make_cfg(D)
    nc = bass.Bass("TRN2", target_bir_lowering=False)
    T = Ctx()
    G = c.G
    ext_in = dict(xall=[2048, D], g_mix=[128, c.NC], w_in=[D, 2 * D],
                  abias=[c.H, 128, 4, 256], cbias=[128, c.H], eladd=[128, 8, 8], eligp=[128, 8, 8], ownm=[128, 8, 8],
                  lam_re=[64, G], lam_im=[64, G], logdt=[64, G], b_re=[64, G, 16], b_im=[64, G, 16], c_re=[64, G, 16], c_im=[64, G, 16],
                  d_bc=[128, c.Ds], mv24=[64, 24], nidx=[64, 256], cmask=[128, 128],
                  w_glu=[c.Ds, c.Ds], g_out=[128, c.NC], w_out=[D, D],
                  g_ffnT=[128, c.NC], g_ffn_bc=[128, D], w_q=[D, 2048], keysT=[128, 16, 128], peer_u=[16384, D], peer_v=[16384, D], g_fin_bc=[128, D], iota16=[128, 16])
    for k, s in ext_in.items():
        setattr(T, k, nc.dram_tensor(k, s, F32, kind="ExternalInput").ap())
    T.QT = nc.dram_tensor("QT", [c.H, 128, 1024], BF16, kind="Internal").ap()
    T.KT = nc.dram_tensor("KT", [c.H, 128, 2048], BF16, kind="Internal").ap()
    T.V = nc.dram_tensor("V", [2048, c.Da], BF16, kind="Internal").ap()
    T.U = nc.dram_tensor("U", [2048, c.Ds], F32, kind="Internal").ap()
    T.YATT = nc.dram_tensor("YATT", [1024, c.Da], F32, kind="Internal").ap()
    T.YSSM = nc.dram_tensor("YSSM", [1024, c.Ds], F32, kind="Internal").ap()
    T.X1 = nc.dram_tensor("X1", [1024, D], F32, kind="Internal").ap()
    T.HN2 = nc.dram_tensor("HN2", [1024, D], BF16, kind="Internal").ap()
    T.PUV = nc.dram_tensor("PUV", [16384, 2, D], BF16, kind="Internal").ap()
    T.SCD = nc.dram_tensor("SCD", [1024, 16, 128], F32, kind="Internal").ap()
    T.conv_state = [0, [(T.peer_u, 0, r) for r in range(0, 16384, 128)] + [(T.peer_v, 1, r) for r in range(0, 16384, 128)]]
    T.conv_per_step = 1
    T.conv_p1 = 2
    T.conv_p3 = 5
    T.QPT = nc.dram_tensor("QPT", [16, 128, 1024], F32, kind="Internal").ap()
    T.OUT = nc.dram_tensor("OUT", [1024, D], F32, kind="ExternalOutput").ap()
    S = Sched(nc); A = Alloc(nc)
    for ph in (phase1, phase2, phase3, phase4, phase5):
        ph(nc, S, A, c, T)
    return nc, c


from concourse.bass_utils import run_bass_kernel_spmd

_PROG = {}

def _bc(v, n=128):
    return np.ascontiguousarray(np.broadcast_to(np.asarray(v, np.float32)[None, :], (n, v.shape[0])))

def _fm(v):
    v = np.asarray(v, np.float32)
    return np.ascontiguousarray(v.reshape(-1, 128).T)

def kernel(x, norm_mix_gain, w_in, rel_bias, ssm_lambda_re, ssm_lambda_im, ssm_log_dt,
           ssm_b_re, ssm_b_im, ssm_c_re, ssm_c_im, ssm_d, ssm_w_glu, attn_out_gain,
           ssm_out_gain, w_out, norm_ffn_gain, peer_w_q, peer_keys_a, peer_keys_b,
           peer_u, peer_v, norm_final_gain):
    f32 = lambda a: np.ascontiguousarray(np.asarray(a, dtype=np.float32))
    x = f32(x)
    B, SEQ, D = x.shape
    assert B == 4 and SEQ == 2048
    if D not in _PROG:
        _PROG[D] = build_program(D)
    nc, c = _PROG[D]
    l = 0
    shared = dict(
        g_mix=_fm(norm_mix_gain[l]), w_in=f32(w_in[l]),
        w_glu=f32(ssm_w_glu[l]),
        g_out=_fm(np.concatenate([np.asarray(attn_out_gain[l], np.float32), np.asarray(ssm_out_gain[l], np.float32)])),
        w_out=f32(w_out[l]),
        g_ffnT=_fm(norm_ffn_gain[l]), g_ffn_bc=_bc(np.asarray(norm_ffn_gain[l], np.float32)),
        w_q=f32(peer_w_q[l]), peer_u=f32(peer_u[l]), peer_v=f32(peer_v[l]),
        g_fin_bc=_bc(np.asarray(norm_final_gain, np.float32)),
        iota16=_bc(np.arange(16, dtype=np.float32)),
    )
    ka = np.asarray(peer_keys_a[l], np.float32); kb = np.asarray(peer_keys_b[l], np.float32)
    keysT = np.empty((128, 16, 128), np.float32)
    for h in range(8):
        keysT[:, 2 * h, :] = ka[h].T
        keysT[:, 2 * h + 1, :] = kb[h].T
    shared["keysT"] = keysT
    shared.update(host_ssm_tables(np.asarray(ssm_lambda_re[l], np.float32), np.asarray(ssm_lambda_im[l], np.float32),
                                  np.asarray(ssm_log_dt[l], np.float32), np.asarray(ssm_b_re[l], np.float32),
                                  np.asarray(ssm_b_im[l], np.float32), np.asarray(ssm_c_re[l], np.float32),
                                  np.asarray(ssm_c_im[l], np.float32), np.asarray(ssm_d[l], np.float32)))
    rb = np.asarray(rel_bias, np.float32)
    attn_tabs = [host_attn_tables(rb, half) for half in range(2)]
    in_maps = []
    for core in range(8):
        b, half = core // 2, core % 2
        xall = np.zeros((2048, D), np.float32)
        if half == 1:
            xall[:1024] = x[b, :1024]
        xall[1024:] = x[b, half * 1024:(half + 1) * 1024]
        ab, cbias, eladd, eligp, ownm = attn_tabs[half]
        m = dict(shared)
        m.update(xall=xall, abias=ab, cbias=cbias, eladd=eladd, eligp=eligp, ownm=ownm)
        in_maps.append(m)
    res = run_bass_kernel_spmd(nc, in_maps, core_ids=list(range(8)))
    out = np.empty((B, SEQ, D), np.float32)
    for core in range(8):
        b, half = core // 2, core % 2
        out[b, half * 1024:(half + 1) * 1024] = np.asarray(res.results[core]["OUT"], np.float32)
    return out
```
